# Optimizing a Trainium2 kernel written in Bass

```python
import jax, jax.numpy as jnp
from jax import lax
import numpy as np

D_MODEL = 1024
BATCH = 4
SEQ = 8192
DEPTH = 1

HEAD_DIM = 64
SB_HEADS = 8
SWA_HEADS = 8
SWA_KV_HEADS = 2
SWA_GROUP = SWA_HEADS // SWA_KV_HEADS
SB_WIDTH = SB_HEADS * HEAD_DIM
SWA_Q_WIDTH = SWA_HEADS * HEAD_DIM
SWA_KV_WIDTH = SWA_KV_HEADS * HEAD_DIM
MIX_WIDTH = SB_WIDTH + SWA_Q_WIDTH
IN_PROJ_WIDTH = 3 * SB_WIDTH + SWA_Q_WIDTH + 2 * SWA_KV_WIDTH
WINDOW = 128
BLOCK = 128
ROPE_THETA = 10000.0
N_KEYS = 128
N_EXPERTS = N_KEYS * N_KEYS
PEER_HEADS = 8
PEER_QUERY_DIM = 256
PEER_HALF = PEER_QUERY_DIM // 2
PEER_TOPK = 16
PEER_CHUNK = 128
EPS = 1e-6

kernel_name = "hymba_stickbreak_swa_sink_peer"


def rms_norm(x, g):
    xf = x.astype(jnp.float32)
    y = xf * lax.rsqrt(jnp.mean(xf * xf, axis=-1, keepdims=True) + EPS)
    return (y * g.astype(jnp.float32)).astype(x.dtype)


def rope(x, positions):
    half = HEAD_DIM // 2
    inv_freq = ROPE_THETA ** (-jnp.arange(half, dtype=jnp.float32) / half)
    ang = positions.astype(jnp.float32)[..., None] * inv_freq
    cos = jnp.cos(ang)[:, :, None, :]
    sin = jnp.sin(ang)[:, :, None, :]
    xf = x.astype(jnp.float32)
    x1, x2 = xf[..., :half], xf[..., half:]
    out = jnp.concatenate([x1 * cos - x2 * sin, x2 * cos + x1 * sin], axis=-1)
    return out.astype(x.dtype)


def stick_breaking_attention(q, k, v):
    B, S, H, D = q.shape
    nblk = S // BLOCK
    scale = D ** -0.5
    qb = q.reshape(B, nblk, BLOCK, H, D).transpose(1, 0, 3, 2, 4)
    kh = k.transpose(0, 2, 1, 3)
    vh = v.transpose(0, 2, 1, 3)
    key_pos = jnp.arange(S)

    def one_block(args):
        i, qi = args
        q_pos = i * BLOCK + jnp.arange(BLOCK)
        z = jnp.einsum('bhqd,bhkd->bhqk', qi, kh).astype(jnp.float32) * scale
        causal = key_pos[None, :] < q_pos[:, None]
        log_one_minus = jnp.where(causal, -jax.nn.softplus(z), 0.0)
        after = lax.cumsum(log_one_minus, axis=3, reverse=True) - log_one_minus
        w = jnp.where(causal, jnp.exp(jax.nn.log_sigmoid(z) + after), 0.0)
        return jnp.einsum('bhqk,bhkd->bhqd', w.astype(v.dtype), vh)

    out = lax.map(one_block, (jnp.arange(nblk), qb))
    return out.transpose(1, 0, 3, 2, 4).reshape(B, S, H, D)


def sliding_window_sink_attention(q, k, v, sinks):
    B, S, Hq, D = q.shape
    nblk = S // BLOCK
    scale = D ** -0.5
    qb = q.reshape(B, nblk, BLOCK, SWA_KV_HEADS, SWA_GROUP, D)
    kb = k.reshape(B, nblk, BLOCK, SWA_KV_HEADS, D)
    vb = v.reshape(B, nblk, BLOCK, SWA_KV_HEADS, D)
    pad_k = jnp.zeros_like(kb[:, :1])
    pad_v = jnp.zeros_like(vb[:, :1])
    kw = jnp.concatenate([jnp.concatenate([pad_k, kb[:, :-1]], axis=1), kb], axis=2)
    vw = jnp.concatenate([jnp.concatenate([pad_v, vb[:, :-1]], axis=1), vb], axis=2)
    z = jnp.einsum('bnqhgd,bnkhd->bnhgqk', qb, kw).astype(jnp.float32) * scale
    q_idx = jnp.arange(BLOCK)[:, None] + BLOCK
    k_idx = jnp.arange(2 * BLOCK)[None, :]
    diff = q_idx - k_idx
    band = (diff >= 0) & (diff < WINDOW)
    not_pad = (jnp.arange(nblk)[:, None, None] > 0) | (k_idx[None] >= BLOCK)
    mask = band[None] & not_pad
    z = jnp.where(mask[None, :, None, None], z, -jnp.inf)
    sink = sinks.astype(jnp.float32).reshape(SWA_KV_HEADS, SWA_GROUP)[None, None, :, :, None, None]
    sink = jnp.broadcast_to(sink, z.shape[:-1] + (1,))
    p = jax.nn.softmax(jnp.concatenate([z, sink], axis=-1), axis=-1)[..., :-1]
    out = jnp.einsum('bnhgqk,bnkhd->bnqhgd', p.astype(v.dtype), vw)
    return out.reshape(B, S, Hq, D)


def peer_ffn(x, w_query, sub_keys_1, sub_keys_2, expert_down, expert_up):
    B, S, D = x.shape
    T = B * S
    xt = x.reshape(T // PEER_CHUNK, PEER_CHUNK, D)

    def one_chunk(xc):
        q = (xc @ w_query).reshape(PEER_CHUNK, PEER_HEADS, PEER_QUERY_DIM)
        q1, q2 = q[..., :PEER_HALF], q[..., PEER_HALF:]
        s1 = jnp.einsum('thd,hkd->thk', q1, sub_keys_1).astype(jnp.float32)
        s2 = jnp.einsum('thd,hkd->thk', q2, sub_keys_2).astype(jnp.float32)
        v1, i1 = lax.top_k(s1, PEER_TOPK)
        v2, i2 = lax.top_k(s2, PEER_TOPK)
        cand = (v1[..., :, None] + v2[..., None, :]).reshape(PEER_CHUNK, PEER_HEADS, PEER_TOPK * PEER_TOPK)
        cand_idx = (i1[..., :, None] * N_KEYS + i2[..., None, :]).reshape(PEER_CHUNK, PEER_HEADS, PEER_TOPK * PEER_TOPK)
        top_s, pos = lax.top_k(cand, PEER_TOPK)
        idx = jnp.take_along_axis(cand_idx, pos, axis=-1)
        gate = jax.nn.softmax(top_s, axis=-1)
        u = expert_down[idx]
        hidden = jax.nn.gelu(jnp.einsum('thkd,td->thk', u, xc).astype(jnp.float32), approximate=False)
        w = (gate * hidden).astype(x.dtype)
        vsel = expert_up[idx]
        return jnp.einsum('thk,thkd->td', w, vsel)

    out = lax.map(one_chunk, xt)
    return out.reshape(B, S, D)


def setup_inputs(seed: int = 0) -> dict:
    key = jax.random.key(seed)
    ks = jax.random.split(key, 16)
    f32 = jnp.float32
    x = jax.random.normal(ks[0], (BATCH, SEQ, D_MODEL), f32)
    offsets = jax.random.randint(ks[1], (BATCH,), 0, 1024, dtype=jnp.int32)
    positions = offsets[:, None] + jnp.arange(SEQ, dtype=jnp.int32)[None, :]

    def gain(k, n):
        return 1.0 + 0.02 * jax.random.normal(k, (DEPTH, n), f32)

    return {
        "x": x,
        "positions": positions,
        "attn_norm": gain(ks[2], D_MODEL),
        "w_in": jax.random.normal(ks[3], (DEPTH, D_MODEL, IN_PROJ_WIDTH), f32) * D_MODEL ** -0.5,
        "sb_out_norm": gain(ks[4], SB_WIDTH),
        "swa_sinks": 0.5 * jax.random.normal(ks[5], (DEPTH, SWA_HEADS), f32),
        "swa_out_norm": gain(ks[6], SWA_Q_WIDTH),
        "w_out": jax.random.normal(ks[7], (DEPTH, MIX_WIDTH, D_MODEL), f32) * MIX_WIDTH ** -0.5,
        "ffn_norm": gain(ks[8], D_MODEL),
        "peer_w_query": jax.random.normal(ks[9], (DEPTH, D_MODEL, PEER_HEADS * PEER_QUERY_DIM), f32) * D_MODEL ** -0.5,
        "peer_sub_keys_1": jax.random.normal(ks[10], (DEPTH, PEER_HEADS, N_KEYS, PEER_HALF), f32) * PEER_HALF ** -0.5,
        "peer_sub_keys_2": jax.random.normal(ks[11], (DEPTH, PEER_HEADS, N_KEYS, PEER_HALF), f32) * PEER_HALF ** -0.5,
        "peer_expert_down": jax.random.normal(ks[12], (DEPTH, N_EXPERTS, D_MODEL), f32) * D_MODEL ** -0.5,
        "peer_expert_up": 0.5 * jax.random.normal(ks[13], (DEPTH, N_EXPERTS, D_MODEL), f32),
        "final_norm": 1.0 + 0.02 * jax.random.normal(ks[14], (D_MODEL,), f32),
    }


def reference(x, positions, attn_norm, w_in, sb_out_norm, swa_sinks, swa_out_norm, w_out,
              ffn_norm, peer_w_query, peer_sub_keys_1, peer_sub_keys_2, peer_expert_down,
              peer_expert_up, final_norm):
    B, S, _ = x.shape
    splits = np.cumsum([SB_WIDTH, SB_WIDTH, SB_WIDTH, SWA_Q_WIDTH, SWA_KV_WIDTH]).tolist()
    for layer in range(DEPTH):
        h = rms_norm(x, attn_norm[layer])
        proj = h @ w_in[layer]
        sb_q, sb_k, sb_v, sw_q, sw_k, sw_v = jnp.split(proj, splits, axis=-1)
        sb_q = sb_q.reshape(B, S, SB_HEADS, HEAD_DIM)
        sb_k = sb_k.reshape(B, S, SB_HEADS, HEAD_DIM)
        sb_v = sb_v.reshape(B, S, SB_HEADS, HEAD_DIM)
        sw_q = rope(sw_q.reshape(B, S, SWA_HEADS, HEAD_DIM), positions)
        sw_k = rope(sw_k.reshape(B, S, SWA_KV_HEADS, HEAD_DIM), positions)
        sw_v = sw_v.reshape(B, S, SWA_KV_HEADS, HEAD_DIM)
        sb_o = stick_breaking_attention(sb_q, sb_k, sb_v).reshape(B, S, SB_WIDTH)
        sw_o = sliding_window_sink_attention(sw_q, sw_k, sw_v, swa_sinks[layer]).reshape(B, S, SWA_Q_WIDTH)
        mix = jnp.concatenate([rms_norm(sb_o, sb_out_norm[layer]),
                               rms_norm(sw_o, swa_out_norm[layer])], axis=-1)
        x = x + mix @ w_out[layer]
        x = x + peer_ffn(rms_norm(x, ffn_norm[layer]), peer_w_query[layer],
                         peer_sub_keys_1[layer], peer_sub_keys_2[layer],
                         peer_expert_down[layer], peer_expert_up[layer])
    return rms_norm(x, final_norm)
```

```python
import numpy as np
import concourse.bass as bass
import concourse.mybir as mybir

F32 = mybir.dt.float32
BF16 = mybir.dt.bfloat16
I32 = mybir.dt.int32
U32 = mybir.dt.uint32
AF = mybir.ActivationFunctionType
ALU = mybir.AluOpType
AX = mybir.AxisListType


class Sched:
    ENG = ("pe", "act", "dve", "pool", "sp")

    def __init__(self, nc):
        self.nc = nc
        self.e = {"pe": nc.tensor, "act": nc.scalar, "dve": nc.vector,
                  "pool": nc.gpsimd, "sp": nc.sync}
        self.sem = {k: nc.alloc_semaphore("sem_" + k) for k in self.ENG}
        self.cnt = {k: 0 for k in self.ENG}
        self.seen = {k: {} for k in self.ENG}
        self.semobj = {}
        for k in self.ENG:
            self.semobj["sem_" + k] = self.sem[k]
        self.dsem = {}
        self.bufs = {}
        self.nwait = 0
        self.ninst = 0

    def _need(self, eng, tok, waits):
        if tok is None:
            return
        sname, val, src = tok
        if src == eng and eng == "pe":
            return
        if self.seen[eng].get(sname, 0) >= val:
            return
        waits[sname] = max(waits.get(sname, 0), val)

    def _deps(self, eng, reads, writes):
        waits = {}
        for k in reads:
            b = self.bufs.get(k)
            if b is not None:
                self._need(eng, b["w"], waits)
        for k in writes:
            b = self.bufs.get(k)
            if b is not None:
                w = b["w"]
                if w is not None:
                    self._need(eng, w, waits)
                for r in b["r"]:
                    self._need(eng, r, waits)
        for sname, val in waits.items():
            self.e[eng].wait_ge(self.semobj[sname], val)
            self.seen[eng][sname] = val
            self.nwait += 1

    def _commit(self, tok, reads, writes):
        for k in reads:
            b = self.bufs.setdefault(k, {"w": None, "r": []})
            b["r"].append(tok)
            if len(b["r"]) > 12:
                last = {}
                for r in b["r"]:
                    if r[0] not in last or last[r[0]][1] < r[1]:
                        last[r[0]] = r
                b["r"] = list(last.values())
        for k in writes:
            self.bufs[k] = {"w": tok, "r": []}

    def op(self, eng, fn, reads=(), writes=()):
        self._deps(eng, reads, writes)
        inst = fn(self.e[eng])
        self.cnt[eng] += 1
        inst.then_inc(self.sem[eng], 1)
        tok = ("sem_" + eng, self.cnt[eng], eng)
        self._commit(tok, reads, writes)
        self.ninst += 1
        return inst

    def dma(self, eng, slot, out, in_, reads=(), writes=(), **kw):
        if slot not in self.dsem:
            s = self.nc.alloc_semaphore("dsem_" + slot)
            self.dsem[slot] = [s, 0]
            self.semobj["dsem_" + slot] = s
        self._deps(eng, reads, writes)
        d = self.dsem[slot]
        inst = self.e[eng].dma_start(out=out, in_=in_, **kw)
        d[1] += 16
        inst.then_inc(d[0], 16)
        tok = ("dsem_" + slot, d[1], None)
        self._commit(tok, reads, writes)
        self.ninst += 1
        return inst

    def barrier(self):
        for eng in self.ENG:
            for other in self.ENG:
                if other == eng or self.cnt[other] == 0:
                    continue
                if self.seen[eng].get("sem_" + other, 0) < self.cnt[other]:
                    self.e[eng].wait_ge(self.sem[other], self.cnt[other])
                    self.seen[eng]["sem_" + other] = self.cnt[other]
            for slot, (s, v) in self.dsem.items():
                if v and self.seen[eng].get("dsem_" + slot, 0) < v:
                    self.e[eng].wait_ge(s, v)
                    self.seen[eng]["dsem_" + slot] = v
        self.bufs = {}

    def finish(self, eng="sp"):
        for slot, (s, v) in self.dsem.items():
            if v:
                self.e[eng].wait_ge(s, v)
        for other in self.ENG:
            if other != eng and self.cnt[other]:
                self.e[eng].wait_ge(self.sem[other], self.cnt[other])


import math
from contextlib import ExitStack
import numpy as np
import concourse.bass as bass
import concourse.mybir as mybir

EPS = 1e-6
NEG = -30000.0
PI = math.pi
C1 = 6.28125
C2 = 2.0 * math.pi - 6.28125
NB = 64
NOWN = 32
DBG3 = False
SKEW1B = False
NUNITS = 64


def build(upto=3, nqt=8, nblk1a=NB):
    nc = bass.Bass("TRN2", target_bir_lowering=False)

    def din(name, shape, dt=F32):
        return nc.dram_tensor(name, shape, dt, kind="ExternalInput").ap()

    def dscr(name, shape, dt):
        return nc.dram_tensor(name, shape, dt, kind="Internal").ap()

    x_all = din("x_all", [8192, 1024])
    x_po = din("x_po", [NOWN, 256, 1024])
    pos_po = din("pos_po", [NOWN * 256], I32)
    w_in = din("w_in", [1024, 2304])
    attn_norm = din("attn_norm", [1024])
    sb_out_norm = din("sb_out_norm", [512])
    swa_sinks = din("swa_sinks", [8])
    swa_out_norm = din("swa_out_norm", [512])
    w_out = din("w_out", [1024, 1024])
    ffn_norm = din("ffn_norm", [1024])
    peer_wq = din("peer_wq", [1024, 2048])
    sk1T = din("sk1T", [8, 128, 128])
    sk2T = din("sk2T", [8, 128, 128])
    if upto >= 3:
        downT = din("downT", [1024, 16384])
        up = din("up", [16384, 1024])
    final_norm = din("final_norm", [1024])
    c_invf = din("c_invf", [128, 1])
    c_sgn = din("c_sgn", [128, 1])
    c_ident = din("c_ident", [128, 128])
    c_uneg = din("c_uneg", [128, 128])
    c_sbm = din("c_sbm", [4, 128, 128])
    c_swm = din("c_swm", [2, 128, 256])
    c_iota = din("c_iota", [128, 128])

    out = nc.dram_tensor("out", [NOWN * 128, 1024], F32, kind="ExternalOutput").ap()
    dbg3 = nc.dram_tensor("dbg3", [128, 640], F32, kind="ExternalOutput").ap() if DBG3 else None

    qsb_d = dscr("qsb_d", [NOWN, 128, 4, 128], BF16)
    qsw_d = dscr("qsw_d", [NOWN, 128, 4, 128], BF16)
    ksw_d = dscr("ksw_d", [NOWN, 128, 256], BF16)
    vsw_d = dscr("vsw_d", [NOWN, 128, 2, 128], BF16)
    mix_d = dscr("mix_d", [NOWN, 128, 1024], BF16)
    dbg = None
    if upto < 3:
        dbg = nc.dram_tensor("dbg", [NOWN, 128, 1024], F32, kind="ExternalOutput").ap()

    S = Sched(nc)
    w_in_v = w_in.rearrange("(c p) n -> p c n", p=128)

    with ExitStack() as top:
        def sb(name, shape, dt, st=top):
            return st.enter_context(nc.sbuf_tensor(name, shape, dt))

        PF = [top.enter_context(nc.psum_tensor("pf%d" % i, [128, 512], F32)) for i in range(7)]
        PB = top.enter_context(nc.psum_tensor("pb", [128, 8, 128], BF16))

        identb = sb("identb", [128, 128], BF16)
        unegb = sb("unegb", [128, 128], BF16)
        invf = sb("invf", [128, 1], F32)
        sgn = sb("sgn", [128, 1], F32)
        S.dma("pool", "c0", identb[:], c_ident, writes=["identb"])
        S.dma("pool", "c1", unegb[:], c_uneg, writes=["unegb"])
        S.dma("sp", "c2", invf[:], c_invf, writes=["invf"])
        S.dma("sp", "c3", sgn[:], c_sgn, writes=["sgn"])

        def norm_T(st, src, k, xt, junk, hb, hT, ss, gb, evac_eng):
            S.dma("sp", "xt%d" % k, xt[k][:], src, writes=[("xt", k)])
            S.op("act", lambda E: E.activation(out=junk[:], in_=xt[k][:], func=AF.Square,
                                               accum_out=ss[:, k:k + 1]),
                 reads=[("xt", k)], writes=["junk", ("ss", k)])
            S.op("dve", lambda E: E.tensor_scalar(out=ss[:, 2 + k:3 + k], in0=ss[:, k:k + 1], scalar1=1.0 / 1024,
                                                  scalar2=EPS, op0=ALU.mult, op1=ALU.add),
                 reads=[("ss", k)], writes=[("ms", k)])
            S.op("act", lambda E: E.activation(out=ss[:, 4 + k:5 + k], in_=ss[:, 2 + k:3 + k], func=AF.Sqrt),
                 reads=[("ms", k)], writes=[("sd", k)])
            S.op("dve", lambda E: E.reciprocal(out=ss[:, 6 + k:7 + k], in_=ss[:, 4 + k:5 + k]),
                 reads=[("sd", k)], writes=[("rstd", k)])
            S.op("dve", lambda E: E.scalar_tensor_tensor(out=hb[k][:], in0=xt[k][:], scalar=ss[:, 6 + k:7 + k],
                                                         in1=gb[:], op0=ALU.mult, op1=ALU.mult),
                 reads=[("xt", k), ("rstd", k), "gb"], writes=[("hb", k)])
            for c in range(8):
                S.op("pe", lambda E: E.transpose(out=PB[:, c, :], in_=hb[k][:, c * 128:(c + 1) * 128],
                                                 identity=identb[:]),
                     reads=[("hb", k), "identb"], writes=["PB"])
            if evac_eng == "act":
                S.op("act", lambda E: E.copy(out=hT[k][:], in_=PB[:]), reads=["PB"], writes=[("hT", k)])
            else:
                S.op("dve", lambda E: E.tensor_copy(out=hT[k][:], in_=PB[:]), reads=["PB"], writes=[("hT", k)])

        if upto >= 3:
            down_b = dscr("down_b", [128, 128, 8, 128], BF16)
            up_b = dscr("up_b", [128, 128, 1024], BF16)
            downT_v = downT.rearrange("(c p) (g e) -> g p c e", p=128, e=512)
            up_v = up.rearrange("(g i e) f -> g e i f", i=4, e=128)
        with ExitStack() as p1:
            W2 = sb("W2", [128, 8, 1920], BF16, p1)
            gb = sb("gb", [128, 1024], F32, p1)
            xt = [sb("xt%d" % i, [128, 1024], F32, p1) for i in range(2)]
            junk = sb("junk", [128, 1024], BF16, p1)
            hb = [sb("hb%d" % i, [128, 1024], BF16, p1) for i in range(2)]
            hT = [sb("hT%d" % i, [128, 8, 128], BF16, p1) for i in range(2)]
            ss = sb("ss", [128, 8], F32, p1)
            posi = sb("posi", [128, 128], I32, p1)
            posf = sb("posf", [128, 128], F32, p1)
            ang = sb("ang", [128, 128], F32, p1)
            tq = sb("tq", [128, 128], F32, p1)
            ki = sb("ki", [128, 128], I32, p1)
            kf = sb("kf", [128, 128], F32, p1)
            rr = sb("rr", [128, 128], F32, p1)
            gg = sb("gg", [128, 128], F32, p1)
            cosb2 = [sb("cosb%d" % i, [128, 1, 128], F32, p1) for i in range(2)]
            sinb2 = [sb("sinb%d" % i, [128, 1, 128], F32, p1) for i in range(2)]
            t1 = sb("t1", [128, 4, 128], F32, p1)
            t2 = sb("t2", [128, 4, 128], F32, p1)
            qsb_s = [sb("qsb_s%d" % i, [128, 4, 128], BF16, p1) for i in range(2)]
            qsw_s = [sb("qsw_s%d" % i, [128, 4, 128], BF16, p1) for i in range(2)]
            ksw_s = [sb("ksw_s%d" % i, [128, 256], BF16, p1) for i in range(2)]
            vsw_s = [sb("vsw_s%d" % i, [128, 2, 128], BF16, p1) for i in range(2)]

            NST = 4
            stf = [sb("stf%d" % i, [128, 4096], F32, p1) for i in range(NST)]
            if upto >= 3:
                stb = [sb("stb%d" % i, [128, 4096], BF16, p1) for i in range(NST)]

            def w2_chunk(dst_c0, src_c0, n, k, eng):
                fv = stf[k][:, 0:8 * n].rearrange("p (c e) -> p c e", c=8)
                S.dma("sp", "w2s%d" % k, fv, w_in_v[:, :, src_c0:src_c0 + n], writes=[("stf", k)])
                S.op(eng, lambda E: E.tensor_copy(out=W2[:, :, dst_c0:dst_c0 + n], in_=fv),
                     reads=[("stf", k)], writes=["W2"])

            def w2_swap(dst_c0, src_c0, n, eng):
                sv = W2[:, :, src_c0:src_c0 + n].rearrange("p c (h two i) -> p c h two i", two=2, i=32)
                dv = W2[:, :, dst_c0:dst_c0 + n].rearrange("p c (h two i) -> p c h two i", two=2, i=32)
                for two in range(2):
                    S.op(eng, lambda E: E.tensor_copy(out=dv[:, :, :, two, :], in_=sv[:, :, :, 1 - two, :]),
                         reads=["W2"], writes=["W2"])

            fkv = stf[3][:, 0:2048].rearrange("p (c e) -> p c e", c=8)
            S.dma("sp", "w2s3", fkv, w_in_v[:, :, 2048:2304], writes=[("stf", 3)])
            S.op("dve", lambda E: E.tensor_copy(out=W2[:, :, 1536:1664], in_=fkv[:, :, 0:128]), reads=[("stf", 3)], writes=["W2"])
            S.op("pool", lambda E: E.tensor_copy(out=W2[:, :, 1792:1920], in_=fkv[:, :, 128:256]), reads=[("stf", 3)], writes=["W2"])
            w2_swap(1664, 1536, 128, "dve")
            w2_chunk(512, 1536, 512, 3, "dve")
            w2_chunk(0, 0, 512, 2, "pool")
            w2_swap(1024, 512, 512, "pool")
            pc_steps = [(g, which) for g in range(32) for which in range(2)]

            def pc_load(n):
                g, which = pc_steps[n]
                k = n % NST
                if which == 0:
                    src = downT_v[g]
                    fv = stf[k][:].rearrange("p (c e) -> p c e", c=8)
                else:
                    src = up_v[g]
                    fv = stf[k][:].rearrange("p (i f) -> p i f", i=4)
                S.dma("sp", "pc_in%d" % k, fv, src, writes=[("stf", k)])

            def pc_cast_store(n):
                g, which = pc_steps[n]
                k = n % NST
                if which == 0:
                    dst = down_b[4 * g:4 * g + 4].rearrange("i p c e -> p i (c e)")
                    co = stb[k][:].rearrange("p (i c e) -> p c i e", i=4, c=8)
                    ci = stf[k][:].rearrange("p (c i e) -> p c i e", c=8, i=4)
                else:
                    dst = up_b[4 * g:4 * g + 4].rearrange("i p f -> p i f")
                    co, ci = stb[k][:], stf[k][:]
                bv = stb[k][:].rearrange("p (i x) -> p i x", i=4)
                eng = "pool" if n % 2 == 0 else "dve"
                S.op(eng, lambda E: E.tensor_copy(out=co, in_=ci), reads=[("stf", k)], writes=[("stb", k)])
                S.dma("sp", "pc_out%d" % k, dst, bv, reads=[("stb", k)], writes=[("tab", which, g)])

            W2_PENDING = True
            S.dma("sp", "gb", gb[:], attn_norm.partition_broadcast(128), writes=["gb"])

            def rope_tables(j, sub):
                cosb, sinb = cosb2[sub], sinb2[sub]
                S.dma("sp", "posi", posi[:], pos_po[j * 256 + sub * 128: j * 256 + sub * 128 + 128].partition_broadcast(128),
                      writes=["posi"])
                S.op("dve", lambda E: E.tensor_copy(out=posf[:], in_=posi[:]), reads=["posi"], writes=["posf"])
                S.op("dve", lambda E: E.tensor_scalar(out=ang[:], in0=posf[:], scalar1=invf[:, 0:1], scalar2=None,
                                                      op0=ALU.mult), reads=["posf", "invf"], writes=["ang"])
                S.op("dve", lambda E: E.tensor_scalar(out=tq[:], in0=ang[:], scalar1=1.0 / (2 * PI), scalar2=None,
                                                      op0=ALU.mult), reads=["ang"], writes=["tq"])
                S.op("dve", lambda E: E.tensor_copy(out=ki[:], in_=tq[:]), reads=["tq"], writes=["ki"])
                S.op("dve", lambda E: E.tensor_copy(out=kf[:], in_=ki[:]), reads=["ki"], writes=["kf"])
                S.op("dve", lambda E: E.scalar_tensor_tensor(out=rr[:], in0=kf[:], scalar=-C1, in1=ang[:],
                                                             op0=ALU.mult, op1=ALU.add),
                     reads=["kf", "ang"], writes=["rr"])
                S.op("dve", lambda E: E.scalar_tensor_tensor(out=rr[:], in0=kf[:], scalar=-C2, in1=rr[:],
                                                             op0=ALU.mult, op1=ALU.add),
                     reads=["kf", "rr"], writes=["rr"])
                S.op("dve", lambda E: E.tensor_scalar(out=gg[:], in0=rr[:], scalar1=PI, scalar2=2 * PI,
                                                      op0=ALU.is_gt, op1=ALU.mult), reads=["rr"], writes=["gg"])
                S.op("dve", lambda E: E.tensor_tensor(out=rr[:], in0=rr[:], in1=gg[:], op=ALU.subtract),
                     reads=["rr", "gg"], writes=["rr"])
                S.op("dve", lambda E: E.tensor_scalar(out=gg[:], in0=rr[:], scalar1=-PI, scalar2=2 * PI,
                                                      op0=ALU.is_lt, op1=ALU.mult), reads=["rr"], writes=["gg"])
                S.op("dve", lambda E: E.tensor_tensor(out=rr[:], in0=rr[:], in1=gg[:], op=ALU.add),
                     reads=["rr", "gg"], writes=["rr"])
                S.op("dve", lambda E: E.tensor_scalar(out=rr[:], in0=rr[:], scalar1=3.14159, scalar2=-3.14159,
                                                      op0=ALU.min, op1=ALU.max), reads=["rr"], writes=["rr"])
                S.op("act", lambda E: E.activation(out=sinb[:, 0, :], in_=rr[:], func=AF.Sin, scale=sgn[:, 0:1]),
                     reads=["rr", "sgn"], writes=[("sinb", sub)])
                S.op("dve", lambda E: E.scalar_tensor_tensor(out=gg[:], in0=rr[:], scalar=-1.0, in1=rr[:], op0=ALU.mult, op1=ALU.max),
                     reads=["rr"], writes=["gg"])
                S.op("act", lambda E: E.activation(out=cosb[:, 0, :], in_=gg[:], func=AF.Sin, scale=-1.0,
                                                   bias=PI / 2), reads=["gg"], writes=[("cosb", sub)])

            def proj_T(ps_ap, wc0, k, nm):
                for c in range(8):
                    S.op("pe", lambda E: E.matmul(ps_ap, lhsT=W2[:, c, wc0:wc0 + 128], rhs=hT[k][:, c, :],
                                                  start=(c == 0), stop=(c == 7)),
                         reads=["W2", ("hT", k)], writes=[nm])

            def stage_a(j, sub):
                norm_T(p1, x_po[j, sub * 128:(sub + 1) * 128, :], sub, xt, junk, hb, hT, ss, gb,
                       "act" if sub == 0 else "dve")
                rope_tables(j, sub)

            def stage_b(j, sub):
                    jb = j % 2
                    k = sub
                    cosb, sinb = cosb2[sub], sinb2[sub]
                    kvb = PF[4] if sub == 0 else PF[3]
                    kvn = "PF4" if sub == 0 else "PF3"
                    proj_T(kvb[:, 0:128], 1536, k, kvn)
                    proj_T(kvb[:, 128:256], 1664, k, kvn)
                    for c in range(8):
                        S.op("pe", lambda E: E.matmul(kvb[:, 256:384], lhsT=hT[k][:, c, :], rhs=W2[:, c, 1792:1920],
                                                      start=(c == 0), stop=(c == 7)),
                             reads=["W2", ("hT", k)], writes=[kvn])
                    S.op("dve", lambda E: E.tensor_tensor(out=t1[:, 0, :], in0=kvb[:, 0:128], in1=cosb[:, 0, :], op=ALU.mult),
                         reads=[kvn, ("cosb", sub)], writes=["t1"])
                    S.op("dve", lambda E: E.tensor_tensor(out=t2[:, 0, :], in0=kvb[:, 128:256], in1=sinb[:, 0, :], op=ALU.mult),
                         reads=[kvn, ("sinb", sub)], writes=["t2"])
                    S.op("pool", lambda E: E.tensor_tensor(out=ksw_s[jb][:, sub * 128:(sub + 1) * 128], in0=t1[:, 0, :],
                                                           in1=t2[:, 0, :], op=ALU.add),
                         reads=["t1", "t2"], writes=[("ksw_s", jb)])
                    S.op("act", lambda E: E.copy(out=vsw_s[jb][:, sub, :], in_=kvb[:, 256:384]),
                         reads=[kvn], writes=[("vsw_s", jb)])
                    if sub == 1:
                        for hp in range(4):
                            proj_T(PF[0][:, hp * 128:(hp + 1) * 128], hp * 128, k, "PF0")
                        for p in range(4):
                            proj_T(PF[1][:, p * 128:(p + 1) * 128], 512 + p * 128, k, "PF1")
                        for p in range(4):
                            proj_T(PF[2][:, p * 128:(p + 1) * 128], 1024 + p * 128, k, "PF2")
                        S.op("act", lambda E: E.activation(out=qsb_s[jb][:].rearrange("p a b -> p (a b)"), in_=PF[0][:],
                                                           func=AF.Copy, scale=0.125),
                             reads=["PF0"], writes=[("qsb_s", jb)])
                        S.op("dve", lambda E: E.scalar_tensor_tensor(
                            out=t1[:], in0=PF[1][:].rearrange("p (a b) -> p a b", a=4), scalar=0.125,
                            in1=cosb[:].to_broadcast([128, 4, 128]), op0=ALU.mult, op1=ALU.mult),
                            reads=["PF1", ("cosb", sub)], writes=["t1"])
                        S.op("dve", lambda E: E.scalar_tensor_tensor(
                            out=t2[:], in0=PF[2][:].rearrange("p (a b) -> p a b", a=4), scalar=0.125,
                            in1=sinb[:].to_broadcast([128, 4, 128]), op0=ALU.mult, op1=ALU.mult),
                            reads=["PF2", ("sinb", sub)], writes=["t2"])
                        S.op("pool", lambda E: E.tensor_tensor(out=qsw_s[jb][:], in0=t1[:], in1=t2[:], op=ALU.add),
                             reads=["t1", "t2"], writes=[("qsw_s", jb)])
            def stage_out(j):
                jb = j % 2
                S.dma("sp", "o_qsb%d" % jb, qsb_d[j], qsb_s[jb][:], reads=[("qsb_s", jb)], writes=[("qsb_d", j)])
                S.dma("sp", "o_qsw%d" % jb, qsw_d[j], qsw_s[jb][:], reads=[("qsw_s", jb)], writes=[("qsw_d", j)])
                S.dma("sp", "o_ksw%d" % jb, ksw_d[j], ksw_s[jb][:], reads=[("ksw_s", jb)], writes=[("ksw_d", j)])
                S.dma("sp", "o_vsw%d" % jb, vsw_d[j], vsw_s[jb][:], reads=[("vsw_s", jb)], writes=[("vsw_d", j)])

            units = [(j, sub) for j in range(NOWN) for sub in range(2)][:NUNITS]
            if SKEW1B:
                stage_a(*units[0])
            if upto >= 3:
                for n in range(NST - 1):
                    pc_load(n)
            for n, (j, sub) in enumerate(units):
                if SKEW1B:
                    if n + 1 < len(units):
                        stage_a(*units[n + 1])
                else:
                    stage_a(j, sub)
                if upto >= 3:
                    if n + NST - 1 < len(pc_steps):
                        pc_load(n + NST - 1)
                    pc_cast_store(n)
                stage_b(j, sub)
                if sub == 1:
                    stage_out(j)
            S.barrier()

        if upto == 0:
            S.finish()
            return nc

        with ExitStack() as p2:
            KT = sb("KT", [128, 4, 8192], BF16, p2)
            V = sb("V", [128, NB, 512], BF16, p2)
            with ExitStack() as p1a:
                W1 = sb("W1", [128, 8, 1024], BF16, p1a)
                gb = sb("gb1", [128, 1024], F32, p1a)
                xt = [sb("xta%d" % i, [128, 1024], F32, p1a) for i in range(2)]
                junk = sb("junka", [128, 1024], BF16, p1a)
                hb = [sb("hba%d" % i, [128, 1024], BF16, p1a) for i in range(2)]
                hT = [sb("hTa%d" % i, [128, 8, 128], BF16, p1a) for i in range(2)]
                ss = sb("ssa", [128, 8], F32, p1a)
                stg1 = [sb("stg1a%d" % i, [128, 4096], F32, p1a) for i in range(2)]
                for i_, (d0_, s0_) in enumerate(((0, 512), (512, 1024))):
                    fv_ = stg1[i_][:].rearrange("p (c e) -> p c e", c=8)
                    S.dma("sp", "w1s%d" % i_, fv_, w_in_v[:, :, s0_:s0_ + 512], writes=[("stg1", i_)])
                    S.op("dve" if i_ == 0 else "pool", lambda E: E.tensor_copy(out=W1[:, :, d0_:d0_ + 512], in_=fv_),
                         reads=[("stg1", i_)], writes=["W1"])
                S.dma("sp", "gb1", gb[:], attn_norm.partition_broadcast(128), writes=["gb"])
                def na(blk):
                    norm_T(p1a, x_all[blk * 128:(blk + 1) * 128, :], blk % 2, xt, junk, hb, hT, ss, gb,
                           "act" if blk % 2 == 0 else "dve")
                na(0)
                for blk in range(nblk1a):
                    k = blk % 2
                    if blk + 1 < nblk1a:
                        na(blk + 1)
                    pk, pkn = PF[2 * k], "PF%d" % (2 * k)
                    pv, pvn = PF[2 * k + 1], "PF%d" % (2 * k + 1)
                    for hp in range(4):
                        for c in range(8):
                            S.op("pe", lambda E: E.matmul(pk[:, hp * 128:(hp + 1) * 128], lhsT=W1[:, c, hp * 128:(hp + 1) * 128],
                                                          rhs=hT[k][:, c, :], start=(c == 0), stop=(c == 7)),
                                 reads=["W1", ("hT", k)], writes=[pkn])
                    for c in range(8):
                        S.op("pe", lambda E: E.matmul(pv[:], lhsT=hT[k][:, c, :], rhs=W1[:, c, 512:1024],
                                                      start=(c == 0), stop=(c == 7)),
                             reads=["W1", ("hT", k)], writes=[pvn])
                    S.op("dve", lambda E: E.tensor_copy(out=KT[:, :, blk * 128:(blk + 1) * 128],
                                                        in_=pk[:].rearrange("p (a b) -> p a b", a=4)),
                         reads=[pkn], writes=[("KT", blk)])
                    S.op("act", lambda E: E.copy(out=V[:, blk, :], in_=pv[:]), reads=[pvn], writes=[("V", blk)])
                S.barrier()

            if upto == 1:
                S.finish()
                return nc

            with ExitStack() as p2b:
                Qz = [sb("Qz%d" % i, [128, 8, 512], BF16, p2b) for i in range(2)]
                ebuf = [sb("ebuf%d" % i, [128, 512], F32, p2b) for i in range(2)]
                spb = [sb("spb%d" % i, [128, 512], BF16, p2b) for i in range(2)]
                wb = [sb("wb%d" % i, [128, 512], BF16, p2b) for i in range(2)]
                Ob = [sb("Ob0", [128, 4, 512], F32, p2b)] * 2
                Call = sb("Call", [128, 8], F32, p2b)
                Cst = [Call[:, 0:4], Call[:, 4:8]]
                call = sb("call", [128, 8], F32, p2b)
                cst = [call[:, 0:4], call[:, 4:8]]
                sbm = sb("sbm", [128, 4, 128], BF16, p2b)
                swm = sb("swm", [128, 2, 256], F32, p2b)
                negones = sb("negones", [128, 2], BF16, p2b)
                gsb = sb("gsb", [128, 512], F32, p2b)
                gsw = sb("gsw", [128, 512], F32, p2b)
                sinkb = sb("sinkb", [128, 8], F32, p2b)
                Qswz = [sb("Qswz%d" % i, [128, 8, 128], BF16, p2b) for i in range(2)]
                kswt = [sb("kswt%d" % i, [128, 256], BF16, p2b) for i in range(2)]
                vswt = [sb("vswt%d" % i, [128, 2, 128], BF16, p2b) for i in range(2)]
                zm = sb("zm", [128, 4, 256], F32, p2b)
                pexp = sb("pexp", [128, 4, 256], BF16, p2b)
                pT = sb("pT", [128, 8, 128], BF16, p2b)
                sst = sb("sst", [128, 40], F32, p2b)
                Osw = sb("Osw", [128, 4, 512], F32, p2b)
                mixb = [sb("mixb0", [128, 4, 1024], BF16, p2b)] * 2
                junk2 = sb("junk2", [128, 512], BF16, p2b)
                rst = sb("rst", [128, 32], F32, p2b)
                tmpo = [sb("tmpo%d" % i, [128, 4, 64], F32, p2b) for i in range(2)]

                S.dma("pool", "sbm", sbm[:], c_sbm.rearrange("m p f -> p m f"), writes=["sbm"])
                S.dma("sp", "swm", swm[:], c_swm.rearrange("m p f -> p m f"), writes=["swm"])
                S.dma("sp", "gsb", gsb[:], sb_out_norm.partition_broadcast(128), writes=["gsb"])
                S.dma("sp", "gsw", gsw[:], swa_out_norm.partition_broadcast(128), writes=["gsw"])
                S.dma("sp", "sinkb", sinkb[:], swa_sinks.partition_broadcast(128), writes=["sinkb"])
                S.op("pool", lambda E: E.memset(negones[:], -1.0), writes=["negones"])
                for i in range(2):
                    S.op("pool", lambda E: E.memset(Qz[i][:], 0.0), writes=[("Qz", i)])
                    S.op("pool", lambda E: E.memset(Qswz[i][:], 0.0), writes=[("Qswz", i)])

                Zb = [PF[0], PF[1], PF[2], PF[3]]
                Zn = ["PF0", "PF1", "PF2", "PF3"]
                PVb = [PF[4], PF[5], PF[6]]
                PVn = ["PF4", "PF5", "PF6"]

                def load_q(t):
                    tb = t % 2
                    for jj in range(4):
                        j = 4 * t + jj
                        for r in range(2):
                            dst = Qz[tb][r * 64:(r + 1) * 64, :, jj * 128:(jj + 1) * 128] \
                                .rearrange("p (hp two) f -> p hp two f", two=2)[:, :, r, :]
                            S.dma("sp", "ldq%d" % tb, dst, qsb_d[j, r * 64:(r + 1) * 64, :, :],
                                  reads=[("qsb_d", j)], writes=[("Qz", tb)])

                def sb_tiles(t):
                    L = []
                    for hp in range(4):
                        kbs = list(range(8 * t + 7, -1, -1))
                        for n, kb in enumerate(kbs):
                            for r in range(2):
                                h = 2 * hp + r
                                if kb >= 8 * t:
                                    d = kb - 8 * t
                                    j0 = d // 2
                                    m = (0 if d % 2 == 1 else 1) + (0 if j0 % 2 == 0 else 2)
                                else:
                                    j0, m = 0, None
                                L.append(dict(h=h, kb=kb, j0=j0, m=m, first=(n == 0), last=(n == len(kbs) - 1),
                                              s=r))
                    return L

                def S1(i, T, tb):
                    z, zn = Zb[i % 4], Zn[i % 4]
                    c0 = T["j0"] * 128
                    hp = T["h"] // 2
                    S.op("pe", lambda E: E.matmul(z[:, c0:512], lhsT=KT[:, hp, T["kb"] * 128:(T["kb"] + 1) * 128],
                                                  rhs=Qz[tb][:, T["h"], c0:512], start=True, stop=(T["m"] is None)),
                         reads=[("KT", T["kb"]), ("Qz", tb)], writes=[zn])
                    if T["m"] is not None:
                        S.op("pe", lambda E: E.matmul(z[:, c0:c0 + 128], lhsT=identb[:], rhs=sbm[:, T["m"], :],
                                                      start=False, stop=True),
                             reads=["identb", "sbm"], writes=[zn])

                def A1(i, T):
                    z, zn = Zb[i % 4], Zn[i % 4]
                    c0 = T["j0"] * 128
                    S.op("act", lambda E: E.activation(out=ebuf[i % 2][:, c0:512], in_=z[:, c0:512], func=AF.Exp),
                         reads=[zn], writes=[("ebuf", i % 2)])

                def A2(i, T):
                    c0 = T["j0"] * 128
                    S.op("act", lambda E: E.activation(out=spb[i % 2][:, c0:512], in_=ebuf[i % 2][:, c0:512],
                                                       func=AF.Ln, bias=1.0, scale=1.0),
                         reads=[("ebuf", i % 2)], writes=[("spb", i % 2)])

                def S3(i, T):
                    z, zn = Zb[i % 4], Zn[i % 4]
                    c0 = T["j0"] * 128
                    S.op("pe", lambda E: E.matmul(z[:, c0:512], lhsT=unegb[:], rhs=spb[i % 2][:, c0:512],
                                                  start=False, stop=True, skip_group_check=True),
                         reads=["unegb", ("spb", i % 2)], writes=[zn])
                    for jj in range(T["j0"], 4):
                        S.op("pe", lambda E: E.matmul(PVb[i % 3][:, 256 + jj:257 + jj],
                                                      lhsT=spb[i % 2][:, jj * 128:(jj + 1) * 128],
                                                      rhs=negones[:, 0:1], start=True, stop=True),
                             reads=[("spb", i % 2), "negones"], writes=[PVn[i % 3]])

                def A3(i, T):
                    z, zn = Zb[i % 4], Zn[i % 4]
                    c0 = T["j0"] * 128
                    S.op("act", lambda E: E.activation(out=wb[i % 2][:, c0:512], in_=z[:, c0:512], func=AF.Exp),
                         reads=[zn], writes=[("wb", i % 2)])

                def S5(i, T):
                    h = T["h"]
                    for jj in range(T["j0"], 4):
                        S.op("pe", lambda E: E.matmul(PVb[i % 3][:, jj * 64:(jj + 1) * 64],
                                                      lhsT=wb[i % 2][:, jj * 128:(jj + 1) * 128],
                                                      rhs=V[:, T["kb"], h * 64:(h + 1) * 64], start=True, stop=True),
                             reads=[("wb", i % 2), ("V", T["kb"])], writes=[PVn[i % 3]])

                def S6(i, T, tb):
                    s, h = T["s"], T["h"]
                    O = Ob[tb]
                    if T["first"]:
                        S.op("dve", lambda E: E.memset(Cst[s], 0.0), writes=[("Cst", s)])
                        S.op("dve", lambda E: E.memset(cst[s], 1.0), writes=[("cst", s)])
                        S.op("pool", lambda E: E.memset(O[:, :, h * 64:(h + 1) * 64], 0.0), writes=[("Ob", 0, h)])
                    j0 = T["j0"]
                    nj = 4 - j0
                    tm = tmpo[i % 2]
                    S.op("dve", lambda E: E.tensor_tensor(
                        out=tm[:, j0:4, :], in0=PVb[i % 3][:, j0 * 64:256].rearrange("p (a b) -> p a b", a=nj),
                        in1=cst[s][:, j0:4].unsqueeze(2).to_broadcast([128, nj, 64]), op=ALU.mult),
                        reads=[PVn[i % 3], ("cst", s)], writes=[("tmpo", i % 2)])
                    S.op("pool", lambda E: E.tensor_tensor(
                        out=O[:, j0:4, h * 64:(h + 1) * 64], in0=O[:, j0:4, h * 64:(h + 1) * 64], in1=tm[:, j0:4, :],
                        op=ALU.add),
                        reads=[("tmpo", i % 2), ("Ob", 0, h)], writes=[("Ob", 0, h)])
                    if not T["last"]:
                        j0 = T["j0"]
                        S.op("dve", lambda E: E.tensor_tensor(out=Cst[s][:, j0:4], in0=PVb[i % 3][:, 256 + j0:260],
                                                              in1=Cst[s][:, j0:4], op=ALU.add),
                             reads=[PVn[i % 3], ("Cst", s)], writes=[("Cst", s)])
                        pend_exp.append(s)

                pend_exp = []

                pend_age = [0]

                def flush_exp():
                    if not pend_exp:
                        pend_age[0] = 0
                        return
                    if len(pend_exp) >= 2:
                        assert sorted(pend_exp[:2]) == [0, 1], pend_exp
                        del pend_exp[:2]
                        S.op("act", lambda E: E.activation(out=call[:], in_=Call[:], func=AF.Exp),
                             reads=[("Cst", 0), ("Cst", 1)], writes=[("cst", 0), ("cst", 1)])
                        pend_age[0] = 0
                        return
                    pend_age[0] += 1
                    if pend_age[0] >= 2:
                        s = pend_exp.pop(0)
                        S.op("act", lambda E: E.activation(out=cst[s], in_=Cst[s], func=AF.Exp),
                             reads=[("Cst", s)], writes=[("cst", s)])
                        pend_age[0] = 0

                def sb_attention(t):
                    tb = t % 2
                    L = sb_tiles(t)
                    n = len(L)
                    S1(0, L[0], tb)
                    S1(1, L[1], tb)
                    A1(0, L[0])
                    for it in range(n + 2):
                        if it + 2 < n:
                            S1(it + 2, L[it + 2], tb)
                        if it < n:
                            A2(it, L[it])
                            S3(it, L[it])
                        if it + 1 < n:
                            A1(it + 1, L[it + 1])
                        flush_exp()
                        if 1 <= it <= n:
                            A3(it - 1, L[it - 1])
                        if 2 <= it:
                            S5(it - 2, L[it - 2])
                            S6(it - 2, L[it - 2], tb)

                def swa_block(t, jj):
                    j = 4 * t + jj
                    jb = j % 2
                    S.dma("sp", "ldk%d" % jb, kswt[jb][:], ksw_d[j], reads=[("ksw_d", j)], writes=[("kswt", jb)])
                    S.dma("sp", "ldv%d" % jb, vswt[jb][:], vsw_d[j], reads=[("vsw_d", j)], writes=[("vswt", jb)])
                    srcv = qsw_d[j].rearrange("(r d) p t -> d p r t", r=2)
                    for kv in range(2):
                        dst = Qswz[jb][kv * 64:(kv + 1) * 64, kv * 4:(kv + 1) * 4, :] \
                            .rearrange("d (p r) t -> d p r t", r=2)
                        for r in range(2):
                            S.dma("sp", "ldqs%d" % jb, dst[:, :, r, :], srcv[:, 2 * kv:2 * kv + 2, r, :],
                                  reads=[("qsw_d", j)], writes=[("Qswz", jb)])
                    mk = swm[:, 0:1, :] if j == 0 else swm[:, 1:2, :]
                    for kv in range(2):
                        for g4 in range(4):
                            g = kv * 4 + g4
                            zb, zbn = (PF[0], "PF0") if g4 < 2 else (PF[1], "PF1")
                            S.op("pe", lambda E: E.matmul(zb[:, (g4 % 2) * 256:(g4 % 2 + 1) * 256], lhsT=Qswz[jb][:, g, :],
                                                          rhs=kswt[jb][:], start=True, stop=True),
                                 reads=[("Qswz", jb), ("kswt", jb)], writes=[zbn])
                        for hb2 in range(2):
                            zb, zbn = (PF[0], "PF0") if hb2 == 0 else (PF[1], "PF1")
                            S.op("dve", lambda E: E.tensor_tensor(out=zm[:, 2 * hb2:2 * hb2 + 2, :],
                                                                  in0=zb[:].rearrange("p (a b) -> p a b", a=2),
                                                                  in1=mk.to_broadcast([128, 2, 256]), op=ALU.add),
                                 reads=[zbn, "swm"], writes=["zm"])
                        o = kv * 4
                        S.op("dve", lambda E: E.tensor_reduce(out=sst[:, o:o + 4], in_=zm[:], axis=AX.X, op=ALU.max),
                             reads=["zm"], writes=["sst_m"])
                        S.op("dve", lambda E: E.tensor_tensor(out=sst[:, o:o + 4], in0=sst[:, o:o + 4],
                                                              in1=sinkb[:, o:o + 4], op=ALU.max),
                             reads=["sst_m", "sinkb"], writes=["sst_m"])
                        S.op("dve", lambda E: E.tensor_scalar(out=sst[:, 8 + o:12 + o], in0=sst[:, o:o + 4], scalar1=-1.0,
                                                              scalar2=None, op0=ALU.mult),
                             reads=["sst_m"], writes=["sst_nm"])
                        for g4 in range(4):
                            S.op("act", lambda E: E.activation(out=pexp[:, g4, :], in_=zm[:, g4, :], func=AF.Exp,
                                                               bias=sst[:, 8 + o + g4:9 + o + g4], scale=1.0,
                                                               accum_out=sst[:, 16 + o + g4:17 + o + g4]),
                                 reads=["zm", "sst_nm"], writes=["pexp", "sst_rs"])
                        S.op("dve", lambda E: E.tensor_tensor(out=sst[:, 24 + o:28 + o], in0=sinkb[:, o:o + 4],
                                                              in1=sst[:, o:o + 4], op=ALU.subtract),
                             reads=["sst_m", "sinkb"], writes=["sst_d"])
                        S.op("act", lambda E: E.activation(out=sst[:, 24 + o:28 + o], in_=sst[:, 24 + o:28 + o], func=AF.Exp),
                             reads=["sst_d"], writes=["sst_es"])
                        S.op("dve", lambda E: E.tensor_tensor(out=sst[:, 32 + o:36 + o], in0=sst[:, 24 + o:28 + o],
                                                              in1=sst[:, 16 + o:20 + o], op=ALU.add),
                             reads=["sst_es", "sst_rs"], writes=["sst_den"])
                        S.op("dve", lambda E: E.reciprocal(out=sst[:, 32 + o:36 + o], in_=sst[:, 32 + o:36 + o]),
                             reads=["sst_den"], writes=["sst_rden"])
                        for g4 in range(4):
                            for hf in range(2):
                                S.op("pe", lambda E: E.transpose(out=PB[:, g4 * 2 + hf, :],
                                                                 in_=pexp[:, g4, hf * 128:(hf + 1) * 128],
                                                                 identity=identb[:]),
                                     reads=["pexp", "identb"], writes=["PB"])
                        S.op("act", lambda E: E.copy(out=pT[:], in_=PB[:]), reads=["PB"], writes=["pT"])
                        for g4 in range(4):
                            g = kv * 4 + g4
                            for hf in range(2):
                                S.op("pe", lambda E: E.matmul(PF[2][:, g * 64:(g + 1) * 64], lhsT=pT[:, g4 * 2 + hf, :],
                                                              rhs=vswt[jb][:, hf, kv * 64:(kv + 1) * 64],
                                                              start=(hf == 0), stop=(hf == 1)),
                                     reads=["pT", ("vswt", jb)], writes=["PF2"])
                        S.op("dve", lambda E: E.tensor_tensor(
                            out=Osw[:, jj, kv * 256:(kv + 1) * 256].rearrange("p (a b) -> p a b", a=4),
                            in0=PF[2][:, kv * 256:(kv + 1) * 256].rearrange("p (a b) -> p a b", a=4),
                            in1=sst[:, 32 + o:36 + o].unsqueeze(2).to_broadcast([128, 4, 64]), op=ALU.mult),
                            reads=["PF2", "sst_rden"], writes=[("Osw", jj)])

                def finish_tile(t):
                    tb = t % 2
                    O = Ob[tb]
                    for jj in range(4):
                        S.op("act", lambda E: E.activation(out=junk2[:], in_=O[:, jj, :], func=AF.Square,
                                                           accum_out=rst[:, jj:jj + 1]),
                             reads=[("Ob", 0, h) for h in range(8)], writes=["junk2", "rst_ss"])
                        S.op("act", lambda E: E.activation(out=junk2[:], in_=Osw[:, jj, :], func=AF.Square,
                                                           accum_out=rst[:, 4 + jj:5 + jj]),
                             reads=[("Osw", jj)], writes=["junk2", "rst_ss"])
                    S.op("dve", lambda E: E.tensor_scalar(out=rst[:, 8:16], in0=rst[:, 0:8], scalar1=1.0 / 512,
                                                          scalar2=EPS, op0=ALU.mult, op1=ALU.add),
                         reads=["rst_ss"], writes=["rst_ms"])
                    S.op("act", lambda E: E.activation(out=rst[:, 16:24], in_=rst[:, 8:16], func=AF.Sqrt),
                         reads=["rst_ms"], writes=["rst_sd"])
                    S.op("dve", lambda E: E.reciprocal(out=rst[:, 24:32], in_=rst[:, 16:24]),
                         reads=["rst_sd"], writes=["rst_r"])
                    for jj in range(4):
                        S.op("dve", lambda E: E.scalar_tensor_tensor(out=mixb[tb][:, jj, 0:512], in0=O[:, jj, :],
                                                                     scalar=rst[:, 24 + jj:25 + jj], in1=gsb[:],
                                                                     op0=ALU.mult, op1=ALU.mult),
                             reads=[("Ob", 0, h) for h in range(8)] + ["rst_r", "gsb"], writes=[("mixb", 0)])
                        S.op("dve", lambda E: E.scalar_tensor_tensor(out=mixb[tb][:, jj, 512:1024], in0=Osw[:, jj, :],
                                                                     scalar=rst[:, 28 + jj:29 + jj], in1=gsw[:],
                                                                     op0=ALU.mult, op1=ALU.mult),
                             reads=[("Osw", jj), "rst_r", "gsw"], writes=[("mixb", 0)])
                    S.dma("sp", "omix", mix_d[4 * t:4 * t + 4].rearrange("j p f -> p j f"), mixb[tb][:],
                          reads=[("mixb", 0)], writes=[("mix_d", t)])

                load_q(0)
                for t in range(nqt):
                    if t + 1 < nqt:
                        load_q(t + 1)
                    sb_attention(t)
                    for jj in range(4):
                        swa_block(t, jj)
                    finish_tile(t)
                S.barrier()

        if upto == 2:
            with ExitStack() as pd:
                mb = sb("dbg_mb", [128, 1024], BF16, pd)
                mf = sb("dbg_mf", [128, 1024], F32, pd)
                for j in range(4 * nqt):
                    S.dma("sp", "dbg_in", mb[:], mix_d[j], writes=["mb"])
                    S.op("dve", lambda E: E.tensor_copy(out=mf[:], in_=mb[:]), reads=["mb"], writes=["mf"])
                    S.dma("sp", "dbg_out", dbg[j], mf[:], reads=["mf"], writes=["dbgo"])
            S.finish()
            return nc


        ntile = 2 * nqt
        with ExitStack() as p3:
            Wout = sb("Wout", [128, 8, 1024], BF16, p3)
            Wq = sb("Wq", [128, 8, 2048], BF16, p3)
            skT = [sb("skT%d" % i, [128, 8, 128], BF16, p3) for i in range(2)]
            gffn = sb("gffn", [128, 1024], F32, p3)
            gfin = sb("gfin", [128, 1024], F32, p3)
            identf = sb("identf", [128, 128], F32, p3)
            iotaf = sb("iotaf", [128, 16], F32, p3)
            iotab = sb("iotab", [128, 128], BF16, p3)
            mixt = sb("mixt", [128, 2, 1024], BF16, p3)
            GT = sb("GT", [128, 128, 256], BF16, p3)
            xo = sb("xo", [128, 2, 1024], F32, p3)
            x1b = [sb("x1b%d" % i, [128, 2, 1024], F32, p3) for i in range(2)]
            xnTb = [sb("xnTb%d" % i, [128, 8, 256], BF16, p3) for i in range(2)]
            st3 = sb("st3", [128, 16], F32, p3)
            sc = sb("sc", [128, 16, 128], F32, p3)
            scw = sb("scw", [128, 16, 128], F32, p3)
            vv = sb("vv", [128, 16, 16], F32, p3)
            ix = sb("ix", [128, 16, 16], U32, p3)
            tops = sb("tops", [128, 8, 16], F32, p3)
            posu = sb("posu", [128, 8, 16], U32, p3)
            posf3 = sb("posf3", [128, 8, 16], F32, p3)
            thr16 = sb("thr16", [128, 16], F32, p3)
            paf = sb("paf", [128, 8, 16], F32, p3)
            pbf = sb("pbf", [128, 8, 16], F32, p3)
            i1f = sb("i1f", [128, 8, 16], F32, p3)
            i2f = sb("i2f", [128, 8, 16], F32, p3)
            sel = sb("sel", [128, 3, 128], F32, p3)
            gsm = sb("gsm", [128, 32], F32, p3)
            hkT = sb("hkT", [128, 3, 256], F32, p3)
            At = [sb("At%d" % i, [128, 128], BF16, p3) for i in range(3)]
            Bt = [sb("Bt%d" % i, [128, 128], BF16, p3) for i in range(3)]
            dsb = [sb("dsb%d" % i, [128, 8, 128], BF16, p3) for i in range(3)]
            usb = [sb("usb%d" % i, [128, 1024], BF16, p3) for i in range(4)]
            gl = [sb("gl%d" % i, [128, 256], F32, p3) for i in range(3)]
            wg = [sb("wg%d" % i, [128, 256], BF16, p3) for i in range(3)]
            xn = mixt
            qTv = scw[:, 0:8, :].bitcast(BF16).rearrange("p a (b c) -> p (a b) c", c=128)
            cand = sc[:].rearrange("p (h x) k -> p h (x k)", x=2)
            candw = scw[:].rearrange("p (h x) k -> p h (x k)", x=2)
            oh = scw[:].rearrange("p (h x) (y a) -> p h (x y) a", x=2, a=16)

            w_out_v = w_out.rearrange("(c p) n -> p c n", p=128)
            wq_v = peer_wq.rearrange("(c p) n -> p c n", p=128)
            gst = [GT[:, 32 * i:32 * i + 32, :].bitcast(F32).rearrange("p a b -> p (a b)").rearrange("p (c e) -> p c e", c=8)
                   for i in range(4)]
            wl = [(Wout, w_out_v, q4, "Wout") for q4 in range(2)] + [(Wq, wq_v, q4, "Wq") for q4 in range(4)]
            for n_, (wt_, wv_, q4, key_) in enumerate(wl):
                k_ = n_ % 4
                S.dma("sp", "w3s%d" % k_, gst[k_], wv_[:, :, q4 * 512:(q4 + 1) * 512], writes=[("gst", k_)])
                S.op(("dve", "pool", "act")[n_ % 3],
                     (lambda E: E.copy(out=wt_[:, :, q4 * 512:(q4 + 1) * 512], in_=gst[k_])) if n_ % 3 == 2 else
                     (lambda E: E.tensor_copy(out=wt_[:, :, q4 * 512:(q4 + 1) * 512], in_=gst[k_])),
                     reads=[("gst", k_)], writes=[key_])
            S.dma("pool", "sk1", skT[0][:], sk1T.rearrange("h d k -> d h k"), writes=["skT"])
            S.dma("pool", "sk2", skT[1][:], sk2T.rearrange("h d k -> d h k"), writes=["skT"])
            S.dma("sp", "gffn", gffn[:], ffn_norm.partition_broadcast(128), writes=["gffn"])
            S.dma("sp", "gfin", gfin[:], final_norm.partition_broadcast(128), writes=["gfin"])
            S.dma("sp", "identf", identf[:], c_ident, writes=["identf"])
            S.dma("sp", "iotaf", iotaf[:], c_iota[:, 0:16], writes=["iotaf"])
            S.dma("pool", "iotab", iotab[:], c_iota, writes=["iotab"])
            S.op("dve", lambda E: E.tensor_scalar(out=thr16[:], in0=iotaf[:, 0:16], scalar1=16.0, scalar2=16.0, op0=ALU.mult, op1=ALU.add), reads=["iotaf"], writes=["thr16"])

            def rms_rows(src, blk, g_t, dst, col, sk, gn, dk):
                S.op("act", lambda E: E.activation(out=xn[:, blk, :], in_=src[:, blk, :], func=AF.Square,
                                                   accum_out=st3[:, col:col + 1]),
                     reads=[sk], writes=[("mixt", blk), ("st3", col)])
                S.op("dve", lambda E: E.tensor_scalar(out=st3[:, col + 1:col + 2], in0=st3[:, col:col + 1],
                                                      scalar1=1.0 / 1024, scalar2=EPS, op0=ALU.mult, op1=ALU.add),
                     reads=[("st3", col)], writes=[("st3", col + 1)])
                S.op("act", lambda E: E.activation(out=st3[:, col + 2:col + 3], in_=st3[:, col + 1:col + 2], func=AF.Sqrt),
                     reads=[("st3", col + 1)], writes=[("st3", col + 2)])
                S.op("dve", lambda E: E.reciprocal(out=st3[:, col + 3:col + 4], in_=st3[:, col + 2:col + 3]),
                     reads=[("st3", col + 2)], writes=[("st3", col + 3)])
                S.op("dve", lambda E: E.scalar_tensor_tensor(out=dst[:, blk, :], in0=src[:, blk, :],
                                                             scalar=st3[:, col + 3:col + 4], in1=g_t[:],
                                                             op0=ALU.mult, op1=ALU.mult),
                     reads=[sk, ("st3", col + 3), gn], writes=[dk])

            def top16(src_ap, work_ap, vals_ap, idx_ap, rk, wk_, vk, ik):
                S.op("dve", lambda E: E.max(out=vals_ap[:, 0:8], in_=src_ap), reads=[rk], writes=[vk])
                S.op("dve", lambda E: E.max_index(out=idx_ap[:, 0:8], in_max=vals_ap[:, 0:8], in_values=src_ap),
                     reads=[rk, vk], writes=[ik])
                S.op("dve", lambda E: E.match_replace(out=work_ap, in_to_replace=vals_ap[:, 0:8], in_values=src_ap,
                                                      imm_value=-1e30), reads=[rk, vk], writes=[wk_])
                S.op("dve", lambda E: E.max(out=vals_ap[:, 8:16], in_=work_ap), reads=[wk_], writes=[vk])
                S.op("dve", lambda E: E.max_index(out=idx_ap[:, 8:16], in_max=vals_ap[:, 8:16], in_values=work_ap),
                     reads=[wk_, vk], writes=[ik])

            def pre_gen(u):
                ub = u % 2
                x1 = x1b[ub]
                xnT = xnTb[ub]
                S.dma("sp", "ldmix", mixt[:], mix_d[2 * u:2 * u + 2].rearrange("j p f -> p j f"),
                      writes=[("mixt", 0), ("mixt", 1)])
                S.dma("sp", "ldxo", xo[:], x_po[2 * u:2 * u + 2, 128:256, :].rearrange("j p f -> p j f"),
                      writes=[("xo", 0), ("xo", 1)])
                yield
                for blk in range(2):
                    for c in range(8):
                        S.op("pe", lambda E: E.transpose(out=PB[:, c, :], in_=mixt[:, blk, c * 128:(c + 1) * 128],
                                                         identity=identb[:]),
                             reads=[("mixt", blk), "identb"], writes=["PB"])
                    S.op("act", lambda E: E.copy(out=xnT[:, :, blk * 128:(blk + 1) * 128], in_=PB[:]),
                         reads=["PB"], writes=[("xnT", ub, blk)])
                    yield
                    for half in range(2):
                        for c in range(8):
                            S.op("pe", lambda E: E.matmul(PF[6][:], lhsT=xnT[:, c, blk * 128:(blk + 1) * 128],
                                                          rhs=Wout[:, c, half * 512:(half + 1) * 512],
                                                          start=(c == 0), stop=(c == 7)),
                                 reads=[("xnT", ub, blk), "Wout"], writes=["PF6"])
                        S.op("dve", lambda E: E.tensor_tensor(out=x1[:, blk, half * 512:(half + 1) * 512], in0=PF[6][:],
                                                              in1=xo[:, blk, half * 512:(half + 1) * 512], op=ALU.add),
                             reads=["PF6", ("xo", blk)], writes=[("x1", ub, blk)])
                        yield
                    rms_rows(x1, blk, gffn, xn, 4 * blk, ("x1", ub, blk), "gffn", ("mixt", blk))
                    yield
                    for c in range(8):
                        S.op("pe", lambda E: E.transpose(out=PB[:, c, :], in_=xn[:, blk, c * 128:(c + 1) * 128],
                                                         identity=identb[:]),
                             reads=[("mixt", blk), "identb"], writes=["PB"])
                    S.op("dve", lambda E: E.tensor_copy(out=xnT[:, :, blk * 128:(blk + 1) * 128], in_=PB[:]),
                         reads=["PB"], writes=[("xnT", ub, blk)])
                    yield
                for blk in range(2):
                    for r4 in range(4):
                        for q in range(4):
                            ch = 4 * r4 + q
                            for c in range(8):
                                S.op("pe", lambda E: E.matmul(PF[6][:, q * 128:(q + 1) * 128],
                                                              lhsT=Wq[:, c, ch * 128:(ch + 1) * 128],
                                                              rhs=xnT[:, c, blk * 128:(blk + 1) * 128],
                                                              start=(c == 0), stop=(c == 7)),
                                     reads=["Wq", ("xnT", ub, blk)], writes=["PF6"])
                        S.op("act", lambda E: E.copy(out=qTv[:, 4 * r4:4 * r4 + 4, :],
                                                     in_=PF[6][:].rearrange("p (a b) -> p a b", a=4)),
                             reads=["PF6"], writes=["scw"])
                        yield
                    for r4 in range(4):
                        for q in range(4):
                            ch = 4 * r4 + q
                            S.op("pe", lambda E: E.matmul(PF[6][:, q * 128:(q + 1) * 128], lhsT=qTv[:, ch, :],
                                                          rhs=skT[ch % 2][:, ch // 2, :], start=True, stop=True),
                                 reads=["scw", "skT"], writes=["PF6"])
                        if r4 % 2 == 0:
                            S.op("act", lambda E: E.copy(out=sc[:, 4 * r4:4 * r4 + 4, :], in_=PF[6][:].rearrange("p (a b) -> p a b", a=4)),
                                 reads=["PF6"], writes=["sc"])
                        else:
                            S.op("dve", lambda E: E.tensor_copy(out=sc[:, 4 * r4:4 * r4 + 4, :], in_=PF[6][:].rearrange("p (a b) -> p a b", a=4)),
                                 reads=["PF6"], writes=["sc"])
                        yield
                    for ch in range(16):
                        top16(sc[:, ch, :], scw[:, ch, :], vv[:, ch, :], ix[:, ch, :], "sc", "scw", "vv", "ix")
                        if ch % 2 == 1:
                            yield
                    vv4 = vv[:].rearrange("p (h s) k -> p h s k", s=2)
                    S.op("dve", lambda E: E.tensor_tensor(
                        out=cand.rearrange("p h (a b) -> p h a b", a=16),
                        in0=vv4[:, :, 0, :].unsqueeze(3).to_broadcast([128, 8, 16, 16]),
                        in1=vv4[:, :, 1, :].unsqueeze(2).to_broadcast([128, 8, 16, 16]), op=ALU.add),
                        reads=["vv"], writes=["sc"])
                    for h in range(8):
                        top16(cand[:, h, :], candw[:, h, :], tops[:, h, :], posu[:, h, :], "sc", "scw", "tops", "posu")
                        if h % 2 == 1:
                            yield
                    S.op("dve", lambda E: E.tensor_tensor(out=sel[:, 2, :].rearrange("p (h k) -> p h k", h=8), in0=tops[:],
                                                          in1=tops[:, :, 0:1].to_broadcast([128, 8, 16]), op=ALU.subtract),
                         reads=["tops"], writes=["gate"])
                    S.op("act", lambda E: E.activation(out=sel[:, 2, :], in_=sel[:, 2, :], func=AF.Exp),
                         reads=["gate"], writes=["gate"])
                    S.op("dve", lambda E: E.tensor_reduce(out=gsm[:, 0:8], in_=sel[:, 2, :].rearrange("p (h k) -> p h k", h=8),
                                                          axis=AX.X, op=ALU.add), reads=["gate"], writes=["gsm"])
                    S.op("dve", lambda E: E.reciprocal(out=gsm[:, 8:16], in_=gsm[:, 0:8]), reads=["gsm"], writes=["gsr"])
                    S.op("dve", lambda E: E.tensor_tensor(out=sel[:, 2, :].rearrange("p (h k) -> p h k", h=8),
                                                          in0=sel[:, 2, :].rearrange("p (h k) -> p h k", h=8),
                                                          in1=gsm[:, 8:16].unsqueeze(2).to_broadcast([128, 8, 16]), op=ALU.mult),
                         reads=["gate", "gsr"], writes=["gate"])
                    yield
                    S.op("dve", lambda E: E.tensor_copy(out=posf3[:], in_=posu[:]), reads=["posu"], writes=["posf3"])
                    for h2 in range(2):
                        hs = slice(4 * h2, 4 * h2 + 4)
                        S.op("dve", lambda E: E.tensor_tensor(
                            out=oh[:, hs].rearrange("p h k a -> p (h k) a"),
                            in0=posf3[:, hs, :].rearrange("p h k -> p (h k)").unsqueeze(2).to_broadcast([128, 64, 16]),
                            in1=thr16[:].unsqueeze(1).to_broadcast([128, 64, 16]), op=ALU.is_ge),
                            reads=["posf3", "thr16"], writes=["scw"])
                        S.op("dve", lambda E: E.tensor_reduce(
                            out=paf[:, hs, :].rearrange("p h k -> p (h k)"),
                            in_=oh[:, hs].rearrange("p h k a -> p (h k) a"), axis=AX.X, op=ALU.add),
                            reads=["scw"], writes=["paf"])
                    S.op("dve", lambda E: E.scalar_tensor_tensor(out=pbf[:], in0=paf[:], scalar=-16.0, in1=posf3[:],
                                                                 op0=ALU.mult, op1=ALU.add),
                         reads=["paf", "posf3"], writes=["pbf"])
                    yield
                    ix4 = ix[:].rearrange("p (h s) k -> p h s k", s=2)
                    S.op("dve", lambda E: E.tensor_copy(out=i1f[:], in_=ix4[:, :, 0, :]), reads=["ix"], writes=["i1f"])
                    S.op("dve", lambda E: E.tensor_copy(out=i2f[:], in_=ix4[:, :, 1, :]), reads=["ix"], writes=["i2f"])
                    for side, (pf_, if_) in enumerate(((paf, i1f), (pbf, i2f))):
                        for h2 in range(2):
                            hs = slice(4 * h2, 4 * h2 + 4)
                            S.op("dve", lambda E: E.tensor_tensor(
                                out=oh[:, hs].rearrange("p h k a -> p (h k) a"),
                                in0=pf_[:, hs, :].rearrange("p h k -> p (h k)").unsqueeze(2).to_broadcast([128, 64, 16]),
                                in1=iotaf[:, 0:16].unsqueeze(1).to_broadcast([128, 64, 16]), op=ALU.is_equal),
                                reads=["paf", "pbf", "iotaf"], writes=["scw"])
                            for h in range(4 * h2, 4 * h2 + 4):
                                S.op("dve", lambda E: E.tensor_tensor(
                                    out=oh[:, h], in0=oh[:, h],
                                    in1=if_[:, h, :].unsqueeze(1).to_broadcast([128, 16, 16]), op=ALU.mult),
                                    reads=["scw", "i1f", "i2f"], writes=["scw"])
                            S.op("dve", lambda E: E.tensor_reduce(
                                out=sel[:, side, 64 * h2:64 * h2 + 64],
                                in_=oh[:, hs].rearrange("p h k a -> p (h k) a"), axis=AX.X, op=ALU.add),
                                reads=["scw"], writes=[("sel", side)])
                            yield
                    for w3 in range(3):
                        S.op("pe", lambda E: E.transpose(out=PF[6][:, w3 * 128:(w3 + 1) * 128], in_=sel[:, w3, :],
                                                         identity=identf[:]),
                             reads=[("sel", 0), ("sel", 1), "gate", "identf"], writes=["PF6"])
                    S.op("act", lambda E: E.copy(out=hkT[:, :, blk * 128:(blk + 1) * 128],
                                                 in_=PF[6][:, 0:384].rearrange("p (a b) -> p a b", a=3)),
                         reads=["PF6"], writes=[("hkT", blk)])
                    yield

            def m_phase(u):
                for t4 in range(64):
                    pf, pfn = PF[5 + t4 % 2], "PF%d" % (5 + t4 % 2)
                    for q in range(4):
                        tk = 4 * t4 + q
                        ab = tk % 3
                        S.op("dve", lambda E: E.tensor_scalar(out=At[ab][:], in0=iotab[:], scalar1=hkT[:, 0, tk:tk + 1],
                                                              scalar2=hkT[:, 2, tk:tk + 1], op0=ALU.is_equal, op1=ALU.mult),
                             reads=["iotab", ("hkT", tk // 128)], writes=[("At", ab)])
                        S.op("dve", lambda E: E.tensor_scalar(out=Bt[ab][:], in0=iotab[:], scalar1=hkT[:, 1, tk:tk + 1],
                                                              scalar2=None, op0=ALU.is_equal),
                             reads=["iotab", ("hkT", tk // 128)], writes=[("Bt", ab)])
                        S.op("pe", lambda E: E.matmul(pf[:, q * 128:(q + 1) * 128], lhsT=Bt[ab][:], rhs=At[ab][:],
                                                      start=True, stop=True),
                             reads=[("At", ab), ("Bt", ab)], writes=[pfn])
                    S.op("act", lambda E: E.copy(out=GT[:, :, 4 * t4:4 * t4 + 4],
                                                 in_=pf[:].rearrange("p (t i) -> p i t", t=4)),
                         reads=[pfn], writes=[("GT", t4)] + ([("gst", t4 % 4)] if (u == 0 and t4 < 4) else []))

            OA = [[PF[0], PF[1]], [PF[2], PF[3]]]
            OAn = [["PF0", "PF1"], ["PF2", "PF3"]]

            def ld_tab(i):
                sl = i % 3
                S.dma("sp", "ldd%d" % sl, dsb[sl][:], down_b[i], writes=[("dsb", sl)])
                su = i % 4
                S.dma("sp", "ldu%d" % su, usb[su][:], up_b[i], writes=[("usb", su)])

            def dense_H(u, i):
                ub = u % 2
                xnT = xnTb[ub]
                sl = i % 3
                hb_, hbn = PF[4 + i % 2], "PF%d" % (4 + i % 2)
                for c in range(8):
                    S.op("pe", lambda E: E.matmul(hb_[:, 0:256], lhsT=dsb[sl][:, c, :],
                                                  rhs=xnT[:, c, :], start=(c == 0), stop=(c == 7)),
                         reads=[("dsb", sl), ("xnT", ub, 0), ("xnT", ub, 1)], writes=[hbn])
                S.op("act", lambda E: E.activation(out=gl[i % 3][:], in_=hb_[:, 0:256], func=AF.Gelu),
                     reads=[hbn], writes=[("gl", i % 3)])
                S.op("pool", lambda E: E.tensor_tensor(out=wg[i % 3][:], in0=gl[i % 3][:], in1=GT[:, i, :], op=ALU.mult),
                     reads=[("gl", i % 3)] + [("GT", t4) for t4 in (0, 63)], writes=[("wg", i % 3)])

            def dense_UP(i):
                su = i % 4
                for blk in range(2):
                    for half in range(2):
                        S.op("pe", lambda E: E.matmul(OA[blk][half][:], lhsT=wg[i % 3][:, blk * 128:(blk + 1) * 128],
                                                      rhs=usb[su][:, half * 512:(half + 1) * 512],
                                                      start=(i == 0), stop=(i == 127)),
                             reads=[("wg", i % 3), ("usb", su)], writes=[OAn[blk][half]])

            def advance(gen):
                if gen is None:
                    return None
                try:
                    next(gen)
                    return gen
                except StopIteration:
                    return None

            def o_phase(u):
                ub = u % 2
                x1 = x1b[ub]
                for blk in range(2):
                    for half in range(2):
                        S.op("dve", lambda E: E.tensor_tensor(out=x1[:, blk, half * 512:(half + 1) * 512], in0=OA[blk][half][:],
                                                              in1=x1[:, blk, half * 512:(half + 1) * 512], op=ALU.add),
                             reads=[OAn[blk][half], ("x1", ub, blk)], writes=[("x1", ub, blk)])
                    rms_rows(x1, blk, gfin, xo, 8 + 4 * blk, ("x1", ub, blk), "gfin", ("xo", blk))
                S.dma("sp", "oout", out[256 * u:256 * u + 256, :].rearrange("(j p) f -> p j f", p=128), xo[:],
                      reads=[("xo", 0), ("xo", 1)], writes=[("out", u)])

            g0 = pre_gen(0)
            while g0 is not None:
                g0 = advance(g0)
            m_phase(0)
            for u in range(ntile):
                nxt = pre_gen(u + 1) if u + 1 < ntile else None
                for i in range(2):
                    ld_tab(i)
                for step in range(130):
                    if step < 128:
                        dense_H(u, step)
                    if step >= 2:
                        dense_UP(step - 2)
                    if step + 2 < 128:
                        ld_tab(step + 2)
                    if step % 2 == 1:
                        nxt = advance(nxt)
                while nxt is not None:
                    nxt = advance(nxt)
                o_phase(u)
                if u + 1 < ntile:
                    m_phase(u + 1)
        S.finish()
    return nc


def own_blocks(half):
    a = (0, 3) if half == 0 else (1, 2)
    return [4 * g + o for g in range(16) for o in a]


def consts(half):
    p = np.arange(128)
    invf = (10000.0 ** (-(p % 32).astype(np.float32) / 32.0)).astype(np.float32).reshape(128, 1)
    sgn = np.where((p % 64) < 32, -1.0, 1.0).astype(np.float32).reshape(128, 1)
    ident = np.eye(128, dtype=np.float32)
    uneg = -(p[:, None] >= p[None, :]).astype(np.float32)
    diag = np.where(p[:, None] < p[None, :], 0.0, NEG).astype(np.float32)
    full = np.full((128, 128), NEG, np.float32)
    none = np.zeros((128, 128), np.float32)
    if half == 0:
        sbm = np.stack([full, diag, diag, none])
    else:
        sbm = np.stack([diag, none, full, diag])
    q = np.arange(128)[:, None] + 128
    k = np.arange(256)[None, :]
    diff = q - k
    band = np.where((diff >= 0) & (diff < 128), 0.0, NEG).astype(np.float32)
    first = band.copy()
    if half == 0:
        first[:, :128] = NEG
    swm = np.stack([first, band])
    iota = np.tile(np.arange(128, dtype=np.float32)[None, :], (128, 1))
    return dict(c_invf=invf, c_sgn=sgn, c_ident=ident, c_uneg=uneg, c_sbm=sbm, c_swm=swm, c_iota=iota)


def prep_core(inp, core):
    b, half = core // 2, core % 2
    x = np.asarray(inp["x"][b], dtype=np.float32)
    pos = np.asarray(inp["positions"][b]).astype(np.int32)
    own = own_blocks(half)
    xb = x.reshape(64, 128, 1024)
    pb = pos.reshape(64, 128)
    x_po = np.zeros((32, 256, 1024), np.float32)
    pos_po = np.zeros((32, 256), np.int32)
    for j, B in enumerate(own):
        if B > 0:
            x_po[j, :128] = xb[B - 1]
            pos_po[j, :128] = pb[B - 1]
        x_po[j, 128:] = xb[B]
        pos_po[j, 128:] = pb[B]
    m = dict(
        x_all=np.ascontiguousarray(x), x_po=x_po, pos_po=pos_po.reshape(-1),
        w_in=np.ascontiguousarray(inp["w_in"][0]), attn_norm=np.ascontiguousarray(inp["attn_norm"][0]),
        sb_out_norm=np.ascontiguousarray(inp["sb_out_norm"][0]), swa_sinks=np.ascontiguousarray(inp["swa_sinks"][0]),
        swa_out_norm=np.ascontiguousarray(inp["swa_out_norm"][0]), w_out=np.ascontiguousarray(inp["w_out"][0]),
        ffn_norm=np.ascontiguousarray(inp["ffn_norm"][0]), peer_wq=np.ascontiguousarray(inp["peer_w_query"][0]),
        sk1T=np.ascontiguousarray(np.transpose(inp["peer_sub_keys_1"][0], (0, 2, 1))),
        sk2T=np.ascontiguousarray(np.transpose(inp["peer_sub_keys_2"][0], (0, 2, 1))),
        final_norm=np.ascontiguousarray(inp["final_norm"]),
    )
    m.update(consts(half))
    return m


from concourse.bass_utils import run_bass_kernel_spmd

_NC = None


def kernel(**inputs):
    global _NC
    inp = {k: np.asarray(v) for k, v in inputs.items()}
    if _NC is None:
        _NC = build(upto=3, nqt=8, nblk1a=64)
    downT = np.ascontiguousarray(inp["peer_expert_down"][0].T)
    up = np.ascontiguousarray(inp["peer_expert_up"][0])
    maps = []
    for c in range(8):
        m = prep_core(inp, c)
        m["downT"] = downT
        m["up"] = up
        maps.append(m)
    res = run_bass_kernel_spmd(_NC, maps, core_ids=list(range(8)))
    out = np.zeros((4, 8192, 1024), np.float32)
    for c in range(8):
        o = np.asarray(res.results[c]["out"]).reshape(32, 128, 1024)
        ob = out[c // 2].reshape(64, 128, 1024)
        for j, B in enumerate(own_blocks(c % 2)):
            ob[B] = o[j]
    return out
```

```python
import numpy as np
import concourse.bass as bass
import concourse.mybir as mybir

F32 = mybir.dt.float32
BF16 = mybir.dt.bfloat16
I32 = mybir.dt.int32
U32 = mybir.dt.uint32
AF = mybir.ActivationFunctionType
ALU = mybir.AluOpType
AX = mybir.AxisListType


class Sched:
    ENG = ("pe", "act", "dve", "pool", "sp")

    def __init__(self, nc):
        self.nc = nc
        self.e = {"pe": nc.tensor, "act": nc.scalar, "dve": nc.vector,
                  "pool": nc.gpsimd, "sp": nc.sync}
        self.sem = {k: nc.alloc_semaphore("sem_" + k) for k in self.ENG}
        self.cnt = {k: 0 for k in self.ENG}
        self.seen = {k: {} for k in self.ENG}
        self.semobj = {}
        for k in self.ENG:
            self.semobj["sem_" + k] = self.sem[k]
        self.dsem = {}
        self.bufs = {}
        self.nwait = 0
        self.ninst = 0

    def _need(self, eng, tok, waits):
        if tok is None:
            return
        sname, val, src = tok
        if src == eng and eng == "pe":
            return
        if self.seen[eng].get(sname, 0) >= val:
            return
        waits[sname] = max(waits.get(sname, 0), val)

    def _deps(self, eng, reads, writes):
        waits = {}
        for k in reads:
            b = self.bufs.get(k)
            if b is not None:
                self._need(eng, b["w"], waits)
        for k in writes:
            b = self.bufs.get(k)
            if b is not None:
                w = b["w"]
                if w is not None:
                    self._need(eng, w, waits)
                for r in b["r"]:
                    self._need(eng, r, waits)
        for sname, val in waits.items():
            self.e[eng].wait_ge(self.semobj[sname], val)
            self.seen[eng][sname] = val
            self.nwait += 1

    def _commit(self, tok, reads, writes):
        for k in reads:
            b = self.bufs.setdefault(k, {"w": None, "r": []})
            b["r"].append(tok)
            if len(b["r"]) > 12:
                last = {}
                for r in b["r"]:
                    if r[0] not in last or last[r[0]][1] < r[1]:
                        last[r[0]] = r
                b["r"] = list(last.values())
        for k in writes:
            self.bufs[k] = {"w": tok, "r": []}

    def op(self, eng, fn, reads=(), writes=()):
        self._deps(eng, reads, writes)
        inst = fn(self.e[eng])
        self.cnt[eng] += 1
        inst.then_inc(self.sem[eng], 1)
        tok = ("sem_" + eng, self.cnt[eng], eng)
        self._commit(tok, reads, writes)
        self.ninst += 1
        return inst

    def dma(self, eng, slot, out, in_, reads=(), writes=(), **kw):
        if slot not in self.dsem:
            s = self.nc.alloc_semaphore("dsem_" + slot)
            self.dsem[slot] = [s, 0]
            self.semobj["dsem_" + slot] = s
        self._deps(eng, reads, writes)
        d = self.dsem[slot]
        inst = self.e[eng].dma_start(out=out, in_=in_, **kw)
        d[1] += 16
        inst.then_inc(d[0], 16)
        tok = ("dsem_" + slot, d[1], None)
        self._commit(tok, reads, writes)
        self.ninst += 1
        return inst

    def barrier(self):
        for eng in self.ENG:
            for other in self.ENG:
                if other == eng or self.cnt[other] == 0:
                    continue
                if self.seen[eng].get("sem_" + other, 0) < self.cnt[other]:
                    self.e[eng].wait_ge(self.sem[other], self.cnt[other])
                    self.seen[eng]["sem_" + other] = self.cnt[other]
            for slot, (s, v) in self.dsem.items():
                if v and self.seen[eng].get("dsem_" + slot, 0) < v:
                    self.e[eng].wait_ge(s, v)
                    self.seen[eng]["dsem_" + slot] = v
        self.bufs = {}

    def finish(self, eng="sp"):
        for slot, (s, v) in self.dsem.items():
            if v:
                self.e[eng].wait_ge(s, v)
        for other in self.ENG:
            if other != eng and self.cnt[other]:
                self.e[eng].wait_ge(self.sem[other], self.cnt[other])


import math
from contextlib import ExitStack
import numpy as np
import concourse.bass as bass
import concourse.mybir as mybir

EPS = 1e-6
NEG = -30000.0
PI = math.pi
C1 = 6.28125
C2 = 2.0 * math.pi - 6.28125
NB = 64
NOWN = 32
DBG3 = False
SKEW1B = False
NUNITS = 64


def build(upto=3, nqt=8, nblk1a=NB):
    nc = bass.Bass("TRN2", target_bir_lowering=False)

    def din(name, shape, dt=F32):
        return nc.dram_tensor(name, shape, dt, kind="ExternalInput").ap()

    def dscr(name, shape, dt):
        return nc.dram_tensor(name, shape, dt, kind="Internal").ap()

    x_all = din("x_all", [8192, 1024])
    x_po = din("x_po", [NOWN, 256, 1024])
    pos_po = din("pos_po", [NOWN * 256], I32)
    w_in = din("w_in", [1024, 2304])
    attn_norm = din("attn_norm", [1024])
    sb_out_norm = din("sb_out_norm", [512])
    swa_sinks = din("swa_sinks", [8])
    swa_out_norm = din("swa_out_norm", [512])
    w_out = din("w_out", [1024, 1024])
    ffn_norm = din("ffn_norm", [1024])
    peer_wq = din("peer_wq", [1024, 2048])
    sk1T = din("sk1T", [8, 128, 128])
    sk2T = din("sk2T", [8, 128, 128])
    if upto >= 3:
        downT = din("downT", [1024, 16384])
        up = din("up", [16384, 1024])
    final_norm = din("final_norm", [1024])
    c_invf = din("c_invf", [128, 1])
    c_sgn = din("c_sgn", [128, 1])
    c_ident = din("c_ident", [128, 128])
    c_uneg = din("c_uneg", [128, 128])
    c_sbm = din("c_sbm", [4, 128, 128])
    c_swm = din("c_swm", [2, 128, 256])
    c_iota = din("c_iota", [128, 128])

    out = nc.dram_tensor("out", [NOWN * 128, 1024], F32, kind="ExternalOutput").ap()
    dbg3 = nc.dram_tensor("dbg3", [128, 640], F32, kind="ExternalOutput").ap() if DBG3 else None

    qsb_d = dscr("qsb_d", [NOWN, 128, 4, 128], BF16)
    qsw_d = dscr("qsw_d", [NOWN, 128, 4, 128], BF16)
    ksw_d = dscr("ksw_d", [NOWN, 128, 256], BF16)
    vsw_d = dscr("vsw_d", [NOWN, 128, 2, 128], BF16)
    mix_d = dscr("mix_d", [NOWN, 128, 1024], BF16)
    dbg = None
    if upto < 3:
        dbg = nc.dram_tensor("dbg", [NOWN, 128, 1024], F32, kind="ExternalOutput").ap()

    S = Sched(nc)
    w_in_v = w_in.rearrange("(c p) n -> p c n", p=128)

    with ExitStack() as top:
        def sb(name, shape, dt, st=top):
            return st.enter_context(nc.sbuf_tensor(name, shape, dt))

        PF = [top.enter_context(nc.psum_tensor("pf%d" % i, [128, 512], F32)) for i in range(7)]
        PB = top.enter_context(nc.psum_tensor("pb", [128, 8, 128], BF16))

        identb = sb("identb", [128, 128], BF16)
        unegb = sb("unegb", [128, 128], BF16)
        invf = sb("invf", [128, 1], F32)
        sgn = sb("sgn", [128, 1], F32)
        S.dma("pool", "c0", identb[:], c_ident, writes=["identb"])
        S.dma("pool", "c1", unegb[:], c_uneg, writes=["unegb"])
        S.dma("sp", "c2", invf[:], c_invf, writes=["invf"])
        S.dma("sp", "c3", sgn[:], c_sgn, writes=["sgn"])

        def norm_T(st, src, k, xt, junk, hb, hT, ss, gb, evac_eng):
            S.dma("sp", "xt%d" % k, xt[k][:], src, writes=[("xt", k)])
            S.op("act", lambda E: E.activation(out=junk[:], in_=xt[k][:], func=AF.Square,
                                               accum_out=ss[:, k:k + 1]),
                 reads=[("xt", k)], writes=["junk", ("ss", k)])
            S.op("dve", lambda E: E.tensor_scalar(out=ss[:, 2 + k:3 + k], in0=ss[:, k:k + 1], scalar1=1.0 / 1024,
                                                  scalar2=EPS, op0=ALU.mult, op1=ALU.add),
                 reads=[("ss", k)], writes=[("ms", k)])
            S.op("act", lambda E: E.activation(out=ss[:, 4 + k:5 + k], in_=ss[:, 2 + k:3 + k], func=AF.Sqrt),
                 reads=[("ms", k)], writes=[("sd", k)])
            S.op("dve", lambda E: E.reciprocal(out=ss[:, 6 + k:7 + k], in_=ss[:, 4 + k:5 + k]),
                 reads=[("sd", k)], writes=[("rstd", k)])
            S.op("dve", lambda E: E.scalar_tensor_tensor(out=hb[k][:], in0=xt[k][:], scalar=ss[:, 6 + k:7 + k],
                                                         in1=gb[:], op0=ALU.mult, op1=ALU.mult),
                 reads=[("xt", k), ("rstd", k), "gb"], writes=[("hb", k)])
            for c in range(8):
                S.op("pe", lambda E: E.transpose(out=PB[:, c, :], in_=hb[k][:, c * 128:(c + 1) * 128],
                                                 identity=identb[:]),
                     reads=[("hb", k), "identb"], writes=["PB"])
            if evac_eng == "act":
                S.op("act", lambda E: E.copy(out=hT[k][:], in_=PB[:]), reads=["PB"], writes=[("hT", k)])
            else:
                S.op("dve", lambda E: E.tensor_copy(out=hT[k][:], in_=PB[:]), reads=["PB"], writes=[("hT", k)])

        if upto >= 3:
            down_b = dscr("down_b", [128, 128, 8, 128], BF16)
            up_b = dscr("up_b", [128, 128, 1024], BF16)
            downT_v = downT.rearrange("(c p) (g e) -> g p c e", p=128, e=512)
            up_v = up.rearrange("(g i e) f -> g e i f", i=4, e=128)
        with ExitStack() as p1:
            W2 = sb("W2", [128, 8, 1920], BF16, p1)
            gb = sb("gb", [128, 1024], F32, p1)
            xt = [sb("xt%d" % i, [128, 1024], F32, p1) for i in range(2)]
            junk = sb("junk", [128, 1024], BF16, p1)
            hb = [sb("hb%d" % i, [128, 1024], BF16, p1) for i in range(2)]
            hT = [sb("hT%d" % i, [128, 8, 128], BF16, p1) for i in range(2)]
            ss = sb("ss", [128, 8], F32, p1)
            posi = sb("posi", [128, 128], I32, p1)
            posf = sb("posf", [128, 128], F32, p1)
            ang = sb("ang", [128, 128], F32, p1)
            tq = sb("tq", [128, 128], F32, p1)
            ki = sb("ki", [128, 128], I32, p1)
            kf = sb("kf", [128, 128], F32, p1)
            rr = sb("rr", [128, 128], F32, p1)
            gg = sb("gg", [128, 128], F32, p1)
            cosb2 = [sb("cosb%d" % i, [128, 1, 128], F32, p1) for i in range(2)]
            sinb2 = [sb("sinb%d" % i, [128, 1, 128], F32, p1) for i in range(2)]
            t1 = sb("t1", [128, 4, 128], F32, p1)
            t2 = sb("t2", [128, 4, 128], F32, p1)
            qsb_s = [sb("qsb_s%d" % i, [128, 4, 128], BF16, p1) for i in range(2)]
            qsw_s = [sb("qsw_s%d" % i, [128, 4, 128], BF16, p1) for i in range(2)]
            ksw_s = [sb("ksw_s%d" % i, [128, 256], BF16, p1) for i in range(2)]
            vsw_s = [sb("vsw_s%d" % i, [128, 2, 128], BF16, p1) for i in range(2)]

            NST = 4
            stf = [sb("stf%d" % i, [128, 4096], F32, p1) for i in range(NST)]
            if upto >= 3:
                stb = [sb("stb%d" % i, [128, 4096], BF16, p1) for i in range(NST)]

            def w2_chunk(dst_c0, src_c0, n, k, eng):
                fv = stf[k][:, 0:8 * n].rearrange("p (c e) -> p c e", c=8)
                S.dma("sp", "w2s%d" % k, fv, w_in_v[:, :, src_c0:src_c0 + n], writes=[("stf", k)])
                S.op(eng, lambda E: E.tensor_copy(out=W2[:, :, dst_c0:dst_c0 + n], in_=fv),
                     reads=[("stf", k)], writes=["W2"])

            def w2_swap(dst_c0, src_c0, n, eng):
                sv = W2[:, :, src_c0:src_c0 + n].rearrange("p c (h two i) -> p c h two i", two=2, i=32)
                dv = W2[:, :, dst_c0:dst_c0 + n].rearrange("p c (h two i) -> p c h two i", two=2, i=32)
                for two in range(2):
                    S.op(eng, lambda E: E.tensor_copy(out=dv[:, :, :, two, :], in_=sv[:, :, :, 1 - two, :]),
                         reads=["W2"], writes=["W2"])

            fkv = stf[3][:, 0:2048].rearrange("p (c e) -> p c e", c=8)
            S.dma("sp", "w2s3", fkv, w_in_v[:, :, 2048:2304], writes=[("stf", 3)])
            S.op("dve", lambda E: E.tensor_copy(out=W2[:, :, 1536:1664], in_=fkv[:, :, 0:128]), reads=[("stf", 3)], writes=["W2"])
            S.op("pool", lambda E: E.tensor_copy(out=W2[:, :, 1792:1920], in_=fkv[:, :, 128:256]), reads=[("stf", 3)], writes=["W2"])
            w2_swap(1664, 1536, 128, "dve")
            w2_chunk(512, 1536, 512, 3, "dve")
            w2_chunk(0, 0, 512, 2, "pool")
            w2_swap(1024, 512, 512, "pool")
            pc_steps = [(g, which) for g in range(32) for which in range(2)]

            def pc_load(n):
                g, which = pc_steps[n]
                k = n % NST
                if which == 0:
                    src = downT_v[g]
                    fv = stf[k][:].rearrange("p (c e) -> p c e", c=8)
                else:
                    src = up_v[g]
                    fv = stf[k][:].rearrange("p (i f) -> p i f", i=4)
                S.dma("sp", "pc_in%d" % k, fv, src, writes=[("stf", k)])

            def pc_cast_store(n):
                g, which = pc_steps[n]
                k = n % NST
                if which == 0:
                    dst = down_b[4 * g:4 * g + 4].rearrange("i p c e -> p i (c e)")
                    co = stb[k][:].rearrange("p (i c e) -> p c i e", i=4, c=8)
                    ci = stf[k][:].rearrange("p (c i e) -> p c i e", c=8, i=4)
                else:
                    dst = up_b[4 * g:4 * g + 4].rearrange("i p f -> p i f")
                    co, ci = stb[k][:], stf[k][:]
                bv = stb[k][:].rearrange("p (i x) -> p i x", i=4)
                eng = "pool" if n % 2 == 0 else "dve"
                S.op(eng, lambda E: E.tensor_copy(out=co, in_=ci), reads=[("stf", k)], writes=[("stb", k)])
                S.dma("sp", "pc_out%d" % k, dst, bv, reads=[("stb", k)], writes=[("tab", which, g)])

            W2_PENDING = True
            S.dma("sp", "gb", gb[:], attn_norm.partition_broadcast(128), writes=["gb"])

            def rope_tables(j, sub):
                cosb, sinb = cosb2[sub], sinb2[sub]
                S.dma("sp", "posi", posi[:], pos_po[j * 256 + sub * 128: j * 256 + sub * 128 + 128].partition_broadcast(128),
                      writes=["posi"])
                S.op("dve", lambda E: E.tensor_copy(out=posf[:], in_=posi[:]), reads=["posi"], writes=["posf"])
                S.op("dve", lambda E: E.tensor_scalar(out=ang[:], in0=posf[:], scalar1=invf[:, 0:1], scalar2=None,
                                                      op0=ALU.mult), reads=["posf", "invf"], writes=["ang"])
                S.op("dve", lambda E: E.tensor_scalar(out=tq[:], in0=ang[:], scalar1=1.0 / (2 * PI), scalar2=None,
                                                      op0=ALU.mult), reads=["ang"], writes=["tq"])
                S.op("dve", lambda E: E.tensor_copy(out=ki[:], in_=tq[:]), reads=["tq"], writes=["ki"])
                S.op("dve", lambda E: E.tensor_copy(out=kf[:], in_=ki[:]), reads=["ki"], writes=["kf"])
                S.op("dve", lambda E: E.scalar_tensor_tensor(out=rr[:], in0=kf[:], scalar=-C1, in1=ang[:],
                                                             op0=ALU.mult, op1=ALU.add),
                     reads=["kf", "ang"], writes=["rr"])
                S.op("dve", lambda E: E.scalar_tensor_tensor(out=rr[:], in0=kf[:], scalar=-C2, in1=rr[:],
                                                             op0=ALU.mult, op1=ALU.add),
                     reads=["kf", "rr"], writes=["rr"])
                S.op("dve", lambda E: E.tensor_scalar(out=gg[:], in0=rr[:], scalar1=PI, scalar2=2 * PI,
                                                      op0=ALU.is_gt, op1=ALU.mult), reads=["rr"], writes=["gg"])
                S.op("dve", lambda E: E.tensor_tensor(out=rr[:], in0=rr[:], in1=gg[:], op=ALU.subtract),
                     reads=["rr", "gg"], writes=["rr"])
                S.op("dve", lambda E: E.tensor_scalar(out=gg[:], in0=rr[:], scalar1=-PI, scalar2=2 * PI,
                                                      op0=ALU.is_lt, op1=ALU.mult), reads=["rr"], writes=["gg"])
                S.op("dve", lambda E: E.tensor_tensor(out=rr[:], in0=rr[:], in1=gg[:], op=ALU.add),
                     reads=["rr", "gg"], writes=["rr"])
                S.op("dve", lambda E: E.tensor_scalar(out=rr[:], in0=rr[:], scalar1=3.14159, scalar2=-3.14159,
                                                      op0=ALU.min, op1=ALU.max), reads=["rr"], writes=["rr"])
                S.op("act", lambda E: E.activation(out=sinb[:, 0, :], in_=rr[:], func=AF.Sin, scale=sgn[:, 0:1]),
                     reads=["rr", "sgn"], writes=[("sinb", sub)])
                S.op("dve", lambda E: E.scalar_tensor_tensor(out=gg[:], in0=rr[:], scalar=-1.0, in1=rr[:], op0=ALU.mult, op1=ALU.max),
                     reads=["rr"], writes=["gg"])
                S.op("act", lambda E: E.activation(out=cosb[:, 0, :], in_=gg[:], func=AF.Sin, scale=-1.0,
                                                   bias=PI / 2), reads=["gg"], writes=[("cosb", sub)])

            def proj_T(ps_ap, wc0, k, nm):
                for c in range(8):
                    S.op("pe", lambda E: E.matmul(ps_ap, lhsT=W2[:, c, wc0:wc0 + 128], rhs=hT[k][:, c, :],
                                                  start=(c == 0), stop=(c == 7)),
                         reads=["W2", ("hT", k)], writes=[nm])

            def stage_a(j, sub):
                norm_T(p1, x_po[j, sub * 128:(sub + 1) * 128, :], sub, xt, junk, hb, hT, ss, gb,
                       "act" if sub == 0 else "dve")
                rope_tables(j, sub)

            def stage_b(j, sub):
                    jb = j % 2
                    k = sub
                    cosb, sinb = cosb2[sub], sinb2[sub]
                    kvb = PF[4] if sub == 0 else PF[3]
                    kvn = "PF4" if sub == 0 else "PF3"
                    proj_T(kvb[:, 0:128], 1536, k, kvn)
                    proj_T(kvb[:, 128:256], 1664, k, kvn)
                    for c in range(8):
                        S.op("pe", lambda E: E.matmul(kvb[:, 256:384], lhsT=hT[k][:, c, :], rhs=W2[:, c, 1792:1920],
                                                      start=(c == 0), stop=(c == 7)),
                             reads=["W2", ("hT", k)], writes=[kvn])
                    S.op("dve", lambda E: E.tensor_tensor(out=t1[:, 0, :], in0=kvb[:, 0:128], in1=cosb[:, 0, :], op=ALU.mult),
                         reads=[kvn, ("cosb", sub)], writes=["t1"])
                    S.op("dve", lambda E: E.tensor_tensor(out=t2[:, 0, :], in0=kvb[:, 128:256], in1=sinb[:, 0, :], op=ALU.mult),
                         reads=[kvn, ("sinb", sub)], writes=["t2"])
                    S.op("pool", lambda E: E.tensor_tensor(out=ksw_s[jb][:, sub * 128:(sub + 1) * 128], in0=t1[:, 0, :],
                                                           in1=t2[:, 0, :], op=ALU.add),
                         reads=["t1", "t2"], writes=[("ksw_s", jb)])
                    S.op("act", lambda E: E.copy(out=vsw_s[jb][:, sub, :], in_=kvb[:, 256:384]),
                         reads=[kvn], writes=[("vsw_s", jb)])
                    if sub == 1:
                        for hp in range(4):
                            proj_T(PF[0][:, hp * 128:(hp + 1) * 128], hp * 128, k, "PF0")
                        for p in range(4):
                            proj_T(PF[1][:, p * 128:(p + 1) * 128], 512 + p * 128, k, "PF1")
                        for p in range(4):
                            proj_T(PF[2][:, p * 128:(p + 1) * 128], 1024 + p * 128, k, "PF2")
                        S.op("act", lambda E: E.activation(out=qsb_s[jb][:].rearrange("p a b -> p (a b)"), in_=PF[0][:],
                                                           func=AF.Copy, scale=0.125),
                             reads=["PF0"], writes=[("qsb_s", jb)])
                        S.op("dve", lambda E: E.scalar_tensor_tensor(
                            out=t1[:], in0=PF[1][:].rearrange("p (a b) -> p a b", a=4), scalar=0.125,
                            in1=cosb[:].to_broadcast([128, 4, 128]), op0=ALU.mult, op1=ALU.mult),
                            reads=["PF1", ("cosb", sub)], writes=["t1"])
                        S.op("dve", lambda E: E.scalar_tensor_tensor(
                            out=t2[:], in0=PF[2][:].rearrange("p (a b) -> p a b", a=4), scalar=0.125,
                            in1=sinb[:].to_broadcast([128, 4, 128]), op0=ALU.mult, op1=ALU.mult),
                            reads=["PF2", ("sinb", sub)], writes=["t2"])
                        S.op("pool", lambda E: E.tensor_tensor(out=qsw_s[jb][:], in0=t1[:], in1=t2[:], op=ALU.add),
                             reads=["t1", "t2"], writes=[("qsw_s", jb)])
            def stage_out(j):
                jb = j % 2
                S.dma("sp", "o_qsb%d" % jb, qsb_d[j], qsb_s[jb][:], reads=[("qsb_s", jb)], writes=[("qsb_d", j)])
                S.dma("sp", "o_qsw%d" % jb, qsw_d[j], qsw_s[jb][:], reads=[("qsw_s", jb)], writes=[("qsw_d", j)])
                S.dma("sp", "o_ksw%d" % jb, ksw_d[j], ksw_s[jb][:], reads=[("ksw_s", jb)], writes=[("ksw_d", j)])
                S.dma("sp", "o_vsw%d" % jb, vsw_d[j], vsw_s[jb][:], reads=[("vsw_s", jb)], writes=[("vsw_d", j)])

            units = [(j, sub) for j in range(NOWN) for sub in range(2)][:NUNITS]
            if SKEW1B:
                stage_a(*units[0])
            if upto >= 3:
                for n in range(NST - 1):
                    pc_load(n)
            for n, (j, sub) in enumerate(units):
                if SKEW1B:
                    if n + 1 < len(units):
                        stage_a(*units[n + 1])
                else:
                    stage_a(j, sub)
                if upto >= 3:
                    if n + NST - 1 < len(pc_steps):
                        pc_load(n + NST - 1)
                    pc_cast_store(n)
                stage_b(j, sub)
                if sub == 1:
                    stage_out(j)
            S.barrier()

        if upto == 0:
            S.finish()
            return nc

        with ExitStack() as p2:
            KT = sb("KT", [128, 4, 8192], BF16, p2)
            V = sb("V", [128, NB, 512], BF16, p2)
            with ExitStack() as p1a:
                W1 = sb("W1", [128, 8, 1024], BF16, p1a)
                gb = sb("gb1", [128, 1024], F32, p1a)
                xt = [sb("xta%d" % i, [128, 1024], F32, p1a) for i in range(2)]
                junk = sb("junka", [128, 1024], BF16, p1a)
                hb = [sb("hba%d" % i, [128, 1024], BF16, p1a) for i in range(2)]
                hT = [sb("hTa%d" % i, [128, 8, 128], BF16, p1a) for i in range(2)]
                ss = sb("ssa", [128, 8], F32, p1a)
                stg1 = [sb("stg1a%d" % i, [128, 4096], F32, p1a) for i in range(2)]
                for i_, (d0_, s0_) in enumerate(((0, 512), (512, 1024))):
                    fv_ = stg1[i_][:].rearrange("p (c e) -> p c e", c=8)
                    S.dma("sp", "w1s%d" % i_, fv_, w_in_v[:, :, s0_:s0_ + 512], writes=[("stg1", i_)])
                    S.op("dve" if i_ == 0 else "pool", lambda E: E.tensor_copy(out=W1[:, :, d0_:d0_ + 512], in_=fv_),
                         reads=[("stg1", i_)], writes=["W1"])
                S.dma("sp", "gb1", gb[:], attn_norm.partition_broadcast(128), writes=["gb"])
                def na(blk):
                    norm_T(p1a, x_all[blk * 128:(blk + 1) * 128, :], blk % 2, xt, junk, hb, hT, ss, gb,
                           "act" if blk % 2 == 0 else "dve")
                na(0)
                for blk in range(nblk1a):
                    k = blk % 2
                    if blk + 1 < nblk1a:
                        na(blk + 1)
                    pk, pkn = PF[2 * k], "PF%d" % (2 * k)
                    pv, pvn = PF[2 * k + 1], "PF%d" % (2 * k + 1)
                    for hp in range(4):
                        for c in range(8):
                            S.op("pe", lambda E: E.matmul(pk[:, hp * 128:(hp + 1) * 128], lhsT=W1[:, c, hp * 128:(hp + 1) * 128],
                                                          rhs=hT[k][:, c, :], start=(c == 0), stop=(c == 7)),
                                 reads=["W1", ("hT", k)], writes=[pkn])
                    for c in range(8):
                        S.op("pe", lambda E: E.matmul(pv[:], lhsT=hT[k][:, c, :], rhs=W1[:, c, 512:1024],
                                                      start=(c == 0), stop=(c == 7)),
                             reads=["W1", ("hT", k)], writes=[pvn])
                    S.op("dve", lambda E: E.tensor_copy(out=KT[:, :, blk * 128:(blk + 1) * 128],
                                                        in_=pk[:].rearrange("p (a b) -> p a b", a=4)),
                         reads=[pkn], writes=[("KT", blk)])
                    S.op("act", lambda E: E.copy(out=V[:, blk, :], in_=pv[:]), reads=[pvn], writes=[("V", blk)])
                S.barrier()

            if upto == 1:
                S.finish()
                return nc

            with ExitStack() as p2b:
                Qz = [sb("Qz%d" % i, [128, 8, 512], BF16, p2b) for i in range(2)]
                ebuf = [sb("ebuf%d" % i, [128, 512], F32, p2b) for i in range(2)]
                spb = [sb("spb%d" % i, [128, 512], BF16, p2b) for i in range(2)]
                wb = [sb("wb%d" % i, [128, 512], BF16, p2b) for i in range(2)]
                Ob = [sb("Ob0", [128, 4, 512], F32, p2b)] * 2
                Call = sb("Call", [128, 8], F32, p2b)
                Cst = [Call[:, 0:4], Call[:, 4:8]]
                call = sb("call", [128, 8], F32, p2b)
                cst = [call[:, 0:4], call[:, 4:8]]
                sbm = sb("sbm", [128, 4, 128], BF16, p2b)
                swm = sb("swm", [128, 2, 256], F32, p2b)
                negones = sb("negones", [128, 2], BF16, p2b)
                gsb = sb("gsb", [128, 512], F32, p2b)
                gsw = sb("gsw", [128, 512], F32, p2b)
                sinkb = sb("sinkb", [128, 8], F32, p2b)
                Qswz = [sb("Qswz%d" % i, [128, 8, 128], BF16, p2b) for i in range(2)]
                kswt = [sb("kswt%d" % i, [128, 256], BF16, p2b) for i in range(2)]
                vswt = [sb("vswt%d" % i, [128, 2, 128], BF16, p2b) for i in range(2)]
                zm = sb("zm", [128, 4, 256], F32, p2b)
                pexp = sb("pexp", [128, 4, 256], BF16, p2b)
                pT = sb("pT", [128, 8, 128], BF16, p2b)
                sst = sb("sst", [128, 40], F32, p2b)
                Osw = sb("Osw", [128, 4, 512], F32, p2b)
                mixb = [sb("mixb0", [128, 4, 1024], BF16, p2b)] * 2
                junk2 = sb("junk2", [128, 512], BF16, p2b)
                rst = sb("rst", [128, 32], F32, p2b)
                tmpo = [sb("tmpo%d" % i, [128, 4, 64], F32, p2b) for i in range(2)]

                S.dma("pool", "sbm", sbm[:], c_sbm.rearrange("m p f -> p m f"), writes=["sbm"])
                S.dma("sp", "swm", swm[:], c_swm.rearrange("m p f -> p m f"), writes=["swm"])
                S.dma("sp", "gsb", gsb[:], sb_out_norm.partition_broadcast(128), writes=["gsb"])
                S.dma("sp", "gsw", gsw[:], swa_out_norm.partition_broadcast(128), writes=["gsw"])
                S.dma("sp", "sinkb", sinkb[:], swa_sinks.partition_broadcast(128), writes=["sinkb"])
                S.op("pool", lambda E: E.memset(negones[:], -1.0), writes=["negones"])
                for i in range(2):
                    S.op("pool", lambda E: E.memset(Qz[i][:], 0.0), writes=[("Qz", i)])
                    S.op("pool", lambda E: E.memset(Qswz[i][:], 0.0), writes=[("Qswz", i)])

                Zb = [PF[0], PF[1], PF[2], PF[3]]
                Zn = ["PF0", "PF1", "PF2", "PF3"]
                PVb = [PF[4], PF[5], PF[6]]
                PVn = ["PF4", "PF5", "PF6"]

                def load_q(t):
                    tb = t % 2
                    for jj in range(4):
                        j = 4 * t + jj
                        for r in range(2):
                            dst = Qz[tb][r * 64:(r + 1) * 64, :, jj * 128:(jj + 1) * 128] \
                                .rearrange("p (hp two) f -> p hp two f", two=2)[:, :, r, :]
                            S.dma("sp", "ldq%d" % tb, dst, qsb_d[j, r * 64:(r + 1) * 64, :, :],
                                  reads=[("qsb_d", j)], writes=[("Qz", tb)])

                def sb_tiles(t):
                    L = []
                    for hp in range(4):
                        kbs = list(range(8 * t + 7, -1, -1))
                        for n, kb in enumerate(kbs):
                            for r in range(2):
                                h = 2 * hp + r
                                if kb >= 8 * t:
                                    d = kb - 8 * t
                                    j0 = d // 2
                                    m = (0 if d % 2 == 1 else 1) + (0 if j0 % 2 == 0 else 2)
                                else:
                                    j0, m = 0, None
                                L.append(dict(h=h, kb=kb, j0=j0, m=m, first=(n == 0), last=(n == len(kbs) - 1),
                                              s=r))
                    return L

                def S1(i, T, tb):
                    z, zn = Zb[i % 4], Zn[i % 4]
                    c0 = T["j0"] * 128
                    hp = T["h"] // 2
                    S.op("pe", lambda E: E.matmul(z[:, c0:512], lhsT=KT[:, hp, T["kb"] * 128:(T["kb"] + 1) * 128],
                                                  rhs=Qz[tb][:, T["h"], c0:512], start=True, stop=(T["m"] is None)),
                         reads=[("KT", T["kb"]), ("Qz", tb)], writes=[zn])
                    if T["m"] is not None:
                        S.op("pe", lambda E: E.matmul(z[:, c0:c0 + 128], lhsT=identb[:], rhs=sbm[:, T["m"], :],
                                                      start=False, stop=True),
                             reads=["identb", "sbm"], writes=[zn])

                def A1(i, T):
                    z, zn = Zb[i % 4], Zn[i % 4]
                    c0 = T["j0"] * 128
                    S.op("act", lambda E: E.activation(out=ebuf[i % 2][:, c0:512], in_=z[:, c0:512], func=AF.Exp),
                         reads=[zn], writes=[("ebuf", i % 2)])

                def A2(i, T):
                    c0 = T["j0"] * 128
                    S.op("act", lambda E: E.activation(out=spb[i % 2][:, c0:512], in_=ebuf[i % 2][:, c0:512],
                                                       func=AF.Ln, bias=1.0, scale=1.0),
                         reads=[("ebuf", i % 2)], writes=[("spb", i % 2)])

                def S3(i, T):
                    z, zn = Zb[i % 4], Zn[i % 4]
                    c0 = T["j0"] * 128
                    S.op("pe", lambda E: E.matmul(z[:, c0:512], lhsT=unegb[:], rhs=spb[i % 2][:, c0:512],
                                                  start=False, stop=True, skip_group_check=True),
                         reads=["unegb", ("spb", i % 2)], writes=[zn])
                    for jj in range(T["j0"], 4):
                        S.op("pe", lambda E: E.matmul(PVb[i % 3][:, 256 + jj:257 + jj],
                                                      lhsT=spb[i % 2][:, jj * 128:(jj + 1) * 128],
                                                      rhs=negones[:, 0:1], start=True, stop=True),
                             reads=[("spb", i % 2), "negones"], writes=[PVn[i % 3]])

                def A3(i, T):
                    z, zn = Zb[i % 4], Zn[i % 4]
                    c0 = T["j0"] * 128
                    S.op("act", lambda E: E.activation(out=wb[i % 2][:, c0:512], in_=z[:, c0:512], func=AF.Exp),
                         reads=[zn], writes=[("wb", i % 2)])

                def S5(i, T):
                    h = T["h"]
                    for jj in range(T["j0"], 4):
                        S.op("pe", lambda E: E.matmul(PVb[i % 3][:, jj * 64:(jj + 1) * 64],
                                                      lhsT=wb[i % 2][:, jj * 128:(jj + 1) * 128],
                                                      rhs=V[:, T["kb"], h * 64:(h + 1) * 64], start=True, stop=True),
                             reads=[("wb", i % 2), ("V", T["kb"])], writes=[PVn[i % 3]])

                def S6(i, T, tb):
                    s, h = T["s"], T["h"]
                    O = Ob[tb]
                    if T["first"]:
                        S.op("dve", lambda E: E.memset(Cst[s], 0.0), writes=[("Cst", s)])
                        S.op("dve", lambda E: E.memset(cst[s], 1.0), writes=[("cst", s)])
                        S.op("pool", lambda E: E.memset(O[:, :, h * 64:(h + 1) * 64], 0.0), writes=[("Ob", 0, h)])
                    j0 = T["j0"]
                    nj = 4 - j0
                    tm = tmpo[i % 2]
                    S.op("dve", lambda E: E.tensor_tensor(
                        out=tm[:, j0:4, :], in0=PVb[i % 3][:, j0 * 64:256].rearrange("p (a b) -> p a b", a=nj),
                        in1=cst[s][:, j0:4].unsqueeze(2).to_broadcast([128, nj, 64]), op=ALU.mult),
                        reads=[PVn[i % 3], ("cst", s)], writes=[("tmpo", i % 2)])
                    S.op("pool", lambda E: E.tensor_tensor(
                        out=O[:, j0:4, h * 64:(h + 1) * 64], in0=O[:, j0:4, h * 64:(h + 1) * 64], in1=tm[:, j0:4, :],
                        op=ALU.add),
                        reads=[("tmpo", i % 2), ("Ob", 0, h)], writes=[("Ob", 0, h)])
                    if not T["last"]:
                        j0 = T["j0"]
                        S.op("dve", lambda E: E.tensor_tensor(out=Cst[s][:, j0:4], in0=PVb[i % 3][:, 256 + j0:260],
                                                              in1=Cst[s][:, j0:4], op=ALU.add),
                             reads=[PVn[i % 3], ("Cst", s)], writes=[("Cst", s)])
                        pend_exp.append(s)

                pend_exp = []

                pend_age = [0]

                def flush_exp():
                    if not pend_exp:
                        pend_age[0] = 0
                        return
                    if len(pend_exp) >= 2:
                        assert sorted(pend_exp[:2]) == [0, 1], pend_exp
                        del pend_exp[:2]
                        S.op("act", lambda E: E.activation(out=call[:], in_=Call[:], func=AF.Exp),
                             reads=[("Cst", 0), ("Cst", 1)], writes=[("cst", 0), ("cst", 1)])
                        pend_age[0] = 0
                        return
                    pend_age[0] += 1
                    if pend_age[0] >= 2:
                        s = pend_exp.pop(0)
                        S.op("act", lambda E: E.activation(out=cst[s], in_=Cst[s], func=AF.Exp),
                             reads=[("Cst", s)], writes=[("cst", s)])
                        pend_age[0] = 0

                def sb_attention(t):
                    tb = t % 2
                    L = sb_tiles(t)
                    n = len(L)
                    S1(0, L[0], tb)
                    S1(1, L[1], tb)
                    A1(0, L[0])
                    for it in range(n + 2):
                        if it + 2 < n:
                            S1(it + 2, L[it + 2], tb)
                        if it < n:
                            A2(it, L[it])
                            S3(it, L[it])
                        if it + 1 < n:
                            A1(it + 1, L[it + 1])
                        flush_exp()
                        if 1 <= it <= n:
                            A3(it - 1, L[it - 1])
                        if 2 <= it:
                            S5(it - 2, L[it - 2])
                            S6(it - 2, L[it - 2], tb)

                def swa_block(t, jj):
                    j = 4 * t + jj
                    jb = j % 2
                    S.dma("sp", "ldk%d" % jb, kswt[jb][:], ksw_d[j], reads=[("ksw_d", j)], writes=[("kswt", jb)])
                    S.dma("sp", "ldv%d" % jb, vswt[jb][:], vsw_d[j], reads=[("vsw_d", j)], writes=[("vswt", jb)])
                    srcv = qsw_d[j].rearrange("(r d) p t -> d p r t", r=2)
                    for kv in range(2):
                        dst = Qswz[jb][kv * 64:(kv + 1) * 64, kv * 4:(kv + 1) * 4, :] \
                            .rearrange("d (p r) t -> d p r t", r=2)
                        for r in range(2):
                            S.dma("sp", "ldqs%d" % jb, dst[:, :, r, :], srcv[:, 2 * kv:2 * kv + 2, r, :],
                                  reads=[("qsw_d", j)], writes=[("Qswz", jb)])
                    mk = swm[:, 0:1, :] if j == 0 else swm[:, 1:2, :]
                    for kv in range(2):
                        for g4 in range(4):
                            g = kv * 4 + g4
                            zb, zbn = (PF[0], "PF0") if g4 < 2 else (PF[1], "PF1")
                            S.op("pe", lambda E: E.matmul(zb[:, (g4 % 2) * 256:(g4 % 2 + 1) * 256], lhsT=Qswz[jb][:, g, :],
                                                          rhs=kswt[jb][:], start=True, stop=True),
                                 reads=[("Qswz", jb), ("kswt", jb)], writes=[zbn])
                        for hb2 in range(2):
                            zb, zbn = (PF[0], "PF0") if hb2 == 0 else (PF[1], "PF1")
                            S.op("dve", lambda E: E.tensor_tensor(out=zm[:, 2 * hb2:2 * hb2 + 2, :],
                                                                  in0=zb[:].rearrange("p (a b) -> p a b", a=2),
                                                                  in1=mk.to_broadcast([128, 2, 256]), op=ALU.add),
                                 reads=[zbn, "swm"], writes=["zm"])
                        o = kv * 4
                        S.op("dve", lambda E: E.tensor_reduce(out=sst[:, o:o + 4], in_=zm[:], axis=AX.X, op=ALU.max),
                             reads=["zm"], writes=["sst_m"])
                        S.op("dve", lambda E: E.tensor_tensor(out=sst[:, o:o + 4], in0=sst[:, o:o + 4],
                                                              in1=sinkb[:, o:o + 4], op=ALU.max),
                             reads=["sst_m", "sinkb"], writes=["sst_m"])
                        S.op("dve", lambda E: E.tensor_scalar(out=sst[:, 8 + o:12 + o], in0=sst[:, o:o + 4], scalar1=-1.0,
                                                              scalar2=None, op0=ALU.mult),
                             reads=["sst_m"], writes=["sst_nm"])
                        for g4 in range(4):
                            S.op("act", lambda E: E.activation(out=pexp[:, g4, :], in_=zm[:, g4, :], func=AF.Exp,
                                                               bias=sst[:, 8 + o + g4:9 + o + g4], scale=1.0,
                                                               accum_out=sst[:, 16 + o + g4:17 + o + g4]),
                                 reads=["zm", "sst_nm"], writes=["pexp", "sst_rs"])
                        S.op("dve", lambda E: E.tensor_tensor(out=sst[:, 24 + o:28 + o], in0=sinkb[:, o:o + 4],
                                                              in1=sst[:, o:o + 4], op=ALU.subtract),
                             reads=["sst_m", "sinkb"], writes=["sst_d"])
                        S.op("act", lambda E: E.activation(out=sst[:, 24 + o:28 + o], in_=sst[:, 24 + o:28 + o], func=AF.Exp),
                             reads=["sst_d"], writes=["sst_es"])
                        S.op("dve", lambda E: E.tensor_tensor(out=sst[:, 32 + o:36 + o], in0=sst[:, 24 + o:28 + o],
                                                              in1=sst[:, 16 + o:20 + o], op=ALU.add),
                             reads=["sst_es", "sst_rs"], writes=["sst_den"])
                        S.op("dve", lambda E: E.reciprocal(out=sst[:, 32 + o:36 + o], in_=sst[:, 32 + o:36 + o]),
                             reads=["sst_den"], writes=["sst_rden"])
                        for g4 in range(4):
                            for hf in range(2):
                                S.op("pe", lambda E: E.transpose(out=PB[:, g4 * 2 + hf, :],
                                                                 in_=pexp[:, g4, hf * 128:(hf + 1) * 128],
                                                                 identity=identb[:]),
                                     reads=["pexp", "identb"], writes=["PB"])
                        S.op("act", lambda E: E.copy(out=pT[:], in_=PB[:]), reads=["PB"], writes=["pT"])
                        for g4 in range(4):
                            g = kv * 4 + g4
                            for hf in range(2):
                                S.op("pe", lambda E: E.matmul(PF[2][:, g * 64:(g + 1) * 64], lhsT=pT[:, g4 * 2 + hf, :],
                                                              rhs=vswt[jb][:, hf, kv * 64:(kv + 1) * 64],
                                                              start=(hf == 0), stop=(hf == 1)),
                                     reads=["pT", ("vswt", jb)], writes=["PF2"])
                        S.op("dve", lambda E: E.tensor_tensor(
                            out=Osw[:, jj, kv * 256:(kv + 1) * 256].rearrange("p (a b) -> p a b", a=4),
                            in0=PF[2][:, kv * 256:(kv + 1) * 256].rearrange("p (a b) -> p a b", a=4),
                            in1=sst[:, 32 + o:36 + o].unsqueeze(2).to_broadcast([128, 4, 64]), op=ALU.mult),
                            reads=["PF2", "sst_rden"], writes=[("Osw", jj)])

                def finish_tile(t):
                    tb = t % 2
                    O = Ob[tb]
                    for jj in range(4):
                        S.op("act", lambda E: E.activation(out=junk2[:], in_=O[:, jj, :], func=AF.Square,
                                                           accum_out=rst[:, jj:jj + 1]),
                             reads=[("Ob", 0, h) for h in range(8)], writes=["junk2", "rst_ss"])
                        S.op("act", lambda E: E.activation(out=junk2[:], in_=Osw[:, jj, :], func=AF.Square,
                                                           accum_out=rst[:, 4 + jj:5 + jj]),
                             reads=[("Osw", jj)], writes=["junk2", "rst_ss"])
                    S.op("dve", lambda E: E.tensor_scalar(out=rst[:, 8:16], in0=rst[:, 0:8], scalar1=1.0 / 512,
                                                          scalar2=EPS, op0=ALU.mult, op1=ALU.add),
                         reads=["rst_ss"], writes=["rst_ms"])
                    S.op("act", lambda E: E.activation(out=rst[:, 16:24], in_=rst[:, 8:16], func=AF.Sqrt),
                         reads=["rst_ms"], writes=["rst_sd"])
                    S.op("dve", lambda E: E.reciprocal(out=rst[:, 24:32], in_=rst[:, 16:24]),
                         reads=["rst_sd"], writes=["rst_r"])
                    for jj in range(4):
                        S.op("dve", lambda E: E.scalar_tensor_tensor(out=mixb[tb][:, jj, 0:512], in0=O[:, jj, :],
                                                                     scalar=rst[:, 24 + jj:25 + jj], in1=gsb[:],
                                                                     op0=ALU.mult, op1=ALU.mult),
                             reads=[("Ob", 0, h) for h in range(8)] + ["rst_r", "gsb"], writes=[("mixb", 0)])
                        S.op("dve", lambda E: E.scalar_tensor_tensor(out=mixb[tb][:, jj, 512:1024], in0=Osw[:, jj, :],
                                                                     scalar=rst[:, 28 + jj:29 + jj], in1=gsw[:],
                                                                     op0=ALU.mult, op1=ALU.mult),
                             reads=[("Osw", jj), "rst_r", "gsw"], writes=[("mixb", 0)])
                    S.dma("sp", "omix", mix_d[4 * t:4 * t + 4].rearrange("j p f -> p j f"), mixb[tb][:],
                          reads=[("mixb", 0)], writes=[("mix_d", t)])

                load_q(0)
                for t in range(nqt):
                    if t + 1 < nqt:
                        load_q(t + 1)
                    sb_attention(t)
                    for jj in range(4):
                        swa_block(t, jj)
                    finish_tile(t)
                S.barrier()

        if upto == 2:
            with ExitStack() as pd:
                mb = sb("dbg_mb", [128, 1024], BF16, pd)
                mf = sb("dbg_mf", [128, 1024], F32, pd)
                for j in range(4 * nqt):
                    S.dma("sp", "dbg_in", mb[:], mix_d[j], writes=["mb"])
                    S.op("dve", lambda E: E.tensor_copy(out=mf[:], in_=mb[:]), reads=["mb"], writes=["mf"])
                    S.dma("sp", "dbg_out", dbg[j], mf[:], reads=["mf"], writes=["dbgo"])
            S.finish()
            return nc


        ntile = 2 * nqt
        with ExitStack() as p3:
            Wout = sb("Wout", [128, 8, 1024], BF16, p3)
            Wq = sb("Wq", [128, 8, 2048], BF16, p3)
            skT = [sb("skT%d" % i, [128, 8, 128], BF16, p3) for i in range(2)]
            gffn = sb("gffn", [128, 1024], F32, p3)
            gfin = sb("gfin", [128, 1024], F32, p3)
            identf = sb("identf", [128, 128], F32, p3)
            iotaf = sb("iotaf", [128, 16], F32, p3)
            iotab = sb("iotab", [128, 128], BF16, p3)
            mixt = sb("mixt", [128, 2, 1024], BF16, p3)
            GT = sb("GT", [128, 128, 256], BF16, p3)
            xo = sb("xo", [128, 2, 1024], F32, p3)
            x1b = [sb("x1b%d" % i, [128, 2, 1024], F32, p3) for i in range(2)]
            xnTb = [sb("xnTb%d" % i, [128, 8, 256], BF16, p3) for i in range(2)]
            st3 = sb("st3", [128, 16], F32, p3)
            sc = sb("sc", [128, 16, 128], F32, p3)
            scw = sb("scw", [128, 16, 128], F32, p3)
            vv = sb("vv", [128, 16, 16], F32, p3)
            ix = sb("ix", [128, 16, 16], U32, p3)
            tops = sb("tops", [128, 8, 16], F32, p3)
            posu = sb("posu", [128, 8, 16], U32, p3)
            posf3 = sb("posf3", [128, 8, 16], F32, p3)
            thr16 = sb("thr16", [128, 16], F32, p3)
            paf = sb("paf", [128, 8, 16], F32, p3)
            pbf = sb("pbf", [128, 8, 16], F32, p3)
            i1f = sb("i1f", [128, 8, 16], F32, p3)
            i2f = sb("i2f", [128, 8, 16], F32, p3)
            sel = sb("sel", [128, 3, 128], F32, p3)
            gsm = sb("gsm", [128, 32], F32, p3)
            hkT = sb("hkT", [128, 3, 256], F32, p3)
            At = [sb("At%d" % i, [128, 128], BF16, p3) for i in range(3)]
            Bt = [sb("Bt%d" % i, [128, 128], BF16, p3) for i in range(3)]
            dsb = [sb("dsb%d" % i, [128, 8, 128], BF16, p3) for i in range(3)]
            usb = [sb("usb%d" % i, [128, 1024], BF16, p3) for i in range(4)]
            gl = [sb("gl%d" % i, [128, 256], F32, p3) for i in range(3)]
            wg = [sb("wg%d" % i, [128, 256], BF16, p3) for i in range(3)]
            xn = mixt
            qTv = scw[:, 0:8, :].bitcast(BF16).rearrange("p a (b c) -> p (a b) c", c=128)
            cand = sc[:].rearrange("p (h x) k -> p h (x k)", x=2)
            candw = scw[:].rearrange("p (h x) k -> p h (x k)", x=2)
            oh = scw[:].rearrange("p (h x) (y a) -> p h (x y) a", x=2, a=16)

            w_out_v = w_out.rearrange("(c p) n -> p c n", p=128)
            wq_v = peer_wq.rearrange("(c p) n -> p c n", p=128)
            gst = [GT[:, 32 * i:32 * i + 32, :].bitcast(F32).rearrange("p a b -> p (a b)").rearrange("p (c e) -> p c e", c=8)
                   for i in range(4)]
            wl = [(Wout, w_out_v, q4, "Wout") for q4 in range(2)] + [(Wq, wq_v, q4, "Wq") for q4 in range(4)]
            for n_, (wt_, wv_, q4, key_) in enumerate(wl):
                k_ = n_ % 4
                S.dma("sp", "w3s%d" % k_, gst[k_], wv_[:, :, q4 * 512:(q4 + 1) * 512], writes=[("gst", k_)])
                S.op(("dve", "pool", "act")[n_ % 3],
                     (lambda E: E.copy(out=wt_[:, :, q4 * 512:(q4 + 1) * 512], in_=gst[k_])) if n_ % 3 == 2 else
                     (lambda E: E.tensor_copy(out=wt_[:, :, q4 * 512:(q4 + 1) * 512], in_=gst[k_])),
                     reads=[("gst", k_)], writes=[key_])
            S.dma("pool", "sk1", skT[0][:], sk1T.rearrange("h d k -> d h k"), writes=["skT"])
            S.dma("pool", "sk2", skT[1][:], sk2T.rearrange("h d k -> d h k"), writes=["skT"])
            S.dma("sp", "gffn", gffn[:], ffn_norm.partition_broadcast(128), writes=["gffn"])
            S.dma("sp", "gfin", gfin[:], final_norm.partition_broadcast(128), writes=["gfin"])
            S.dma("sp", "identf", identf[:], c_ident, writes=["identf"])
            S.dma("sp", "iotaf", iotaf[:], c_iota[:, 0:16], writes=["iotaf"])
            S.dma("pool", "iotab", iotab[:], c_iota, writes=["iotab"])
            S.op("dve", lambda E: E.tensor_scalar(out=thr16[:], in0=iotaf[:, 0:16], scalar1=16.0, scalar2=16.0, op0=ALU.mult, op1=ALU.add), reads=["iotaf"], writes=["thr16"])

            def rms_rows(src, blk, g_t, dst, col, sk, gn, dk):
                S.op("act", lambda E: E.activation(out=xn[:, blk, :], in_=src[:, blk, :], func=AF.Square,
                                                   accum_out=st3[:, col:col + 1]),
                     reads=[sk], writes=[("mixt", blk), ("st3", col)])
                S.op("dve", lambda E: E.tensor_scalar(out=st3[:, col + 1:col + 2], in0=st3[:, col:col + 1],
                                                      scalar1=1.0 / 1024, scalar2=EPS, op0=ALU.mult, op1=ALU.add),
                     reads=[("st3", col)], writes=[("st3", col + 1)])
                S.op("act", lambda E: E.activation(out=st3[:, col + 2:col + 3], in_=st3[:, col + 1:col + 2], func=AF.Sqrt),
                     reads=[("st3", col + 1)], writes=[("st3", col + 2)])
                S.op("dve", lambda E: E.reciprocal(out=st3[:, col + 3:col + 4], in_=st3[:, col + 2:col + 3]),
                     reads=[("st3", col + 2)], writes=[("st3", col + 3)])
                S.op("dve", lambda E: E.scalar_tensor_tensor(out=dst[:, blk, :], in0=src[:, blk, :],
                                                             scalar=st3[:, col + 3:col + 4], in1=g_t[:],
                                                             op0=ALU.mult, op1=ALU.mult),
                     reads=[sk, ("st3", col + 3), gn], writes=[dk])

            def top16(src_ap, work_ap, vals_ap, idx_ap, rk, wk_, vk, ik):
                S.op("dve", lambda E: E.max(out=vals_ap[:, 0:8], in_=src_ap), reads=[rk], writes=[vk])
                S.op("dve", lambda E: E.max_index(out=idx_ap[:, 0:8], in_max=vals_ap[:, 0:8], in_values=src_ap),
                     reads=[rk, vk], writes=[ik])
                S.op("dve", lambda E: E.match_replace(out=work_ap, in_to_replace=vals_ap[:, 0:8], in_values=src_ap,
                                                      imm_value=-1e30), reads=[rk, vk], writes=[wk_])
                S.op("dve", lambda E: E.max(out=vals_ap[:, 8:16], in_=work_ap), reads=[wk_], writes=[vk])
                S.op("dve", lambda E: E.max_index(out=idx_ap[:, 8:16], in_max=vals_ap[:, 8:16], in_values=work_ap),
                     reads=[wk_, vk], writes=[ik])

            def pre_gen(u):
                ub = u % 2
                x1 = x1b[ub]
                xnT = xnTb[ub]
                S.dma("sp", "ldmix", mixt[:], mix_d[2 * u:2 * u + 2].rearrange("j p f -> p j f"),
                      writes=[("mixt", 0), ("mixt", 1)])
                S.dma("sp", "ldxo", xo[:], x_po[2 * u:2 * u + 2, 128:256, :].rearrange("j p f -> p j f"),
                      writes=[("xo", 0), ("xo", 1)])
                yield
                for blk in range(2):
                    for c in range(8):
                        S.op("pe", lambda E: E.transpose(out=PB[:, c, :], in_=mixt[:, blk, c * 128:(c + 1) * 128],
                                                         identity=identb[:]),
                             reads=[("mixt", blk), "identb"], writes=["PB"])
                    S.op("act", lambda E: E.copy(out=xnT[:, :, blk * 128:(blk + 1) * 128], in_=PB[:]),
                         reads=["PB"], writes=[("xnT", ub, blk)])
                    yield
                    for half in range(2):
                        for c in range(8):
                            S.op("pe", lambda E: E.matmul(PF[6][:], lhsT=xnT[:, c, blk * 128:(blk + 1) * 128],
                                                          rhs=Wout[:, c, half * 512:(half + 1) * 512],
                                                          start=(c == 0), stop=(c == 7)),
                                 reads=[("xnT", ub, blk), "Wout"], writes=["PF6"])
                        S.op("dve", lambda E: E.tensor_tensor(out=x1[:, blk, half * 512:(half + 1) * 512], in0=PF[6][:],
                                                              in1=xo[:, blk, half * 512:(half + 1) * 512], op=ALU.add),
                             reads=["PF6", ("xo", blk)], writes=[("x1", ub, blk)])
                        yield
                    rms_rows(x1, blk, gffn, xn, 4 * blk, ("x1", ub, blk), "gffn", ("mixt", blk))
                    yield
                    for c in range(8):
                        S.op("pe", lambda E: E.transpose(out=PB[:, c, :], in_=xn[:, blk, c * 128:(c + 1) * 128],
                                                         identity=identb[:]),
                             reads=[("mixt", blk), "identb"], writes=["PB"])
                    S.op("dve", lambda E: E.tensor_copy(out=xnT[:, :, blk * 128:(blk + 1) * 128], in_=PB[:]),
                         reads=["PB"], writes=[("xnT", ub, blk)])
                    yield
                for blk in range(2):
                    for r4 in range(4):
                        for q in range(4):
                            ch = 4 * r4 + q
                            for c in range(8):
                                S.op("pe", lambda E: E.matmul(PF[6][:, q * 128:(q + 1) * 128],
                                                              lhsT=Wq[:, c, ch * 128:(ch + 1) * 128],
                                                              rhs=xnT[:, c, blk * 128:(blk + 1) * 128],
                                                              start=(c == 0), stop=(c == 7)),
                                     reads=["Wq", ("xnT", ub, blk)], writes=["PF6"])
                        S.op("act", lambda E: E.copy(out=qTv[:, 4 * r4:4 * r4 + 4, :],
                                                     in_=PF[6][:].rearrange("p (a b) -> p a b", a=4)),
                             reads=["PF6"], writes=["scw"])
                        yield
                    for r4 in range(4):
                        for q in range(4):
                            ch = 4 * r4 + q
                            S.op("pe", lambda E: E.matmul(PF[6][:, q * 128:(q + 1) * 128], lhsT=qTv[:, ch, :],
                                                          rhs=skT[ch % 2][:, ch // 2, :], start=True, stop=True),
                                 reads=["scw", "skT"], writes=["PF6"])
                        if r4 % 2 == 0:
                            S.op("act", lambda E: E.copy(out=sc[:, 4 * r4:4 * r4 + 4, :], in_=PF[6][:].rearrange("p (a b) -> p a b", a=4)),
                                 reads=["PF6"], writes=["sc"])
                        else:
                            S.op("dve", lambda E: E.tensor_copy(out=sc[:, 4 * r4:4 * r4 + 4, :], in_=PF[6][:].rearrange("p (a b) -> p a b", a=4)),
                                 reads=["PF6"], writes=["sc"])
                        yield
                    for ch in range(16):
                        top16(sc[:, ch, :], scw[:, ch, :], vv[:, ch, :], ix[:, ch, :], "sc", "scw", "vv", "ix")
                        if ch % 2 == 1:
                            yield
                    vv4 = vv[:].rearrange("p (h s) k -> p h s k", s=2)
                    S.op("dve", lambda E: E.tensor_tensor(
                        out=cand.rearrange("p h (a b) -> p h a b", a=16),
                        in0=vv4[:, :, 0, :].unsqueeze(3).to_broadcast([128, 8, 16, 16]),
                        in1=vv4[:, :, 1, :].unsqueeze(2).to_broadcast([128, 8, 16, 16]), op=ALU.add),
                        reads=["vv"], writes=["sc"])
                    for h in range(8):
                        top16(cand[:, h, :], candw[:, h, :], tops[:, h, :], posu[:, h, :], "sc", "scw", "tops", "posu")
                        if h % 2 == 1:
                            yield
                    S.op("dve", lambda E: E.tensor_tensor(out=sel[:, 2, :].rearrange("p (h k) -> p h k", h=8), in0=tops[:],
                                                          in1=tops[:, :, 0:1].to_broadcast([128, 8, 16]), op=ALU.subtract),
                         reads=["tops"], writes=["gate"])
                    S.op("act", lambda E: E.activation(out=sel[:, 2, :], in_=sel[:, 2, :], func=AF.Exp),
                         reads=["gate"], writes=["gate"])
                    S.op("dve", lambda E: E.tensor_reduce(out=gsm[:, 0:8], in_=sel[:, 2, :].rearrange("p (h k) -> p h k", h=8),
                                                          axis=AX.X, op=ALU.add), reads=["gate"], writes=["gsm"])
                    S.op("dve", lambda E: E.reciprocal(out=gsm[:, 8:16], in_=gsm[:, 0:8]), reads=["gsm"], writes=["gsr"])
                    S.op("dve", lambda E: E.tensor_tensor(out=sel[:, 2, :].rearrange("p (h k) -> p h k", h=8),
                                                          in0=sel[:, 2, :].rearrange("p (h k) -> p h k", h=8),
                                                          in1=gsm[:, 8:16].unsqueeze(2).to_broadcast([128, 8, 16]), op=ALU.mult),
                         reads=["gate", "gsr"], writes=["gate"])
                    yield
                    S.op("dve", lambda E: E.tensor_copy(out=posf3[:], in_=posu[:]), reads=["posu"], writes=["posf3"])
                    for h2 in range(2):
                        hs = slice(4 * h2, 4 * h2 + 4)
                        S.op("dve", lambda E: E.tensor_tensor(
                            out=oh[:, hs].rearrange("p h k a -> p (h k) a"),
                            in0=posf3[:, hs, :].rearrange("p h k -> p (h k)").unsqueeze(2).to_broadcast([128, 64, 16]),
                            in1=thr16[:].unsqueeze(1).to_broadcast([128, 64, 16]), op=ALU.is_ge),
                            reads=["posf3", "thr16"], writes=["scw"])
                        S.op("dve", lambda E: E.tensor_reduce(
                            out=paf[:, hs, :].rearrange("p h k -> p (h k)"),
                            in_=oh[:, hs].rearrange("p h k a -> p (h k) a"), axis=AX.X, op=ALU.add),
                            reads=["scw"], writes=["paf"])
                    S.op("dve", lambda E: E.scalar_tensor_tensor(out=pbf[:], in0=paf[:], scalar=-16.0, in1=posf3[:],
                                                                 op0=ALU.mult, op1=ALU.add),
                         reads=["paf", "posf3"], writes=["pbf"])
                    yield
                    ix4 = ix[:].rearrange("p (h s) k -> p h s k", s=2)
                    S.op("dve", lambda E: E.tensor_copy(out=i1f[:], in_=ix4[:, :, 0, :]), reads=["ix"], writes=["i1f"])
                    S.op("dve", lambda E: E.tensor_copy(out=i2f[:], in_=ix4[:, :, 1, :]), reads=["ix"], writes=["i2f"])
                    for side, (pf_, if_) in enumerate(((paf, i1f), (pbf, i2f))):
                        for h2 in range(2):
                            hs = slice(4 * h2, 4 * h2 + 4)
                            S.op("dve", lambda E: E.tensor_tensor(
                                out=oh[:, hs].rearrange("p h k a -> p (h k) a"),
                                in0=pf_[:, hs, :].rearrange("p h k -> p (h k)").unsqueeze(2).to_broadcast([128, 64, 16]),
                                in1=iotaf[:, 0:16].unsqueeze(1).to_broadcast([128, 64, 16]), op=ALU.is_equal),
                                reads=["paf", "pbf", "iotaf"], writes=["scw"])
                            for h in range(4 * h2, 4 * h2 + 4):
                                S.op("dve", lambda E: E.tensor_tensor(
                                    out=oh[:, h], in0=oh[:, h],
                                    in1=if_[:, h, :].unsqueeze(1).to_broadcast([128, 16, 16]), op=ALU.mult),
                                    reads=["scw", "i1f", "i2f"], writes=["scw"])
                            S.op("dve", lambda E: E.tensor_reduce(
                                out=sel[:, side, 64 * h2:64 * h2 + 64],
                                in_=oh[:, hs].rearrange("p h k a -> p (h k) a"), axis=AX.X, op=ALU.add),
                                reads=["scw"], writes=[("sel", side)])
                            yield
                    for w3 in range(3):
                        S.op("pe", lambda E: E.transpose(out=PF[6][:, w3 * 128:(w3 + 1) * 128], in_=sel[:, w3, :],
                                                         identity=identf[:]),
                             reads=[("sel", 0), ("sel", 1), "gate", "identf"], writes=["PF6"])
                    S.op("act", lambda E: E.copy(out=hkT[:, :, blk * 128:(blk + 1) * 128],
                                                 in_=PF[6][:, 0:384].rearrange("p (a b) -> p a b", a=3)),
                         reads=["PF6"], writes=[("hkT", blk)])
                    yield

            def m_phase(u):
                for t4 in range(64):
                    pf, pfn = PF[5 + t4 % 2], "PF%d" % (5 + t4 % 2)
                    for q in range(4):
                        tk = 4 * t4 + q
                        ab = tk % 3
                        S.op("dve", lambda E: E.tensor_scalar(out=At[ab][:], in0=iotab[:], scalar1=hkT[:, 0, tk:tk + 1],
                                                              scalar2=hkT[:, 2, tk:tk + 1], op0=ALU.is_equal, op1=ALU.mult),
                             reads=["iotab", ("hkT", tk // 128)], writes=[("At", ab)])
                        S.op("dve", lambda E: E.tensor_scalar(out=Bt[ab][:], in0=iotab[:], scalar1=hkT[:, 1, tk:tk + 1],
                                                              scalar2=None, op0=ALU.is_equal),
                             reads=["iotab", ("hkT", tk // 128)], writes=[("Bt", ab)])
                        S.op("pe", lambda E: E.matmul(pf[:, q * 128:(q + 1) * 128], lhsT=Bt[ab][:], rhs=At[ab][:],
                                                      start=True, stop=True),
                             reads=[("At", ab), ("Bt", ab)], writes=[pfn])
                    S.op("act", lambda E: E.copy(out=GT[:, :, 4 * t4:4 * t4 + 4],
                                                 in_=pf[:].rearrange("p (t i) -> p i t", t=4)),
                         reads=[pfn], writes=[("GT", t4)] + ([("gst", t4 % 4)] if (u == 0 and t4 < 4) else []))

            OA = [[PF[0], PF[1]], [PF[2], PF[3]]]
            OAn = [["PF0", "PF1"], ["PF2", "PF3"]]

            def ld_tab(i):
                sl = i % 3
                S.dma("sp", "ldd%d" % sl, dsb[sl][:], down_b[i], writes=[("dsb", sl)])
                su = i % 4
                S.dma("sp", "ldu%d" % su, usb[su][:], up_b[i], writes=[("usb", su)])

            def dense_H(u, i):
                ub = u % 2
                xnT = xnTb[ub]
                sl = i % 3
                hb_, hbn = PF[4 + i % 2], "PF%d" % (4 + i % 2)
                for c in range(8):
                    S.op("pe", lambda E: E.matmul(hb_[:, 0:256], lhsT=dsb[sl][:, c, :],
                                                  rhs=xnT[:, c, :], start=(c == 0), stop=(c == 7)),
                         reads=[("dsb", sl), ("xnT", ub, 0), ("xnT", ub, 1)], writes=[hbn])
                S.op("act", lambda E: E.activation(out=gl[i % 3][:], in_=hb_[:, 0:256], func=AF.Gelu),
                     reads=[hbn], writes=[("gl", i % 3)])
                S.op("pool", lambda E: E.tensor_tensor(out=wg[i % 3][:], in0=gl[i % 3][:], in1=GT[:, i, :], op=ALU.mult),
                     reads=[("gl", i % 3)] + [("GT", t4) for t4 in (0, 63)], writes=[("wg", i % 3)])

            def dense_UP(i):
                su = i % 4
                for blk in range(2):
                    for half in range(2):
                        S.op("pe", lambda E: E.matmul(OA[blk][half][:], lhsT=wg[i % 3][:, blk * 128:(blk + 1) * 128],
                                                      rhs=usb[su][:, half * 512:(half + 1) * 512],
                                                      start=(i == 0), stop=(i == 127)),
                             reads=[("wg", i % 3), ("usb", su)], writes=[OAn[blk][half]])

            def advance(gen):
                if gen is None:
                    return None
                try:
                    next(gen)
                    return gen
                except StopIteration:
                    return None

            def o_phase(u):
                ub = u % 2
                x1 = x1b[ub]
                for blk in range(2):
                    for half in range(2):
                        S.op("dve", lambda E: E.tensor_tensor(out=x1[:, blk, half * 512:(half + 1) * 512], in0=OA[blk][half][:],
                                                              in1=x1[:, blk, half * 512:(half + 1) * 512], op=ALU.add),
                             reads=[OAn[blk][half], ("x1", ub, blk)], writes=[("x1", ub, blk)])
                    rms_rows(x1, blk, gfin, xo, 8 + 4 * blk, ("x1", ub, blk), "gfin", ("xo", blk))
                S.dma("sp", "oout", out[256 * u:256 * u + 256, :].rearrange("(j p) f -> p j f", p=128), xo[:],
                      reads=[("xo", 0), ("xo", 1)], writes=[("out", u)])

            g0 = pre_gen(0)
            while g0 is not None:
                g0 = advance(g0)
            m_phase(0)
            for u in range(ntile):
                nxt = pre_gen(u + 1) if u + 1 < ntile else None
                nadv = 0
                for i in range(2):
                    ld_tab(i)
                for step in range(130):
                    if step < 128:
                        dense_H(u, step)
                    if step >= 2:
                        dense_UP(step - 2)
                    if step + 2 < 128:
                        ld_tab(step + 2)
                    if nadv < 20 or step % 2 == 1:
                        nxt = advance(nxt)
                        nadv += 1
                while nxt is not None:
                    nxt = advance(nxt)
                o_phase(u)
                if u + 1 < ntile:
                    m_phase(u + 1)
        S.finish()
    return nc


def own_blocks(half):
    a = (0, 3) if half == 0 else (1, 2)
    return [4 * g + o for g in range(16) for o in a]


def consts(half):
    p = np.arange(128)
    invf = (10000.0 ** (-(p % 32).astype(np.float32) / 32.0)).astype(np.float32).reshape(128, 1)
    sgn = np.where((p % 64) < 32, -1.0, 1.0).astype(np.float32).reshape(128, 1)
    ident = np.eye(128, dtype=np.float32)
    uneg = -(p[:, None] >= p[None, :]).astype(np.float32)
    diag = np.where(p[:, None] < p[None, :], 0.0, NEG).astype(np.float32)
    full = np.full((128, 128), NEG, np.float32)
    none = np.zeros((128, 128), np.float32)
    if half == 0:
        sbm = np.stack([full, diag, diag, none])
    else:
        sbm = np.stack([diag, none, full, diag])
    q = np.arange(128)[:, None] + 128
    k = np.arange(256)[None, :]
    diff = q - k
    band = np.where((diff >= 0) & (diff < 128), 0.0, NEG).astype(np.float32)
    first = band.copy()
    if half == 0:
        first[:, :128] = NEG
    swm = np.stack([first, band])
    iota = np.tile(np.arange(128, dtype=np.float32)[None, :], (128, 1))
    return dict(c_invf=invf, c_sgn=sgn, c_ident=ident, c_uneg=uneg, c_sbm=sbm, c_swm=swm, c_iota=iota)


def prep_core(inp, core):
    b, half = core // 2, core % 2
    x = np.asarray(inp["x"][b], dtype=np.float32)
    pos = np.asarray(inp["positions"][b]).astype(np.int32)
    own = own_blocks(half)
    xb = x.reshape(64, 128, 1024)
    pb = pos.reshape(64, 128)
    x_po = np.zeros((32, 256, 1024), np.float32)
    pos_po = np.zeros((32, 256), np.int32)
    for j, B in enumerate(own):
        if B > 0:
            x_po[j, :128] = xb[B - 1]
            pos_po[j, :128] = pb[B - 1]
        x_po[j, 128:] = xb[B]
        pos_po[j, 128:] = pb[B]
    m = dict(
        x_all=np.ascontiguousarray(x), x_po=x_po, pos_po=pos_po.reshape(-1),
        w_in=np.ascontiguousarray(inp["w_in"][0]), attn_norm=np.ascontiguousarray(inp["attn_norm"][0]),
        sb_out_norm=np.ascontiguousarray(inp["sb_out_norm"][0]), swa_sinks=np.ascontiguousarray(inp["swa_sinks"][0]),
        swa_out_norm=np.ascontiguousarray(inp["swa_out_norm"][0]), w_out=np.ascontiguousarray(inp["w_out"][0]),
        ffn_norm=np.ascontiguousarray(inp["ffn_norm"][0]), peer_wq=np.ascontiguousarray(inp["peer_w_query"][0]),
        sk1T=np.ascontiguousarray(np.transpose(inp["peer_sub_keys_1"][0], (0, 2, 1))),
        sk2T=np.ascontiguousarray(np.transpose(inp["peer_sub_keys_2"][0], (0, 2, 1))),
        final_norm=np.ascontiguousarray(inp["final_norm"]),
    )
    m.update(consts(half))
    return m


from concourse.bass_utils import run_bass_kernel_spmd

_NC = None


def kernel(**inputs):
    global _NC
    inp = {k: np.asarray(v) for k, v in inputs.items()}
    if _NC is None:
        _NC = build(upto=3, nqt=8, nblk1a=64)
    downT = np.ascontiguousarray(inp["peer_expert_down"][0].T)
    up = np.ascontiguousarray(inp["peer_expert_up"][0])
    maps = []
    for c in range(8):
        m = prep_core(inp, c)
        m["downT"] = downT
        m["up"] = up
        maps.append(m)
    res = run_bass_kernel_spmd(_NC, maps, core_ids=list(range(8)))
    out = np.zeros((4, 8192, 1024), np.float32)
    for c in range(8):
        o = np.asarray(res.results[c]["out"]).reshape(32, 128, 1024)
        ob = out[c // 2].reshape(64, 128, 1024)
        for j, B in enumerate(own_blocks(c % 2)):
            ob[B] = o[j]
    return out
```

```python
import numpy as np
import concourse.bass as bass
import concourse.mybir as mybir

F32 = mybir.dt.float32
BF16 = mybir.dt.bfloat16
I32 = mybir.dt.int32
U32 = mybir.dt.uint32
AF = mybir.ActivationFunctionType
ALU = mybir.AluOpType
AX = mybir.AxisListType


class Sched:
    ENG = ("pe", "act", "dve", "pool", "sp")

    def __init__(self, nc):
        self.nc = nc
        self.e = {"pe": nc.tensor, "act": nc.scalar, "dve": nc.vector,
                  "pool": nc.gpsimd, "sp": nc.sync}
        self.sem = {k: nc.alloc_semaphore("sem_" + k) for k in self.ENG}
        self.cnt = {k: 0 for k in self.ENG}
        self.seen = {k: {} for k in self.ENG}
        self.semobj = {}
        for k in self.ENG:
            self.semobj["sem_" + k] = self.sem[k]
        self.dsem = {}
        self.bufs = {}
        self.nwait = 0
        self.ninst = 0

    def _need(self, eng, tok, waits):
        if tok is None:
            return
        sname, val, src = tok
        if src == eng and eng == "pe":
            return
        if self.seen[eng].get(sname, 0) >= val:
            return
        waits[sname] = max(waits.get(sname, 0), val)

    def _deps(self, eng, reads, writes):
        waits = {}
        for k in reads:
            b = self.bufs.get(k)
            if b is not None:
                self._need(eng, b["w"], waits)
        for k in writes:
            b = self.bufs.get(k)
            if b is not None:
                w = b["w"]
                if w is not None:
                    self._need(eng, w, waits)
                for r in b["r"]:
                    self._need(eng, r, waits)
        for sname, val in waits.items():
            self.e[eng].wait_ge(self.semobj[sname], val)
            self.seen[eng][sname] = val
            self.nwait += 1

    def _commit(self, tok, reads, writes):
        for k in reads:
            b = self.bufs.setdefault(k, {"w": None, "r": []})
            b["r"].append(tok)
            if len(b["r"]) > 12:
                last = {}
                for r in b["r"]:
                    if r[0] not in last or last[r[0]][1] < r[1]:
                        last[r[0]] = r
                b["r"] = list(last.values())
        for k in writes:
            self.bufs[k] = {"w": tok, "r": []}

    def op(self, eng, fn, reads=(), writes=()):
        self._deps(eng, reads, writes)
        inst = fn(self.e[eng])
        self.cnt[eng] += 1
        inst.then_inc(self.sem[eng], 1)
        tok = ("sem_" + eng, self.cnt[eng], eng)
        self._commit(tok, reads, writes)
        self.ninst += 1
        return inst

    def dma(self, eng, slot, out, in_, reads=(), writes=(), **kw):
        if slot not in self.dsem:
            s = self.nc.alloc_semaphore("dsem_" + slot)
            self.dsem[slot] = [s, 0]
            self.semobj["dsem_" + slot] = s
        self._deps(eng, reads, writes)
        d = self.dsem[slot]
        inst = self.e[eng].dma_start(out=out, in_=in_, **kw)
        d[1] += 16
        inst.then_inc(d[0], 16)
        tok = ("dsem_" + slot, d[1], None)
        self._commit(tok, reads, writes)
        self.ninst += 1
        return inst

    def barrier(self):
        for eng in self.ENG:
            for other in self.ENG:
                if other == eng or self.cnt[other] == 0:
                    continue
                if self.seen[eng].get("sem_" + other, 0) < self.cnt[other]:
                    self.e[eng].wait_ge(self.sem[other], self.cnt[other])
                    self.seen[eng]["sem_" + other] = self.cnt[other]
            for slot, (s, v) in self.dsem.items():
                if v and self.seen[eng].get("dsem_" + slot, 0) < v:
                    self.e[eng].wait_ge(s, v)
                    self.seen[eng]["dsem_" + slot] = v
        self.bufs = {}

    def finish(self, eng="sp"):
        for slot, (s, v) in self.dsem.items():
            if v:
                self.e[eng].wait_ge(s, v)
        for other in self.ENG:
            if other != eng and self.cnt[other]:
                self.e[eng].wait_ge(self.sem[other], self.cnt[other])


import math
from contextlib import ExitStack
import numpy as np
import concourse.bass as bass
import concourse.mybir as mybir

EPS = 1e-6
NEG = -30000.0
PI = math.pi
C1 = 6.28125
C2 = 2.0 * math.pi - 6.28125
NB = 64
NOWN = 32
DBG3 = False
SKEW1B = False
NUNITS = 64


def build(upto=3, nqt=8, nblk1a=NB):
    nc = bass.Bass("TRN2", target_bir_lowering=False)

    def din(name, shape, dt=F32):
        return nc.dram_tensor(name, shape, dt, kind="ExternalInput").ap()

    def dscr(name, shape, dt):
        return nc.dram_tensor(name, shape, dt, kind="Internal").ap()

    x_all = din("x_all", [8192, 1024])
    x_po = din("x_po", [NOWN, 256, 1024])
    pos_po = din("pos_po", [NOWN * 256], I32)
    w_in = din("w_in", [1024, 2304])
    attn_norm = din("attn_norm", [1024])
    sb_out_norm = din("sb_out_norm", [512])
    swa_sinks = din("swa_sinks", [8])
    swa_out_norm = din("swa_out_norm", [512])
    w_out = din("w_out", [1024, 1024])
    ffn_norm = din("ffn_norm", [1024])
    peer_wq = din("peer_wq", [1024, 2048])
    sk1T = din("sk1T", [8, 128, 128])
    sk2T = din("sk2T", [8, 128, 128])
    if upto >= 3:
        downT = din("downT", [1024, 16384])
        up = din("up", [16384, 1024])
    final_norm = din("final_norm", [1024])
    c_invf = din("c_invf", [128, 1])
    c_sgn = din("c_sgn", [128, 1])
    c_ident = din("c_ident", [128, 128])
    c_uneg = din("c_uneg", [128, 128])
    c_sbm = din("c_sbm", [4, 128, 128])
    c_swm = din("c_swm", [2, 128, 256])
    c_iota = din("c_iota", [128, 128])

    out = nc.dram_tensor("out", [NOWN * 128, 1024], F32, kind="ExternalOutput").ap()
    dbg3 = nc.dram_tensor("dbg3", [128, 640], F32, kind="ExternalOutput").ap() if DBG3 else None

    qsb_d = dscr("qsb_d", [NOWN, 128, 4, 128], BF16)
    qsw_d = dscr("qsw_d", [NOWN, 128, 4, 128], BF16)
    ksw_d = dscr("ksw_d", [NOWN, 128, 256], BF16)
    vsw_d = dscr("vsw_d", [NOWN, 128, 2, 128], BF16)
    mix_d = dscr("mix_d", [NOWN, 128, 1024], BF16)
    dbg = None
    if upto < 3:
        dbg = nc.dram_tensor("dbg", [NOWN, 128, 1024], F32, kind="ExternalOutput").ap()

    S = Sched(nc)
    w_in_v = w_in.rearrange("(c p) n -> p c n", p=128)

    with ExitStack() as top:
        def sb(name, shape, dt, st=top):
            return st.enter_context(nc.sbuf_tensor(name, shape, dt))

        PF = [top.enter_context(nc.psum_tensor("pf%d" % i, [128, 512], F32)) for i in range(7)]
        PB = top.enter_context(nc.psum_tensor("pb", [128, 8, 128], BF16))

        identb = sb("identb", [128, 128], BF16)
        unegb = sb("unegb", [128, 128], BF16)
        invf = sb("invf", [128, 1], F32)
        sgn = sb("sgn", [128, 1], F32)
        S.dma("pool", "c0", identb[:], c_ident, writes=["identb"])
        S.dma("pool", "c1", unegb[:], c_uneg, writes=["unegb"])
        S.dma("sp", "c2", invf[:], c_invf, writes=["invf"])
        S.dma("sp", "c3", sgn[:], c_sgn, writes=["sgn"])

        def norm_T(st, src, k, xt, junk, hb, hT, ss, gb, evac_eng):
            S.dma("sp", "xt%d" % k, xt[k][:], src, writes=[("xt", k)])
            S.op("act", lambda E: E.activation(out=junk[:], in_=xt[k][:], func=AF.Square,
                                               accum_out=ss[:, k:k + 1]),
                 reads=[("xt", k)], writes=["junk", ("ss", k)])
            S.op("dve", lambda E: E.tensor_scalar(out=ss[:, 2 + k:3 + k], in0=ss[:, k:k + 1], scalar1=1.0 / 1024,
                                                  scalar2=EPS, op0=ALU.mult, op1=ALU.add),
                 reads=[("ss", k)], writes=[("ms", k)])
            S.op("act", lambda E: E.activation(out=ss[:, 4 + k:5 + k], in_=ss[:, 2 + k:3 + k], func=AF.Sqrt),
                 reads=[("ms", k)], writes=[("sd", k)])
            S.op("dve", lambda E: E.reciprocal(out=ss[:, 6 + k:7 + k], in_=ss[:, 4 + k:5 + k]),
                 reads=[("sd", k)], writes=[("rstd", k)])
            S.op("dve", lambda E: E.scalar_tensor_tensor(out=hb[k][:], in0=xt[k][:], scalar=ss[:, 6 + k:7 + k],
                                                         in1=gb[:], op0=ALU.mult, op1=ALU.mult),
                 reads=[("xt", k), ("rstd", k), "gb"], writes=[("hb", k)])
            for c in range(8):
                S.op("pe", lambda E: E.transpose(out=PB[:, c, :], in_=hb[k][:, c * 128:(c + 1) * 128],
                                                 identity=identb[:]),
                     reads=[("hb", k), "identb"], writes=["PB"])
            if evac_eng == "act":
                S.op("act", lambda E: E.copy(out=hT[k][:], in_=PB[:]), reads=["PB"], writes=[("hT", k)])
            else:
                S.op("dve", lambda E: E.tensor_copy(out=hT[k][:], in_=PB[:]), reads=["PB"], writes=[("hT", k)])

        if upto >= 3:
            down_b = dscr("down_b", [128, 128, 8, 128], BF16)
            up_b = dscr("up_b", [128, 128, 1024], BF16)
            downT_v = downT.rearrange("(c p) (g e) -> g p c e", p=128, e=512)
            up_v = up.rearrange("(g i e) f -> g e i f", i=4, e=128)
        with ExitStack() as p1:
            W2 = sb("W2", [128, 8, 1920], BF16, p1)
            gb = sb("gb", [128, 1024], F32, p1)
            xt = [sb("xt%d" % i, [128, 1024], F32, p1) for i in range(2)]
            junk = sb("junk", [128, 1024], BF16, p1)
            hb = [sb("hb%d" % i, [128, 1024], BF16, p1) for i in range(2)]
            hT = [sb("hT%d" % i, [128, 8, 128], BF16, p1) for i in range(2)]
            ss = sb("ss", [128, 8], F32, p1)
            posi = sb("posi", [128, 128], I32, p1)
            posf = sb("posf", [128, 128], F32, p1)
            ang = sb("ang", [128, 128], F32, p1)
            tq = sb("tq", [128, 128], F32, p1)
            ki = sb("ki", [128, 128], I32, p1)
            kf = sb("kf", [128, 128], F32, p1)
            rr = sb("rr", [128, 128], F32, p1)
            gg = sb("gg", [128, 128], F32, p1)
            cosb2 = [sb("cosb%d" % i, [128, 1, 128], F32, p1) for i in range(2)]
            sinb2 = [sb("sinb%d" % i, [128, 1, 128], F32, p1) for i in range(2)]
            t1 = sb("t1", [128, 4, 128], F32, p1)
            t2 = sb("t2", [128, 4, 128], F32, p1)
            qsb_s = [sb("qsb_s%d" % i, [128, 4, 128], BF16, p1) for i in range(2)]
            qsw_s = [sb("qsw_s%d" % i, [128, 4, 128], BF16, p1) for i in range(2)]
            ksw_s = [sb("ksw_s%d" % i, [128, 256], BF16, p1) for i in range(2)]
            vsw_s = [sb("vsw_s%d" % i, [128, 2, 128], BF16, p1) for i in range(2)]

            NST = 4
            stf = [sb("stf%d" % i, [128, 4096], F32, p1) for i in range(NST)]
            if upto >= 3:
                stb = [sb("stb%d" % i, [128, 4096], BF16, p1) for i in range(NST)]

            def w2_chunk(dst_c0, src_c0, n, k, eng):
                fv = stf[k][:, 0:8 * n].rearrange("p (c e) -> p c e", c=8)
                S.dma("sp", "w2s%d" % k, fv, w_in_v[:, :, src_c0:src_c0 + n], writes=[("stf", k)])
                S.op(eng, lambda E: E.tensor_copy(out=W2[:, :, dst_c0:dst_c0 + n], in_=fv),
                     reads=[("stf", k)], writes=["W2"])

            def w2_swap(dst_c0, src_c0, n, eng):
                sv = W2[:, :, src_c0:src_c0 + n].rearrange("p c (h two i) -> p c h two i", two=2, i=32)
                dv = W2[:, :, dst_c0:dst_c0 + n].rearrange("p c (h two i) -> p c h two i", two=2, i=32)
                for two in range(2):
                    S.op(eng, lambda E: E.tensor_copy(out=dv[:, :, :, two, :], in_=sv[:, :, :, 1 - two, :]),
                         reads=["W2"], writes=["W2"])

            fkv = stf[3][:, 0:2048].rearrange("p (c e) -> p c e", c=8)
            S.dma("sp", "w2s3", fkv, w_in_v[:, :, 2048:2304], writes=[("stf", 3)])
            S.op("dve", lambda E: E.tensor_copy(out=W2[:, :, 1536:1664], in_=fkv[:, :, 0:128]), reads=[("stf", 3)], writes=["W2"])
            S.op("pool", lambda E: E.tensor_copy(out=W2[:, :, 1792:1920], in_=fkv[:, :, 128:256]), reads=[("stf", 3)], writes=["W2"])
            w2_swap(1664, 1536, 128, "dve")
            w2_chunk(512, 1536, 512, 3, "dve")
            w2_chunk(0, 0, 512, 2, "pool")
            w2_swap(1024, 512, 512, "pool")
            pc_steps = [(g, which) for g in range(32) for which in range(2)]

            def pc_load(n):
                g, which = pc_steps[n]
                k = n % NST
                if which == 0:
                    src = downT_v[g]
                    fv = stf[k][:].rearrange("p (c e) -> p c e", c=8)
                else:
                    src = up_v[g]
                    fv = stf[k][:].rearrange("p (i f) -> p i f", i=4)
                S.dma("sp", "pc_in%d" % k, fv, src, writes=[("stf", k)])

            def pc_cast_store(n):
                g, which = pc_steps[n]
                k = n % NST
                if which == 0:
                    dst = down_b[4 * g:4 * g + 4].rearrange("i p c e -> p i (c e)")
                    co = stb[k][:].rearrange("p (i c e) -> p c i e", i=4, c=8)
                    ci = stf[k][:].rearrange("p (c i e) -> p c i e", c=8, i=4)
                else:
                    dst = up_b[4 * g:4 * g + 4].rearrange("i p f -> p i f")
                    co, ci = stb[k][:], stf[k][:]
                bv = stb[k][:].rearrange("p (i x) -> p i x", i=4)
                eng = "pool" if n % 2 == 0 else "dve"
                S.op(eng, lambda E: E.tensor_copy(out=co, in_=ci), reads=[("stf", k)], writes=[("stb", k)])
                S.dma("sp", "pc_out%d" % k, dst, bv, reads=[("stb", k)], writes=[("tab", which, g)])

            W2_PENDING = True
            S.dma("sp", "gb", gb[:], attn_norm.partition_broadcast(128), writes=["gb"])

            def rope_tables(j, sub):
                cosb, sinb = cosb2[sub], sinb2[sub]
                S.dma("sp", "posi", posi[:], pos_po[j * 256 + sub * 128: j * 256 + sub * 128 + 128].partition_broadcast(128),
                      writes=["posi"])
                S.op("dve", lambda E: E.tensor_copy(out=posf[:], in_=posi[:]), reads=["posi"], writes=["posf"])
                S.op("dve", lambda E: E.tensor_scalar(out=ang[:], in0=posf[:], scalar1=invf[:, 0:1], scalar2=None,
                                                      op0=ALU.mult), reads=["posf", "invf"], writes=["ang"])
                S.op("dve", lambda E: E.tensor_scalar(out=tq[:], in0=ang[:], scalar1=1.0 / (2 * PI), scalar2=None,
                                                      op0=ALU.mult), reads=["ang"], writes=["tq"])
                S.op("dve", lambda E: E.tensor_copy(out=ki[:], in_=tq[:]), reads=["tq"], writes=["ki"])
                S.op("dve", lambda E: E.tensor_copy(out=kf[:], in_=ki[:]), reads=["ki"], writes=["kf"])
                S.op("dve", lambda E: E.scalar_tensor_tensor(out=rr[:], in0=kf[:], scalar=-C1, in1=ang[:],
                                                             op0=ALU.mult, op1=ALU.add),
                     reads=["kf", "ang"], writes=["rr"])
                S.op("dve", lambda E: E.scalar_tensor_tensor(out=rr[:], in0=kf[:], scalar=-C2, in1=rr[:],
                                                             op0=ALU.mult, op1=ALU.add),
                     reads=["kf", "rr"], writes=["rr"])
                S.op("dve", lambda E: E.tensor_scalar(out=gg[:], in0=rr[:], scalar1=PI, scalar2=2 * PI,
                                                      op0=ALU.is_gt, op1=ALU.mult), reads=["rr"], writes=["gg"])
                S.op("dve", lambda E: E.tensor_tensor(out=rr[:], in0=rr[:], in1=gg[:], op=ALU.subtract),
                     reads=["rr", "gg"], writes=["rr"])
                S.op("dve", lambda E: E.tensor_scalar(out=gg[:], in0=rr[:], scalar1=-PI, scalar2=2 * PI,
                                                      op0=ALU.is_lt, op1=ALU.mult), reads=["rr"], writes=["gg"])
                S.op("dve", lambda E: E.tensor_tensor(out=rr[:], in0=rr[:], in1=gg[:], op=ALU.add),
                     reads=["rr", "gg"], writes=["rr"])
                S.op("dve", lambda E: E.tensor_scalar(out=rr[:], in0=rr[:], scalar1=3.14159, scalar2=-3.14159,
                                                      op0=ALU.min, op1=ALU.max), reads=["rr"], writes=["rr"])
                S.op("act", lambda E: E.activation(out=sinb[:, 0, :], in_=rr[:], func=AF.Sin, scale=sgn[:, 0:1]),
                     reads=["rr", "sgn"], writes=[("sinb", sub)])
                S.op("dve", lambda E: E.scalar_tensor_tensor(out=gg[:], in0=rr[:], scalar=-1.0, in1=rr[:], op0=ALU.mult, op1=ALU.max),
                     reads=["rr"], writes=["gg"])
                S.op("act", lambda E: E.activation(out=cosb[:, 0, :], in_=gg[:], func=AF.Sin, scale=-1.0,
                                                   bias=PI / 2), reads=["gg"], writes=[("cosb", sub)])

            def proj_T(ps_ap, wc0, k, nm):
                for c in range(8):
                    S.op("pe", lambda E: E.matmul(ps_ap, lhsT=W2[:, c, wc0:wc0 + 128], rhs=hT[k][:, c, :],
                                                  start=(c == 0), stop=(c == 7)),
                         reads=["W2", ("hT", k)], writes=[nm])

            def stage_a(j, sub):
                norm_T(p1, x_po[j, sub * 128:(sub + 1) * 128, :], sub, xt, junk, hb, hT, ss, gb,
                       "act" if sub == 0 else "dve")
                rope_tables(j, sub)

            def stage_b(j, sub):
                    jb = j % 2
                    k = sub
                    cosb, sinb = cosb2[sub], sinb2[sub]
                    kvb = PF[4] if sub == 0 else PF[3]
                    kvn = "PF4" if sub == 0 else "PF3"
                    proj_T(kvb[:, 0:128], 1536, k, kvn)
                    proj_T(kvb[:, 128:256], 1664, k, kvn)
                    for c in range(8):
                        S.op("pe", lambda E: E.matmul(kvb[:, 256:384], lhsT=hT[k][:, c, :], rhs=W2[:, c, 1792:1920],
                                                      start=(c == 0), stop=(c == 7)),
                             reads=["W2", ("hT", k)], writes=[kvn])
                    S.op("dve", lambda E: E.tensor_tensor(out=t1[:, 0, :], in0=kvb[:, 0:128], in1=cosb[:, 0, :], op=ALU.mult),
                         reads=[kvn, ("cosb", sub)], writes=["t1"])
                    S.op("dve", lambda E: E.tensor_tensor(out=t2[:, 0, :], in0=kvb[:, 128:256], in1=sinb[:, 0, :], op=ALU.mult),
                         reads=[kvn, ("sinb", sub)], writes=["t2"])
                    S.op("pool", lambda E: E.tensor_tensor(out=ksw_s[jb][:, sub * 128:(sub + 1) * 128], in0=t1[:, 0, :],
                                                           in1=t2[:, 0, :], op=ALU.add),
                         reads=["t1", "t2"], writes=[("ksw_s", jb)])
                    S.op("act", lambda E: E.copy(out=vsw_s[jb][:, sub, :], in_=kvb[:, 256:384]),
                         reads=[kvn], writes=[("vsw_s", jb)])
                    if sub == 1:
                        for hp in range(4):
                            proj_T(PF[0][:, hp * 128:(hp + 1) * 128], hp * 128, k, "PF0")
                        for p in range(4):
                            proj_T(PF[1][:, p * 128:(p + 1) * 128], 512 + p * 128, k, "PF1")
                        for p in range(4):
                            proj_T(PF[2][:, p * 128:(p + 1) * 128], 1024 + p * 128, k, "PF2")
                        S.op("act", lambda E: E.activation(out=qsb_s[jb][:].rearrange("p a b -> p (a b)"), in_=PF[0][:],
                                                           func=AF.Copy, scale=0.125),
                             reads=["PF0"], writes=[("qsb_s", jb)])
                        S.op("dve", lambda E: E.scalar_tensor_tensor(
                            out=t1[:], in0=PF[1][:].rearrange("p (a b) -> p a b", a=4), scalar=0.125,
                            in1=cosb[:].to_broadcast([128, 4, 128]), op0=ALU.mult, op1=ALU.mult),
                            reads=["PF1", ("cosb", sub)], writes=["t1"])
                        S.op("dve", lambda E: E.scalar_tensor_tensor(
                            out=t2[:], in0=PF[2][:].rearrange("p (a b) -> p a b", a=4), scalar=0.125,
                            in1=sinb[:].to_broadcast([128, 4, 128]), op0=ALU.mult, op1=ALU.mult),
                            reads=["PF2", ("sinb", sub)], writes=["t2"])
                        S.op("pool", lambda E: E.tensor_tensor(out=qsw_s[jb][:], in0=t1[:], in1=t2[:], op=ALU.add),
                             reads=["t1", "t2"], writes=[("qsw_s", jb)])
            def stage_out(j):
                jb = j % 2
                S.dma("sp", "o_qsb%d" % jb, qsb_d[j], qsb_s[jb][:], reads=[("qsb_s", jb)], writes=[("qsb_d", j)])
                S.dma("sp", "o_qsw%d" % jb, qsw_d[j], qsw_s[jb][:], reads=[("qsw_s", jb)], writes=[("qsw_d", j)])
                S.dma("sp", "o_ksw%d" % jb, ksw_d[j], ksw_s[jb][:], reads=[("ksw_s", jb)], writes=[("ksw_d", j)])
                S.dma("sp", "o_vsw%d" % jb, vsw_d[j], vsw_s[jb][:], reads=[("vsw_s", jb)], writes=[("vsw_d", j)])

            units = [(j, sub) for j in range(NOWN) for sub in range(2)][:NUNITS]
            if SKEW1B:
                stage_a(*units[0])
            if upto >= 3:
                for n in range(NST - 1):
                    pc_load(n)
            for n, (j, sub) in enumerate(units):
                if SKEW1B:
                    if n + 1 < len(units):
                        stage_a(*units[n + 1])
                else:
                    stage_a(j, sub)
                if upto >= 3:
                    if n + NST - 1 < len(pc_steps):
                        pc_load(n + NST - 1)
                    pc_cast_store(n)
                stage_b(j, sub)
                if sub == 1:
                    stage_out(j)
            S.barrier()

        if upto == 0:
            S.finish()
            return nc

        with ExitStack() as p2:
            KT = sb("KT", [128, 4, 8192], BF16, p2)
            V = sb("V", [128, NB, 512], BF16, p2)
            with ExitStack() as p1a:
                W1 = sb("W1", [128, 8, 1024], BF16, p1a)
                gb = sb("gb1", [128, 1024], F32, p1a)
                xt = [sb("xta%d" % i, [128, 1024], F32, p1a) for i in range(2)]
                junk = sb("junka", [128, 1024], BF16, p1a)
                hb = [sb("hba%d" % i, [128, 1024], BF16, p1a) for i in range(2)]
                hT = [sb("hTa%d" % i, [128, 8, 128], BF16, p1a) for i in range(2)]
                ss = sb("ssa", [128, 8], F32, p1a)
                stg1 = [sb("stg1a%d" % i, [128, 4096], F32, p1a) for i in range(2)]
                for i_, (d0_, s0_) in enumerate(((0, 512), (512, 1024))):
                    fv_ = stg1[i_][:].rearrange("p (c e) -> p c e", c=8)
                    S.dma("sp", "w1s%d" % i_, fv_, w_in_v[:, :, s0_:s0_ + 512], writes=[("stg1", i_)])
                    S.op("dve" if i_ == 0 else "pool", lambda E: E.tensor_copy(out=W1[:, :, d0_:d0_ + 512], in_=fv_),
                         reads=[("stg1", i_)], writes=["W1"])
                S.dma("sp", "gb1", gb[:], attn_norm.partition_broadcast(128), writes=["gb"])
                def na(blk):
                    norm_T(p1a, x_all[blk * 128:(blk + 1) * 128, :], blk % 2, xt, junk, hb, hT, ss, gb,
                           "act" if blk % 2 == 0 else "dve")
                na(0)
                for blk in range(nblk1a):
                    k = blk % 2
                    if blk + 1 < nblk1a:
                        na(blk + 1)
                    pk, pkn = PF[2 * k], "PF%d" % (2 * k)
                    pv, pvn = PF[2 * k + 1], "PF%d" % (2 * k + 1)
                    for hp in range(4):
                        for c in range(8):
                            S.op("pe", lambda E: E.matmul(pk[:, hp * 128:(hp + 1) * 128], lhsT=W1[:, c, hp * 128:(hp + 1) * 128],
                                                          rhs=hT[k][:, c, :], start=(c == 0), stop=(c == 7)),
                                 reads=["W1", ("hT", k)], writes=[pkn])
                    for c in range(8):
                        S.op("pe", lambda E: E.matmul(pv[:], lhsT=hT[k][:, c, :], rhs=W1[:, c, 512:1024],
                                                      start=(c == 0), stop=(c == 7)),
                             reads=["W1", ("hT", k)], writes=[pvn])
                    S.op("dve", lambda E: E.tensor_copy(out=KT[:, :, blk * 128:(blk + 1) * 128],
                                                        in_=pk[:].rearrange("p (a b) -> p a b", a=4)),
                         reads=[pkn], writes=[("KT", blk)])
                    S.op("act", lambda E: E.copy(out=V[:, blk, :], in_=pv[:]), reads=[pvn], writes=[("V", blk)])
                S.barrier()

            if upto == 1:
                S.finish()
                return nc

            with ExitStack() as p2b:
                Qz = [sb("Qz%d" % i, [128, 8, 512], BF16, p2b) for i in range(2)]
                ebuf = [sb("ebuf%d" % i, [128, 512], F32, p2b) for i in range(2)]
                spb = [sb("spb%d" % i, [128, 512], BF16, p2b) for i in range(2)]
                wb = [sb("wb%d" % i, [128, 512], BF16, p2b) for i in range(2)]
                Ob = [sb("Ob0", [128, 4, 512], F32, p2b)] * 2
                Call = sb("Call", [128, 8], F32, p2b)
                Cst = [Call[:, 0:4], Call[:, 4:8]]
                call = sb("call", [128, 8], F32, p2b)
                cst = [call[:, 0:4], call[:, 4:8]]
                sbm = sb("sbm", [128, 4, 128], BF16, p2b)
                swm = sb("swm", [128, 2, 256], F32, p2b)
                negones = sb("negones", [128, 2], BF16, p2b)
                gsb = sb("gsb", [128, 512], F32, p2b)
                gsw = sb("gsw", [128, 512], F32, p2b)
                sinkb = sb("sinkb", [128, 8], F32, p2b)
                Qswz = [sb("Qswz%d" % i, [128, 8, 128], BF16, p2b) for i in range(2)]
                kswt = [sb("kswt%d" % i, [128, 256], BF16, p2b) for i in range(2)]
                vswt = [sb("vswt%d" % i, [128, 2, 128], BF16, p2b) for i in range(2)]
                zm = sb("zm", [128, 4, 256], F32, p2b)
                pexp = sb("pexp", [128, 4, 256], BF16, p2b)
                pT = sb("pT", [128, 8, 128], BF16, p2b)
                sst = sb("sst", [128, 40], F32, p2b)
                Osw = sb("Osw", [128, 4, 512], F32, p2b)
                mixb = [sb("mixb0", [128, 4, 1024], BF16, p2b)] * 2
                junk2 = sb("junk2", [128, 512], BF16, p2b)
                rst = sb("rst", [128, 32], F32, p2b)
                tmpo = [sb("tmpo%d" % i, [128, 4, 64], F32, p2b) for i in range(2)]

                S.dma("pool", "sbm", sbm[:], c_sbm.rearrange("m p f -> p m f"), writes=["sbm"])
                S.dma("sp", "swm", swm[:], c_swm.rearrange("m p f -> p m f"), writes=["swm"])
                S.dma("sp", "gsb", gsb[:], sb_out_norm.partition_broadcast(128), writes=["gsb"])
                S.dma("sp", "gsw", gsw[:], swa_out_norm.partition_broadcast(128), writes=["gsw"])
                S.dma("sp", "sinkb", sinkb[:], swa_sinks.partition_broadcast(128), writes=["sinkb"])
                S.op("pool", lambda E: E.memset(negones[:], -1.0), writes=["negones"])
                for i in range(2):
                    S.op("pool", lambda E: E.memset(Qz[i][:], 0.0), writes=[("Qz", i)])
                    S.op("pool", lambda E: E.memset(Qswz[i][:], 0.0), writes=[("Qswz", i)])

                Zb = [PF[0], PF[1], PF[2], PF[3]]
                Zn = ["PF0", "PF1", "PF2", "PF3"]
                PVb = [PF[4], PF[5], PF[6]]
                PVn = ["PF4", "PF5", "PF6"]

                def load_q(t):
                    tb = t % 2
                    for jj in range(4):
                        j = 4 * t + jj
                        for r in range(2):
                            dst = Qz[tb][r * 64:(r + 1) * 64, :, jj * 128:(jj + 1) * 128] \
                                .rearrange("p (hp two) f -> p hp two f", two=2)[:, :, r, :]
                            S.dma("sp", "ldq%d" % tb, dst, qsb_d[j, r * 64:(r + 1) * 64, :, :],
                                  reads=[("qsb_d", j)], writes=[("Qz", tb)])

                def sb_tiles(t):
                    L = []
                    for hp in range(4):
                        kbs = list(range(8 * t + 7, -1, -1))
                        for n, kb in enumerate(kbs):
                            for r in range(2):
                                h = 2 * hp + r
                                if kb >= 8 * t:
                                    d = kb - 8 * t
                                    j0 = d // 2
                                    m = (0 if d % 2 == 1 else 1) + (0 if j0 % 2 == 0 else 2)
                                else:
                                    j0, m = 0, None
                                L.append(dict(h=h, kb=kb, j0=j0, m=m, first=(n == 0), last=(n == len(kbs) - 1),
                                              s=r))
                    return L

                def S1(i, T, tb):
                    z, zn = Zb[i % 4], Zn[i % 4]
                    c0 = T["j0"] * 128
                    hp = T["h"] // 2
                    S.op("pe", lambda E: E.matmul(z[:, c0:512], lhsT=KT[:, hp, T["kb"] * 128:(T["kb"] + 1) * 128],
                                                  rhs=Qz[tb][:, T["h"], c0:512], start=True, stop=(T["m"] is None)),
                         reads=[("KT", T["kb"]), ("Qz", tb)], writes=[zn])
                    if T["m"] is not None:
                        S.op("pe", lambda E: E.matmul(z[:, c0:c0 + 128], lhsT=identb[:], rhs=sbm[:, T["m"], :],
                                                      start=False, stop=True),
                             reads=["identb", "sbm"], writes=[zn])

                def A1(i, T):
                    z, zn = Zb[i % 4], Zn[i % 4]
                    c0 = T["j0"] * 128
                    S.op("act", lambda E: E.activation(out=ebuf[i % 2][:, c0:512], in_=z[:, c0:512], func=AF.Exp),
                         reads=[zn], writes=[("ebuf", i % 2)])

                def A2(i, T):
                    c0 = T["j0"] * 128
                    S.op("act", lambda E: E.activation(out=spb[i % 2][:, c0:512], in_=ebuf[i % 2][:, c0:512],
                                                       func=AF.Ln, bias=1.0, scale=1.0),
                         reads=[("ebuf", i % 2)], writes=[("spb", i % 2)])

                def S3(i, T):
                    z, zn = Zb[i % 4], Zn[i % 4]
                    c0 = T["j0"] * 128
                    S.op("pe", lambda E: E.matmul(z[:, c0:512], lhsT=unegb[:], rhs=spb[i % 2][:, c0:512],
                                                  start=False, stop=True, skip_group_check=True),
                         reads=["unegb", ("spb", i % 2)], writes=[zn])
                    for jj in range(T["j0"], 4):
                        S.op("pe", lambda E: E.matmul(PVb[i % 3][:, 256 + jj:257 + jj],
                                                      lhsT=spb[i % 2][:, jj * 128:(jj + 1) * 128],
                                                      rhs=negones[:, 0:1], start=True, stop=True),
                             reads=[("spb", i % 2), "negones"], writes=[PVn[i % 3]])

                def A3(i, T):
                    z, zn = Zb[i % 4], Zn[i % 4]
                    c0 = T["j0"] * 128
                    S.op("act", lambda E: E.activation(out=wb[i % 2][:, c0:512], in_=z[:, c0:512], func=AF.Exp),
                         reads=[zn], writes=[("wb", i % 2)])

                def S5(i, T):
                    h = T["h"]
                    for jj in range(T["j0"], 4):
                        S.op("pe", lambda E: E.matmul(PVb[i % 3][:, jj * 64:(jj + 1) * 64],
                                                      lhsT=wb[i % 2][:, jj * 128:(jj + 1) * 128],
                                                      rhs=V[:, T["kb"], h * 64:(h + 1) * 64], start=True, stop=True),
                             reads=[("wb", i % 2), ("V", T["kb"])], writes=[PVn[i % 3]])

                def S6(i, T, tb):
                    s, h = T["s"], T["h"]
                    O = Ob[tb]
                    if T["first"]:
                        S.op("dve", lambda E: E.memset(Cst[s], 0.0), writes=[("Cst", s)])
                        S.op("dve", lambda E: E.memset(cst[s], 1.0), writes=[("cst", s)])
                        S.op("pool", lambda E: E.memset(O[:, :, h * 64:(h + 1) * 64], 0.0), writes=[("Ob", 0, h)])
                    j0 = T["j0"]
                    nj = 4 - j0
                    tm = tmpo[i % 2]
                    S.op("dve", lambda E: E.tensor_tensor(
                        out=tm[:, j0:4, :], in0=PVb[i % 3][:, j0 * 64:256].rearrange("p (a b) -> p a b", a=nj),
                        in1=cst[s][:, j0:4].unsqueeze(2).to_broadcast([128, nj, 64]), op=ALU.mult),
                        reads=[PVn[i % 3], ("cst", s)], writes=[("tmpo", i % 2)])
                    S.op("pool", lambda E: E.tensor_tensor(
                        out=O[:, j0:4, h * 64:(h + 1) * 64], in0=O[:, j0:4, h * 64:(h + 1) * 64], in1=tm[:, j0:4, :],
                        op=ALU.add),
                        reads=[("tmpo", i % 2), ("Ob", 0, h)], writes=[("Ob", 0, h)])
                    if not T["last"]:
                        j0 = T["j0"]
                        S.op("dve", lambda E: E.tensor_tensor(out=Cst[s][:, j0:4], in0=PVb[i % 3][:, 256 + j0:260],
                                                              in1=Cst[s][:, j0:4], op=ALU.add),
                             reads=[PVn[i % 3], ("Cst", s)], writes=[("Cst", s)])
                        pend_exp.append(s)

                pend_exp = []

                pend_age = [0]

                def flush_exp():
                    if not pend_exp:
                        pend_age[0] = 0
                        return
                    if len(pend_exp) >= 2:
                        assert sorted(pend_exp[:2]) == [0, 1], pend_exp
                        del pend_exp[:2]
                        S.op("act", lambda E: E.activation(out=call[:], in_=Call[:], func=AF.Exp),
                             reads=[("Cst", 0), ("Cst", 1)], writes=[("cst", 0), ("cst", 1)])
                        pend_age[0] = 0
                        return
                    pend_age[0] += 1
                    if pend_age[0] >= 2:
                        s = pend_exp.pop(0)
                        S.op("act", lambda E: E.activation(out=cst[s], in_=Cst[s], func=AF.Exp),
                             reads=[("Cst", s)], writes=[("cst", s)])
                        pend_age[0] = 0

                def sb_attention(t):
                    tb = t % 2
                    L = sb_tiles(t)
                    n = len(L)
                    S1(0, L[0], tb)
                    S1(1, L[1], tb)
                    A1(0, L[0])
                    for it in range(n + 2):
                        if it + 2 < n:
                            S1(it + 2, L[it + 2], tb)
                        if it < n:
                            A2(it, L[it])
                            S3(it, L[it])
                        if it + 1 < n:
                            A1(it + 1, L[it + 1])
                        flush_exp()
                        if 1 <= it <= n:
                            A3(it - 1, L[it - 1])
                        if 2 <= it:
                            S5(it - 2, L[it - 2])
                            S6(it - 2, L[it - 2], tb)

                def swa_block(t, jj):
                    j = 4 * t + jj
                    jb = j % 2
                    S.dma("sp", "ldk%d" % jb, kswt[jb][:], ksw_d[j], reads=[("ksw_d", j)], writes=[("kswt", jb)])
                    S.dma("sp", "ldv%d" % jb, vswt[jb][:], vsw_d[j], reads=[("vsw_d", j)], writes=[("vswt", jb)])
                    srcv = qsw_d[j].rearrange("(r d) p t -> d p r t", r=2)
                    for kv in range(2):
                        dst = Qswz[jb][kv * 64:(kv + 1) * 64, kv * 4:(kv + 1) * 4, :] \
                            .rearrange("d (p r) t -> d p r t", r=2)
                        for r in range(2):
                            S.dma("sp", "ldqs%d" % jb, dst[:, :, r, :], srcv[:, 2 * kv:2 * kv + 2, r, :],
                                  reads=[("qsw_d", j)], writes=[("Qswz", jb)])
                    mk = swm[:, 0:1, :] if j == 0 else swm[:, 1:2, :]
                    for kv in range(2):
                        for g4 in range(4):
                            g = kv * 4 + g4
                            zb, zbn = (PF[0], "PF0") if g4 < 2 else (PF[1], "PF1")
                            S.op("pe", lambda E: E.matmul(zb[:, (g4 % 2) * 256:(g4 % 2 + 1) * 256], lhsT=Qswz[jb][:, g, :],
                                                          rhs=kswt[jb][:], start=True, stop=True),
                                 reads=[("Qswz", jb), ("kswt", jb)], writes=[zbn])
                        for hb2 in range(2):
                            zb, zbn = (PF[0], "PF0") if hb2 == 0 else (PF[1], "PF1")
                            S.op("dve", lambda E: E.tensor_tensor(out=zm[:, 2 * hb2:2 * hb2 + 2, :],
                                                                  in0=zb[:].rearrange("p (a b) -> p a b", a=2),
                                                                  in1=mk.to_broadcast([128, 2, 256]), op=ALU.add),
                                 reads=[zbn, "swm"], writes=["zm"])
                        o = kv * 4
                        S.op("dve", lambda E: E.tensor_reduce(out=sst[:, o:o + 4], in_=zm[:], axis=AX.X, op=ALU.max),
                             reads=["zm"], writes=["sst_m"])
                        S.op("dve", lambda E: E.tensor_tensor(out=sst[:, o:o + 4], in0=sst[:, o:o + 4],
                                                              in1=sinkb[:, o:o + 4], op=ALU.max),
                             reads=["sst_m", "sinkb"], writes=["sst_m"])
                        S.op("dve", lambda E: E.tensor_scalar(out=sst[:, 8 + o:12 + o], in0=sst[:, o:o + 4], scalar1=-1.0,
                                                              scalar2=None, op0=ALU.mult),
                             reads=["sst_m"], writes=["sst_nm"])
                        for g4 in range(4):
                            S.op("act", lambda E: E.activation(out=pexp[:, g4, :], in_=zm[:, g4, :], func=AF.Exp,
                                                               bias=sst[:, 8 + o + g4:9 + o + g4], scale=1.0,
                                                               accum_out=sst[:, 16 + o + g4:17 + o + g4]),
                                 reads=["zm", "sst_nm"], writes=["pexp", "sst_rs"])
                        S.op("dve", lambda E: E.tensor_tensor(out=sst[:, 24 + o:28 + o], in0=sinkb[:, o:o + 4],
                                                              in1=sst[:, o:o + 4], op=ALU.subtract),
                             reads=["sst_m", "sinkb"], writes=["sst_d"])
                        S.op("act", lambda E: E.activation(out=sst[:, 24 + o:28 + o], in_=sst[:, 24 + o:28 + o], func=AF.Exp),
                             reads=["sst_d"], writes=["sst_es"])
                        S.op("dve", lambda E: E.tensor_tensor(out=sst[:, 32 + o:36 + o], in0=sst[:, 24 + o:28 + o],
                                                              in1=sst[:, 16 + o:20 + o], op=ALU.add),
                             reads=["sst_es", "sst_rs"], writes=["sst_den"])
                        S.op("dve", lambda E: E.reciprocal(out=sst[:, 32 + o:36 + o], in_=sst[:, 32 + o:36 + o]),
                             reads=["sst_den"], writes=["sst_rden"])
                        for g4 in range(4):
                            for hf in range(2):
                                S.op("pe", lambda E: E.transpose(out=PB[:, g4 * 2 + hf, :],
                                                                 in_=pexp[:, g4, hf * 128:(hf + 1) * 128],
                                                                 identity=identb[:]),
                                     reads=["pexp", "identb"], writes=["PB"])
                        S.op("act", lambda E: E.copy(out=pT[:], in_=PB[:]), reads=["PB"], writes=["pT"])
                        for g4 in range(4):
                            g = kv * 4 + g4
                            for hf in range(2):
                                S.op("pe", lambda E: E.matmul(PF[2][:, g * 64:(g + 1) * 64], lhsT=pT[:, g4 * 2 + hf, :],
                                                              rhs=vswt[jb][:, hf, kv * 64:(kv + 1) * 64],
                                                              start=(hf == 0), stop=(hf == 1)),
                                     reads=["pT", ("vswt", jb)], writes=["PF2"])
                        S.op("dve", lambda E: E.tensor_tensor(
                            out=Osw[:, jj, kv * 256:(kv + 1) * 256].rearrange("p (a b) -> p a b", a=4),
                            in0=PF[2][:, kv * 256:(kv + 1) * 256].rearrange("p (a b) -> p a b", a=4),
                            in1=sst[:, 32 + o:36 + o].unsqueeze(2).to_broadcast([128, 4, 64]), op=ALU.mult),
                            reads=["PF2", "sst_rden"], writes=[("Osw", jj)])

                def finish_tile(t):
                    tb = t % 2
                    O = Ob[tb]
                    for jj in range(4):
                        S.op("act", lambda E: E.activation(out=junk2[:], in_=O[:, jj, :], func=AF.Square,
                                                           accum_out=rst[:, jj:jj + 1]),
                             reads=[("Ob", 0, h) for h in range(8)], writes=["junk2", "rst_ss"])
                        S.op("act", lambda E: E.activation(out=junk2[:], in_=Osw[:, jj, :], func=AF.Square,
                                                           accum_out=rst[:, 4 + jj:5 + jj]),
                             reads=[("Osw", jj)], writes=["junk2", "rst_ss"])
                    S.op("dve", lambda E: E.tensor_scalar(out=rst[:, 8:16], in0=rst[:, 0:8], scalar1=1.0 / 512,
                                                          scalar2=EPS, op0=ALU.mult, op1=ALU.add),
                         reads=["rst_ss"], writes=["rst_ms"])
                    S.op("act", lambda E: E.activation(out=rst[:, 16:24], in_=rst[:, 8:16], func=AF.Sqrt),
                         reads=["rst_ms"], writes=["rst_sd"])
                    S.op("dve", lambda E: E.reciprocal(out=rst[:, 24:32], in_=rst[:, 16:24]),
                         reads=["rst_sd"], writes=["rst_r"])
                    for jj in range(4):
                        S.op("dve", lambda E: E.scalar_tensor_tensor(out=mixb[tb][:, jj, 0:512], in0=O[:, jj, :],
                                                                     scalar=rst[:, 24 + jj:25 + jj], in1=gsb[:],
                                                                     op0=ALU.mult, op1=ALU.mult),
                             reads=[("Ob", 0, h) for h in range(8)] + ["rst_r", "gsb"], writes=[("mixb", 0)])
                        S.op("dve", lambda E: E.scalar_tensor_tensor(out=mixb[tb][:, jj, 512:1024], in0=Osw[:, jj, :],
                                                                     scalar=rst[:, 28 + jj:29 + jj], in1=gsw[:],
                                                                     op0=ALU.mult, op1=ALU.mult),
                             reads=[("Osw", jj), "rst_r", "gsw"], writes=[("mixb", 0)])
                    S.dma("sp", "omix", mix_d[4 * t:4 * t + 4].rearrange("j p f -> p j f"), mixb[tb][:],
                          reads=[("mixb", 0)], writes=[("mix_d", t)])

                load_q(0)
                for t in range(nqt):
                    if t + 1 < nqt:
                        load_q(t + 1)
                    sb_attention(t)
                    for jj in range(4):
                        swa_block(t, jj)
                    finish_tile(t)
                S.barrier()

        if upto == 2:
            with ExitStack() as pd:
                mb = sb("dbg_mb", [128, 1024], BF16, pd)
                mf = sb("dbg_mf", [128, 1024], F32, pd)
                for j in range(4 * nqt):
                    S.dma("sp", "dbg_in", mb[:], mix_d[j], writes=["mb"])
                    S.op("dve", lambda E: E.tensor_copy(out=mf[:], in_=mb[:]), reads=["mb"], writes=["mf"])
                    S.dma("sp", "dbg_out", dbg[j], mf[:], reads=["mf"], writes=["dbgo"])
            S.finish()
            return nc


        ntile = 2 * nqt
        with ExitStack() as p3:
            Wout = sb("Wout", [128, 8, 1024], BF16, p3)
            Wq = sb("Wq", [128, 8, 2048], BF16, p3)
            skT = [sb("skT%d" % i, [128, 8, 128], BF16, p3) for i in range(2)]
            gffn = sb("gffn", [128, 1024], F32, p3)
            gfin = sb("gfin", [128, 1024], F32, p3)
            identf = sb("identf", [128, 128], F32, p3)
            iotaf = sb("iotaf", [128, 16], F32, p3)
            iotab = sb("iotab", [128, 128], BF16, p3)
            mixt = sb("mixt", [128, 2, 1024], BF16, p3)
            GT = sb("GT", [128, 128, 256], BF16, p3)
            xo = sb("xo", [128, 2, 1024], F32, p3)
            x1b = [sb("x1b%d" % i, [128, 2, 1024], F32, p3) for i in range(2)]
            xnTb = [sb("xnTb%d" % i, [128, 8, 256], BF16, p3) for i in range(2)]
            st3 = sb("st3", [128, 16], F32, p3)
            sc = sb("sc", [128, 16, 128], F32, p3)
            scw = sb("scw", [128, 16, 128], F32, p3)
            vv = sb("vv", [128, 16, 16], F32, p3)
            ix = sb("ix", [128, 16, 16], U32, p3)
            tops = sb("tops", [128, 8, 16], F32, p3)
            posu = sb("posu", [128, 8, 16], U32, p3)
            posf3 = sb("posf3", [128, 8, 16], F32, p3)
            thr16 = sb("thr16", [128, 16], F32, p3)
            paf = sb("paf", [128, 8, 16], F32, p3)
            pbf = sb("pbf", [128, 8, 16], F32, p3)
            i1f = sb("i1f", [128, 8, 16], F32, p3)
            i2f = sb("i2f", [128, 8, 16], F32, p3)
            sel = sb("sel", [128, 3, 128], F32, p3)
            gsm = sb("gsm", [128, 32], F32, p3)
            hkT = sb("hkT", [128, 3, 256], F32, p3)
            At = [sb("At%d" % i, [128, 128], BF16, p3) for i in range(3)]
            Bt = [sb("Bt%d" % i, [128, 128], BF16, p3) for i in range(3)]
            dsb = [sb("dsb%d" % i, [128, 8, 128], BF16, p3) for i in range(3)]
            usb = [sb("usb%d" % i, [128, 1024], BF16, p3) for i in range(4)]
            gl = [sb("gl%d" % i, [128, 256], F32, p3) for i in range(3)]
            wg = [sb("wg%d" % i, [128, 256], BF16, p3) for i in range(3)]
            xn = mixt
            qTv = scw[:, 0:8, :].bitcast(BF16).rearrange("p a (b c) -> p (a b) c", c=128)
            cand = sc[:].rearrange("p (h x) k -> p h (x k)", x=2)
            candw = scw[:].rearrange("p (h x) k -> p h (x k)", x=2)
            oh = scw[:].rearrange("p (h x) (y a) -> p h (x y) a", x=2, a=16)

            w_out_v = w_out.rearrange("(c p) n -> p c n", p=128)
            wq_v = peer_wq.rearrange("(c p) n -> p c n", p=128)
            gst = [GT[:, 32 * i:32 * i + 32, :].bitcast(F32).rearrange("p a b -> p (a b)").rearrange("p (c e) -> p c e", c=8)
                   for i in range(4)]
            wl = [(Wout, w_out_v, q4, "Wout") for q4 in range(2)] + [(Wq, wq_v, q4, "Wq") for q4 in range(4)]
            for n_, (wt_, wv_, q4, key_) in enumerate(wl):
                k_ = n_ % 4
                S.dma("sp" if key_ == "Wout" else "act", "w3s%d" % k_, gst[k_], wv_[:, :, q4 * 512:(q4 + 1) * 512], writes=[("gst", k_)])
                S.op(("dve", "pool", "act")[n_ % 3],
                     (lambda E: E.copy(out=wt_[:, :, q4 * 512:(q4 + 1) * 512], in_=gst[k_])) if n_ % 3 == 2 else
                     (lambda E: E.tensor_copy(out=wt_[:, :, q4 * 512:(q4 + 1) * 512], in_=gst[k_])),
                     reads=[("gst", k_)], writes=[key_])
            S.dma("pool", "sk1", skT[0][:], sk1T.rearrange("h d k -> d h k"), writes=["skT"])
            S.dma("pool", "sk2", skT[1][:], sk2T.rearrange("h d k -> d h k"), writes=["skT"])
            S.dma("sp", "gffn", gffn[:], ffn_norm.partition_broadcast(128), writes=["gffn"])
            S.dma("sp", "gfin", gfin[:], final_norm.partition_broadcast(128), writes=["gfin"])
            S.dma("sp", "identf", identf[:], c_ident, writes=["identf"])
            S.dma("sp", "iotaf", iotaf[:], c_iota[:, 0:16], writes=["iotaf"])
            S.dma("pool", "iotab", iotab[:], c_iota, writes=["iotab"])
            S.op("dve", lambda E: E.tensor_scalar(out=thr16[:], in0=iotaf[:, 0:16], scalar1=16.0, scalar2=16.0, op0=ALU.mult, op1=ALU.add), reads=["iotaf"], writes=["thr16"])

            def rms_rows(src, blk, g_t, dst, col, sk, gn, dk):
                S.op("act", lambda E: E.activation(out=xn[:, blk, :], in_=src[:, blk, :], func=AF.Square,
                                                   accum_out=st3[:, col:col + 1]),
                     reads=[sk], writes=[("mixt", blk), ("st3", col)])
                S.op("dve", lambda E: E.tensor_scalar(out=st3[:, col + 1:col + 2], in0=st3[:, col:col + 1],
                                                      scalar1=1.0 / 1024, scalar2=EPS, op0=ALU.mult, op1=ALU.add),
                     reads=[("st3", col)], writes=[("st3", col + 1)])
                S.op("act", lambda E: E.activation(out=st3[:, col + 2:col + 3], in_=st3[:, col + 1:col + 2], func=AF.Sqrt),
                     reads=[("st3", col + 1)], writes=[("st3", col + 2)])
                S.op("dve", lambda E: E.reciprocal(out=st3[:, col + 3:col + 4], in_=st3[:, col + 2:col + 3]),
                     reads=[("st3", col + 2)], writes=[("st3", col + 3)])
                S.op("dve", lambda E: E.scalar_tensor_tensor(out=dst[:, blk, :], in0=src[:, blk, :],
                                                             scalar=st3[:, col + 3:col + 4], in1=g_t[:],
                                                             op0=ALU.mult, op1=ALU.mult),
                     reads=[sk, ("st3", col + 3), gn], writes=[dk])

            def top16(src_ap, work_ap, vals_ap, idx_ap, rk, wk_, vk, ik):
                S.op("dve", lambda E: E.max(out=vals_ap[:, 0:8], in_=src_ap), reads=[rk], writes=[vk])
                S.op("dve", lambda E: E.max_index(out=idx_ap[:, 0:8], in_max=vals_ap[:, 0:8], in_values=src_ap),
                     reads=[rk, vk], writes=[ik])
                S.op("dve", lambda E: E.match_replace(out=work_ap, in_to_replace=vals_ap[:, 0:8], in_values=src_ap,
                                                      imm_value=-1e30), reads=[rk, vk], writes=[wk_])
                S.op("dve", lambda E: E.max(out=vals_ap[:, 8:16], in_=work_ap), reads=[wk_], writes=[vk])
                S.op("dve", lambda E: E.max_index(out=idx_ap[:, 8:16], in_max=vals_ap[:, 8:16], in_values=work_ap),
                     reads=[wk_, vk], writes=[ik])

            def pre_gen(u):
                ub = u % 2
                x1 = x1b[ub]
                xnT = xnTb[ub]
                S.dma("sp", "ldmix", mixt[:], mix_d[2 * u:2 * u + 2].rearrange("j p f -> p j f"),
                      writes=[("mixt", 0), ("mixt", 1)])
                S.dma("sp", "ldxo", xo[:], x_po[2 * u:2 * u + 2, 128:256, :].rearrange("j p f -> p j f"),
                      writes=[("xo", 0), ("xo", 1)])
                yield
                for blk in range(2):
                    for c in range(8):
                        S.op("pe", lambda E: E.transpose(out=PB[:, c, :], in_=mixt[:, blk, c * 128:(c + 1) * 128],
                                                         identity=identb[:]),
                             reads=[("mixt", blk), "identb"], writes=["PB"])
                    S.op("act", lambda E: E.copy(out=xnT[:, :, blk * 128:(blk + 1) * 128], in_=PB[:]),
                         reads=["PB"], writes=[("xnT", ub, blk)])
                    yield
                    for half in range(2):
                        for c in range(8):
                            S.op("pe", lambda E: E.matmul(PF[6][:], lhsT=xnT[:, c, blk * 128:(blk + 1) * 128],
                                                          rhs=Wout[:, c, half * 512:(half + 1) * 512],
                                                          start=(c == 0), stop=(c == 7)),
                                 reads=[("xnT", ub, blk), "Wout"], writes=["PF6"])
                        S.op("dve", lambda E: E.tensor_tensor(out=x1[:, blk, half * 512:(half + 1) * 512], in0=PF[6][:],
                                                              in1=xo[:, blk, half * 512:(half + 1) * 512], op=ALU.add),
                             reads=["PF6", ("xo", blk)], writes=[("x1", ub, blk)])
                        yield
                    rms_rows(x1, blk, gffn, xn, 4 * blk, ("x1", ub, blk), "gffn", ("mixt", blk))
                    yield
                    for c in range(8):
                        S.op("pe", lambda E: E.transpose(out=PB[:, c, :], in_=xn[:, blk, c * 128:(c + 1) * 128],
                                                         identity=identb[:]),
                             reads=[("mixt", blk), "identb"], writes=["PB"])
                    S.op("dve", lambda E: E.tensor_copy(out=xnT[:, :, blk * 128:(blk + 1) * 128], in_=PB[:]),
                         reads=["PB"], writes=[("xnT", ub, blk)])
                    yield
                for blk in range(2):
                    for r4 in range(4):
                        for q in range(4):
                            ch = 4 * r4 + q
                            for c in range(8):
                                S.op("pe", lambda E: E.matmul(PF[6][:, q * 128:(q + 1) * 128],
                                                              lhsT=Wq[:, c, ch * 128:(ch + 1) * 128],
                                                              rhs=xnT[:, c, blk * 128:(blk + 1) * 128],
                                                              start=(c == 0), stop=(c == 7)),
                                     reads=["Wq", ("xnT", ub, blk)], writes=["PF6"])
                        S.op("act", lambda E: E.copy(out=qTv[:, 4 * r4:4 * r4 + 4, :],
                                                     in_=PF[6][:].rearrange("p (a b) -> p a b", a=4)),
                             reads=["PF6"], writes=["scw"])
                        yield
                    for r4 in range(4):
                        for q in range(4):
                            ch = 4 * r4 + q
                            S.op("pe", lambda E: E.matmul(PF[6][:, q * 128:(q + 1) * 128], lhsT=qTv[:, ch, :],
                                                          rhs=skT[ch % 2][:, ch // 2, :], start=True, stop=True),
                                 reads=["scw", "skT"], writes=["PF6"])
                        if r4 % 2 == 0:
                            S.op("act", lambda E: E.copy(out=sc[:, 4 * r4:4 * r4 + 4, :], in_=PF[6][:].rearrange("p (a b) -> p a b", a=4)),
                                 reads=["PF6"], writes=["sc"])
                        else:
                            S.op("dve", lambda E: E.tensor_copy(out=sc[:, 4 * r4:4 * r4 + 4, :], in_=PF[6][:].rearrange("p (a b) -> p a b", a=4)),
                                 reads=["PF6"], writes=["sc"])
                        yield
                    for ch in range(16):
                        top16(sc[:, ch, :], scw[:, ch, :], vv[:, ch, :], ix[:, ch, :], "sc", "scw", "vv", "ix")
                        if ch % 2 == 1:
                            yield
                    vv4 = vv[:].rearrange("p (h s) k -> p h s k", s=2)
                    S.op("dve", lambda E: E.tensor_tensor(
                        out=cand.rearrange("p h (a b) -> p h a b", a=16),
                        in0=vv4[:, :, 0, :].unsqueeze(3).to_broadcast([128, 8, 16, 16]),
                        in1=vv4[:, :, 1, :].unsqueeze(2).to_broadcast([128, 8, 16, 16]), op=ALU.add),
                        reads=["vv"], writes=["sc"])
                    for h in range(8):
                        top16(cand[:, h, :], candw[:, h, :], tops[:, h, :], posu[:, h, :], "sc", "scw", "tops", "posu")
                        if h % 2 == 1:
                            yield
                    S.op("dve", lambda E: E.tensor_tensor(out=sel[:, 2, :].rearrange("p (h k) -> p h k", h=8), in0=tops[:],
                                                          in1=tops[:, :, 0:1].to_broadcast([128, 8, 16]), op=ALU.subtract),
                         reads=["tops"], writes=["gate"])
                    S.op("act", lambda E: E.activation(out=sel[:, 2, :], in_=sel[:, 2, :], func=AF.Exp),
                         reads=["gate"], writes=["gate"])
                    S.op("dve", lambda E: E.tensor_reduce(out=gsm[:, 0:8], in_=sel[:, 2, :].rearrange("p (h k) -> p h k", h=8),
                                                          axis=AX.X, op=ALU.add), reads=["gate"], writes=["gsm"])
                    S.op("dve", lambda E: E.reciprocal(out=gsm[:, 8:16], in_=gsm[:, 0:8]), reads=["gsm"], writes=["gsr"])
                    S.op("dve", lambda E: E.tensor_tensor(out=sel[:, 2, :].rearrange("p (h k) -> p h k", h=8),
                                                          in0=sel[:, 2, :].rearrange("p (h k) -> p h k", h=8),
                                                          in1=gsm[:, 8:16].unsqueeze(2).to_broadcast([128, 8, 16]), op=ALU.mult),
                         reads=["gate", "gsr"], writes=["gate"])
                    yield
                    S.op("dve", lambda E: E.tensor_copy(out=posf3[:], in_=posu[:]), reads=["posu"], writes=["posf3"])
                    for h2 in range(2):
                        hs = slice(4 * h2, 4 * h2 + 4)
                        S.op("dve", lambda E: E.tensor_tensor(
                            out=oh[:, hs].rearrange("p h k a -> p (h k) a"),
                            in0=posf3[:, hs, :].rearrange("p h k -> p (h k)").unsqueeze(2).to_broadcast([128, 64, 16]),
                            in1=thr16[:].unsqueeze(1).to_broadcast([128, 64, 16]), op=ALU.is_ge),
                            reads=["posf3", "thr16"], writes=["scw"])
                        S.op("dve", lambda E: E.tensor_reduce(
                            out=paf[:, hs, :].rearrange("p h k -> p (h k)"),
                            in_=oh[:, hs].rearrange("p h k a -> p (h k) a"), axis=AX.X, op=ALU.add),
                            reads=["scw"], writes=["paf"])
                    S.op("dve", lambda E: E.scalar_tensor_tensor(out=pbf[:], in0=paf[:], scalar=-16.0, in1=posf3[:],
                                                                 op0=ALU.mult, op1=ALU.add),
                         reads=["paf", "posf3"], writes=["pbf"])
                    yield
                    ix4 = ix[:].rearrange("p (h s) k -> p h s k", s=2)
                    S.op("dve", lambda E: E.tensor_copy(out=i1f[:], in_=ix4[:, :, 0, :]), reads=["ix"], writes=["i1f"])
                    S.op("dve", lambda E: E.tensor_copy(out=i2f[:], in_=ix4[:, :, 1, :]), reads=["ix"], writes=["i2f"])
                    for side, (pf_, if_) in enumerate(((paf, i1f), (pbf, i2f))):
                        for h2 in range(2):
                            hs = slice(4 * h2, 4 * h2 + 4)
                            S.op("dve", lambda E: E.tensor_tensor(
                                out=oh[:, hs].rearrange("p h k a -> p (h k) a"),
                                in0=pf_[:, hs, :].rearrange("p h k -> p (h k)").unsqueeze(2).to_broadcast([128, 64, 16]),
                                in1=iotaf[:, 0:16].unsqueeze(1).to_broadcast([128, 64, 16]), op=ALU.is_equal),
                                reads=["paf", "pbf", "iotaf"], writes=["scw"])
                            for h in range(4 * h2, 4 * h2 + 4):
                                S.op("dve", lambda E: E.tensor_tensor(
                                    out=oh[:, h], in0=oh[:, h],
                                    in1=if_[:, h, :].unsqueeze(1).to_broadcast([128, 16, 16]), op=ALU.mult),
                                    reads=["scw", "i1f", "i2f"], writes=["scw"])
                            S.op("dve", lambda E: E.tensor_reduce(
                                out=sel[:, side, 64 * h2:64 * h2 + 64],
                                in_=oh[:, hs].rearrange("p h k a -> p (h k) a"), axis=AX.X, op=ALU.add),
                                reads=["scw"], writes=[("sel", side)])
                            yield
                    for w3 in range(3):
                        S.op("pe", lambda E: E.transpose(out=PF[6][:, w3 * 128:(w3 + 1) * 128], in_=sel[:, w3, :],
                                                         identity=identf[:]),
                             reads=[("sel", 0), ("sel", 1), "gate", "identf"], writes=["PF6"])
                    S.op("act", lambda E: E.copy(out=hkT[:, :, blk * 128:(blk + 1) * 128],
                                                 in_=PF[6][:, 0:384].rearrange("p (a b) -> p a b", a=3)),
                         reads=["PF6"], writes=[("hkT", blk)])
                    yield

            def m_phase(u):
                for t4 in range(64):
                    pf, pfn = PF[5 + t4 % 2], "PF%d" % (5 + t4 % 2)
                    for q in range(4):
                        tk = 4 * t4 + q
                        ab = tk % 3
                        S.op("dve", lambda E: E.tensor_scalar(out=At[ab][:], in0=iotab[:], scalar1=hkT[:, 0, tk:tk + 1],
                                                              scalar2=hkT[:, 2, tk:tk + 1], op0=ALU.is_equal, op1=ALU.mult),
                             reads=["iotab", ("hkT", tk // 128)], writes=[("At", ab)])
                        S.op("dve", lambda E: E.tensor_scalar(out=Bt[ab][:], in0=iotab[:], scalar1=hkT[:, 1, tk:tk + 1],
                                                              scalar2=None, op0=ALU.is_equal),
                             reads=["iotab", ("hkT", tk // 128)], writes=[("Bt", ab)])
                        S.op("pe", lambda E: E.matmul(pf[:, q * 128:(q + 1) * 128], lhsT=Bt[ab][:], rhs=At[ab][:],
                                                      start=True, stop=True),
                             reads=[("At", ab), ("Bt", ab)], writes=[pfn])
                    S.op("act", lambda E: E.copy(out=GT[:, :, 4 * t4:4 * t4 + 4],
                                                 in_=pf[:].rearrange("p (t i) -> p i t", t=4)),
                         reads=[pfn], writes=[("GT", t4)] + ([("gst", t4 % 4)] if (u == 0 and t4 < 4) else []))

            OA = [[PF[0], PF[1]], [PF[2], PF[3]]]
            OAn = [["PF0", "PF1"], ["PF2", "PF3"]]

            def ld_tab(i):
                sl = i % 3
                S.dma("sp", "ldd%d" % sl, dsb[sl][:], down_b[i], writes=[("dsb", sl)])
                su = i % 4
                S.dma("sp", "ldu%d" % su, usb[su][:], up_b[i], writes=[("usb", su)])

            def dense_H(u, i):
                ub = u % 2
                xnT = xnTb[ub]
                sl = i % 3
                hb_, hbn = PF[4 + i % 2], "PF%d" % (4 + i % 2)
                for c in range(8):
                    S.op("pe", lambda E: E.matmul(hb_[:, 0:256], lhsT=dsb[sl][:, c, :],
                                                  rhs=xnT[:, c, :], start=(c == 0), stop=(c == 7)),
                         reads=[("dsb", sl), ("xnT", ub, 0), ("xnT", ub, 1)], writes=[hbn])
                S.op("act", lambda E: E.activation(out=gl[i % 3][:], in_=hb_[:, 0:256], func=AF.Gelu),
                     reads=[hbn], writes=[("gl", i % 3)])
                S.op("pool", lambda E: E.tensor_tensor(out=wg[i % 3][:], in0=gl[i % 3][:], in1=GT[:, i, :], op=ALU.mult),
                     reads=[("gl", i % 3)] + [("GT", t4) for t4 in (0, 63)], writes=[("wg", i % 3)])

            def dense_UP(i):
                su = i % 4
                for blk in range(2):
                    for half in range(2):
                        S.op("pe", lambda E: E.matmul(OA[blk][half][:], lhsT=wg[i % 3][:, blk * 128:(blk + 1) * 128],
                                                      rhs=usb[su][:, half * 512:(half + 1) * 512],
                                                      start=(i == 0), stop=(i == 127)),
                             reads=[("wg", i % 3), ("usb", su)], writes=[OAn[blk][half]])

            def advance(gen):
                if gen is None:
                    return None
                try:
                    next(gen)
                    return gen
                except StopIteration:
                    return None

            def o_phase(u):
                ub = u % 2
                x1 = x1b[ub]
                for blk in range(2):
                    for half in range(2):
                        S.op("dve", lambda E: E.tensor_tensor(out=x1[:, blk, half * 512:(half + 1) * 512], in0=OA[blk][half][:],
                                                              in1=x1[:, blk, half * 512:(half + 1) * 512], op=ALU.add),
                             reads=[OAn[blk][half], ("x1", ub, blk)], writes=[("x1", ub, blk)])
                    rms_rows(x1, blk, gfin, xo, 8 + 4 * blk, ("x1", ub, blk), "gfin", ("xo", blk))
                S.dma("sp", "oout", out[256 * u:256 * u + 256, :].rearrange("(j p) f -> p j f", p=128), xo[:],
                      reads=[("xo", 0), ("xo", 1)], writes=[("out", u)])

            g0 = pre_gen(0)
            while g0 is not None:
                g0 = advance(g0)
            m_phase(0)
            for u in range(ntile):
                nxt = pre_gen(u + 1) if u + 1 < ntile else None
                for i in range(2):
                    ld_tab(i)
                for step in range(130):
                    if step < 128:
                        dense_H(u, step)
                    if step >= 2:
                        dense_UP(step - 2)
                    if step + 2 < 128:
                        ld_tab(step + 2)
                    if step % 2 == 1:
                        nxt = advance(nxt)
                while nxt is not None:
                    nxt = advance(nxt)
                o_phase(u)
                if u + 1 < ntile:
                    m_phase(u + 1)
        S.finish()
    return nc


def own_blocks(half):
    a = (0, 3) if half == 0 else (1, 2)
    return [4 * g + o for g in range(16) for o in a]


def consts(half):
    p = np.arange(128)
    invf = (10000.0 ** (-(p % 32).astype(np.float32) / 32.0)).astype(np.float32).reshape(128, 1)
    sgn = np.where((p % 64) < 32, -1.0, 1.0).astype(np.float32).reshape(128, 1)
    ident = np.eye(128, dtype=np.float32)
    uneg = -(p[:, None] >= p[None, :]).astype(np.float32)
    diag = np.where(p[:, None] < p[None, :], 0.0, NEG).astype(np.float32)
    full = np.full((128, 128), NEG, np.float32)
    none = np.zeros((128, 128), np.float32)
    if half == 0:
        sbm = np.stack([full, diag, diag, none])
    else:
        sbm = np.stack([diag, none, full, diag])
    q = np.arange(128)[:, None] + 128
    k = np.arange(256)[None, :]
    diff = q - k
    band = np.where((diff >= 0) & (diff < 128), 0.0, NEG).astype(np.float32)
    first = band.copy()
    if half == 0:
        first[:, :128] = NEG
    swm = np.stack([first, band])
    iota = np.tile(np.arange(128, dtype=np.float32)[None, :], (128, 1))
    return dict(c_invf=invf, c_sgn=sgn, c_ident=ident, c_uneg=uneg, c_sbm=sbm, c_swm=swm, c_iota=iota)


def prep_core(inp, core):
    b, half = core // 2, core % 2
    x = np.asarray(inp["x"][b], dtype=np.float32)
    pos = np.asarray(inp["positions"][b]).astype(np.int32)
    own = own_blocks(half)
    xb = x.reshape(64, 128, 1024)
    pb = pos.reshape(64, 128)
    x_po = np.zeros((32, 256, 1024), np.float32)
    pos_po = np.zeros((32, 256), np.int32)
    for j, B in enumerate(own):
        if B > 0:
            x_po[j, :128] = xb[B - 1]
            pos_po[j, :128] = pb[B - 1]
        x_po[j, 128:] = xb[B]
        pos_po[j, 128:] = pb[B]
    m = dict(
        x_all=np.ascontiguousarray(x), x_po=x_po, pos_po=pos_po.reshape(-1),
        w_in=np.ascontiguousarray(inp["w_in"][0]), attn_norm=np.ascontiguousarray(inp["attn_norm"][0]),
        sb_out_norm=np.ascontiguousarray(inp["sb_out_norm"][0]), swa_sinks=np.ascontiguousarray(inp["swa_sinks"][0]),
        swa_out_norm=np.ascontiguousarray(inp["swa_out_norm"][0]), w_out=np.ascontiguousarray(inp["w_out"][0]),
        ffn_norm=np.ascontiguousarray(inp["ffn_norm"][0]), peer_wq=np.ascontiguousarray(inp["peer_w_query"][0]),
        sk1T=np.ascontiguousarray(np.transpose(inp["peer_sub_keys_1"][0], (0, 2, 1))),
        sk2T=np.ascontiguousarray(np.transpose(inp["peer_sub_keys_2"][0], (0, 2, 1))),
        final_norm=np.ascontiguousarray(inp["final_norm"]),
    )
    m.update(consts(half))
    return m


from concourse.bass_utils import run_bass_kernel_spmd

_NC = None


def kernel(**inputs):
    global _NC
    inp = {k: np.asarray(v) for k, v in inputs.items()}
    if _NC is None:
        _NC = build(upto=3, nqt=8, nblk1a=64)
    downT = np.ascontiguousarray(inp["peer_expert_down"][0].T)
    up = np.ascontiguousarray(inp["peer_expert_up"][0])
    maps = []
    for c in range(8):
        m = prep_core(inp, c)
        m["downT"] = downT
        m["up"] = up
        maps.append(m)
    res = run_bass_kernel_spmd(_NC, maps, core_ids=list(range(8)))
    out = np.zeros((4, 8192, 1024), np.float32)
    for c in range(8):
        o = np.asarray(res.results[c]["out"]).reshape(32, 128, 1024)
        ob = out[c // 2].reshape(64, 128, 1024)
        for j, B in enumerate(own_blocks(c % 2)):
            ob[B] = o[j]
    return out
```
